# Optimizing a Trainium2 kernel written in Bass

```python
import math
import jax
import jax.numpy as jnp
from jax import lax
import numpy as np

D_MODEL = 1024
BATCH = 4
SEQ = 8192
DEPTH = 2

HEAD_DIM = 64
Q_BLOCK = 128
N_FOX_HEADS = 8
DIL_PAIRS = ((128, 1), (512, 4), (2048, 16))
N_DIL_GROUPS = 3
N_DIL_SLOTS = 4
N_DIL_HEADS = N_DIL_GROUPS * N_DIL_SLOTS
N_DSA_HEADS = 16
KV_RANK = 256
N_IDX_HEADS = 8
IDX_DIM = 64
DSA_TOPK = 256
N_BUCKETS = 32
MAX_DISTANCE = 2048
N_BIAS_HEADS = 16
N_EXPERTS = 32
TOP_K = 4
D_EXPERT = 1024
SWIGLU_ALPHA = 1.702
SWIGLU_LIMIT = 7.0
MOE_CHUNK = 256
EPS = 1e-6

FOX_QKV = 3 * N_FOX_HEADS * HEAD_DIM
DIL_QKV = 3 * N_DIL_HEADS * HEAD_DIM
EVEN_IN = FOX_QKV + N_FOX_HEADS + DIL_QKV
EVEN_OUT = (N_FOX_HEADS + N_DIL_SLOTS) * HEAD_DIM
DSA_Q = N_DSA_HEADS * HEAD_DIM
IDX_Q = N_IDX_HEADS * IDX_DIM
ODD_IN = DSA_Q + KV_RANK + IDX_Q + IDX_DIM + N_IDX_HEADS
ODD_OUT = DSA_Q

kernel_name = "hybrid_fox_dilated_dsa_moe_adaln"


def rmsnorm(x, g):
    xf = x.astype(jnp.float32)
    y = xf * lax.rsqrt(jnp.mean(xf * xf, axis=-1, keepdims=True) + EPS)
    return (y * g.astype(jnp.float32)).astype(x.dtype)


def t5_bucket(dist):
    max_exact = N_BUCKETS // 2
    d = jnp.maximum(dist, 0)
    df = jnp.maximum(d, 1).astype(jnp.float32)
    large = max_exact + (jnp.log(df / max_exact) / math.log(MAX_DISTANCE / max_exact)
                         * (N_BUCKETS - max_exact)).astype(jnp.int32)
    large = jnp.minimum(large, N_BUCKETS - 1)
    return jnp.where(d < max_exact, d, large)


def forgetting_attention(q, k, v, log_f):
    b, s, h, hd = q.shape
    scale = hd ** -0.5
    cum = jnp.cumsum(log_f, axis=1).transpose(0, 2, 1)
    kpos = jnp.arange(s)

    def block(i):
        start = i * Q_BLOCK
        qpos = start + jnp.arange(Q_BLOCK)
        qb = lax.dynamic_slice_in_dim(q, start, Q_BLOCK, axis=1)
        cq = lax.dynamic_slice_in_dim(cum, start, Q_BLOCK, axis=2)
        sc = jnp.einsum('bqhd,bkhd->bhqk', qb, k, preferred_element_type=jnp.float32) * scale
        sc = sc + (cq[..., :, None] - cum[..., None, :])
        sc = jnp.where(kpos[None, :] <= qpos[:, None], sc, -jnp.inf)
        p = jax.nn.softmax(sc, axis=-1)
        return jnp.einsum('bhqk,bkhd->bqhd', p.astype(v.dtype), v)

    out = lax.map(block, jnp.arange(s // Q_BLOCK))
    return out.transpose(1, 0, 2, 3, 4).reshape(b, s, h, hd)


def dilated_attention(q, k, v, rel_bias):
    b, s, _, _, hd = q.shape
    scale = hd ** -0.5
    ks = [k[:, :, g] for g in range(N_DIL_GROUPS)]
    vs = [v[:, :, g] for g in range(N_DIL_GROUPS)]
    offsets, biases = [], []
    for g, (window, dil) in enumerate(DIL_PAIRS):
        dist = jnp.arange(window // dil + 1) * dil
        offsets.append(dist)
        tab = rel_bias[t5_bucket(dist)][:, g * N_DIL_SLOTS:(g + 1) * N_DIL_SLOTS]
        biases.append(tab.T.astype(jnp.float32))

    def block(i):
        start = i * Q_BLOCK
        qpos = start + jnp.arange(Q_BLOCK)
        qb = lax.dynamic_slice_in_dim(q, start, Q_BLOCK, axis=1)
        outs, lses = [], []
        for g in range(N_DIL_GROUPS):
            kp = qpos[:, None] - offsets[g][None, :]
            valid = kp >= 0
            kidx = jnp.maximum(kp, 0)
            kg = ks[g][:, kidx]
            vg = vs[g][:, kidx]
            sc = jnp.einsum('bqjd,bqnjd->bjqn', qb[:, :, g], kg,
                            preferred_element_type=jnp.float32) * scale
            sc = jnp.where(valid[None, None], sc + biases[g][None, :, None, :], -jnp.inf)
            mx = jnp.max(sc, axis=-1, keepdims=True)
            e = jnp.exp(sc - mx)
            den = jnp.sum(e, axis=-1, keepdims=True)
            outs.append(jnp.einsum('bjqn,bqnjd->bqjd', (e / den).astype(vg.dtype), vg))
            lses.append((mx + jnp.log(den))[..., 0])
        alpha = jax.nn.softmax(jnp.stack(lses), axis=0)
        return sum(alpha[g].transpose(0, 2, 1)[..., None].astype(outs[g].dtype) * outs[g]
                   for g in range(N_DIL_GROUPS))

    out = lax.map(block, jnp.arange(s // Q_BLOCK))
    return out.transpose(1, 0, 2, 3, 4).reshape(b, s, N_DIL_SLOTS, hd)


def dsa_attention(q, k, v, q_idx, k_idx, w_idx, rel_bias):
    b, s, h, hd = q.shape
    n_sel = min(DSA_TOPK, s // 4)
    scale = hd ** -0.5
    idx_scale = (N_IDX_HEADS * IDX_DIM) ** -0.5
    kpos = jnp.arange(s)

    def block(i):
        start = i * Q_BLOCK
        qpos = start + jnp.arange(Q_BLOCK)
        qi = lax.dynamic_slice_in_dim(q_idx, start, Q_BLOCK, axis=1)
        wi = lax.dynamic_slice_in_dim(w_idx, start, Q_BLOCK, axis=1)
        dots = jnp.einsum('bqhd,bkd->bqhk', qi, k_idx, preferred_element_type=jnp.float32)
        score = jnp.einsum('bqh,bqhk->bqk', wi.astype(jnp.float32), jax.nn.relu(dots)) * idx_scale
        score = jnp.where(kpos[None, None, :] <= qpos[None, :, None], score, -jnp.inf)
        _, sel = lax.top_k(score, n_sel)
        valid = sel <= qpos[None, :, None]
        kb = jax.vmap(lambda kk, ss: kk[ss])(k, sel)
        vb = jax.vmap(lambda vv, ss: vv[ss])(v, sel)
        qb = lax.dynamic_slice_in_dim(q, start, Q_BLOCK, axis=1)
        sc = jnp.einsum('bqhd,bqnhd->bhqn', qb, kb, preferred_element_type=jnp.float32) * scale
        bias = rel_bias[t5_bucket(qpos[None, :, None] - sel)].astype(jnp.float32)
        sc = jnp.where(valid[:, None], sc + bias.transpose(0, 3, 1, 2), -jnp.inf)
        p = jax.nn.softmax(sc, axis=-1)
        return jnp.einsum('bhqn,bqnhd->bqhd', p.astype(vb.dtype), vb)

    out = lax.map(block, jnp.arange(s // Q_BLOCK))
    return out.transpose(1, 0, 2, 3, 4).reshape(b, s, h, hd)


def even_mixer(h, w_in, fox_fb, w_out, rel_bias):
    b, s, _ = h.shape
    proj = h @ w_in
    fox_qkv, fox_f, dil_qkv = jnp.split(proj, [FOX_QKV, FOX_QKV + N_FOX_HEADS], axis=-1)
    fq, fk, fv = (t.reshape(b, s, N_FOX_HEADS, HEAD_DIM) for t in jnp.split(fox_qkv, 3, axis=-1))
    log_f = jax.nn.log_sigmoid((fox_f + fox_fb).astype(jnp.float32))
    fox_out = forgetting_attention(fq, fk, fv, log_f)
    dq, dk, dv = (t.reshape(b, s, N_DIL_GROUPS, N_DIL_SLOTS, HEAD_DIM)
                  for t in jnp.split(dil_qkv, 3, axis=-1))
    dil_out = dilated_attention(dq, dk, dv, rel_bias)
    merged = jnp.concatenate([fox_out.reshape(b, s, -1), dil_out.reshape(b, s, -1)], axis=-1)
    return merged @ w_out


def odd_mixer(h, w_in, kv_norm, w_ukv, w_out, rel_bias):
    b, s, _ = h.shape
    proj = h @ w_in
    q, ckv, qi, ki, wi = jnp.split(
        proj, [DSA_Q, DSA_Q + KV_RANK, DSA_Q + KV_RANK + IDX_Q, DSA_Q + KV_RANK + IDX_Q + IDX_DIM],
        axis=-1)
    kv = rmsnorm(ckv, kv_norm) @ w_ukv
    k, v = (t.reshape(b, s, N_DSA_HEADS, HEAD_DIM) for t in jnp.split(kv, 2, axis=-1))
    q = q.reshape(b, s, N_DSA_HEADS, HEAD_DIM)
    qi = qi.reshape(b, s, N_IDX_HEADS, IDX_DIM)
    out = dsa_attention(q, k, v, qi, ki, wi, rel_bias)
    return out.reshape(b, s, -1) @ w_out


def moe_ffn(h, router_w, router_b, w1, b1, w2, b2):
    b, s, d = h.shape
    xt = h.reshape(-1, d)
    n = xt.shape[0]
    logits = (xt @ router_w + router_b).astype(jnp.float32)
    top_vals, top_idx = lax.top_k(logits, TOP_K)
    gates = jax.nn.softmax(top_vals, axis=-1)
    flat_e = top_idx.reshape(-1)
    flat_tok = jnp.repeat(jnp.arange(n, dtype=jnp.int32), TOP_K)
    flat_g = gates.reshape(-1)
    order = jnp.argsort(flat_e)
    e_sorted, tok_sorted, g_sorted = flat_e[order], flat_tok[order], flat_g[order]
    counts = jnp.bincount(flat_e, length=N_EXPERTS)
    padded = ((counts + MOE_CHUNK - 1) // MOE_CHUNK) * MOE_CHUNK
    start_raw = jnp.cumsum(counts) - counts
    ends_pad = jnp.cumsum(padded)
    start_pad = ends_pad - padded
    dest = start_pad[e_sorted] + (jnp.arange(n * TOP_K) - start_raw[e_sorted])
    n_chunks = -(-(n * TOP_K) // MOE_CHUNK) + N_EXPERTS
    rows = n_chunks * MOE_CHUNK
    row_tok = jnp.full((rows,), n, dtype=jnp.int32).at[dest].set(tok_sorted)
    row_gate = jnp.zeros((rows,), jnp.float32).at[dest].set(g_sorted)
    chunk_exp = jnp.minimum(
        jnp.searchsorted(ends_pad, jnp.arange(n_chunks) * MOE_CHUNK, side='right'), N_EXPERTS - 1)
    x_pad = jnp.concatenate([xt, jnp.zeros((1, d), xt.dtype)], axis=0)

    def run_chunk(args):
        toks, e = args
        hm = x_pad[toks] @ w1[e] + b1[e]
        glu = jnp.minimum(hm[:, :D_EXPERT], SWIGLU_LIMIT)
        lin = jnp.clip(hm[:, D_EXPERT:], -SWIGLU_LIMIT, SWIGLU_LIMIT)
        act = glu * jax.nn.sigmoid(SWIGLU_ALPHA * glu) * (lin + 1.0)
        return act @ w2[e] + b2[e]

    y = lax.map(run_chunk, (row_tok.reshape(n_chunks, MOE_CHUNK), chunk_exp))
    y = y.reshape(rows, d) * row_gate[:, None].astype(y.dtype)
    out = jax.ops.segment_sum(y, row_tok, num_segments=n + 1)[:n]
    return out.reshape(b, s, d)


def setup_inputs(seed: int = 0) -> dict:
    key = jax.random.key(seed)
    ks = iter(jax.random.split(key, 40))
    nrm = lambda shape, sc: jax.random.normal(next(ks), shape, jnp.float32) * sc
    d = D_MODEL
    return {
        "x": nrm((BATCH, SEQ, d), 1.0),
        "c": nrm((BATCH, d), 1.0),
        "rel_bias": nrm((N_BUCKETS, N_BIAS_HEADS), 0.5),
        "l0_norm1": 1.0 + nrm((d,), 0.02),
        "l0_ada_w": nrm((d, 6 * d), 0.5 * d ** -0.5),
        "l0_ada_b": nrm((6 * d,), 0.02),
        "l0_w_in": nrm((d, EVEN_IN), d ** -0.5),
        "l0_fox_fb": 4.0 + nrm((N_FOX_HEADS,), 0.5),
        "l0_w_out": nrm((EVEN_OUT, d), EVEN_OUT ** -0.5),
        "l0_norm2": 1.0 + nrm((d,), 0.02),
        "l0_router_w": nrm((d, N_EXPERTS), d ** -0.5),
        "l0_router_b": nrm((N_EXPERTS,), 0.01),
        "l0_w1": nrm((N_EXPERTS, d, 2 * D_EXPERT), d ** -0.5),
        "l0_b1": nrm((N_EXPERTS, 2 * D_EXPERT), 0.01),
        "l0_w2": nrm((N_EXPERTS, D_EXPERT, d), D_EXPERT ** -0.5),
        "l0_b2": nrm((N_EXPERTS, d), 0.01),
        "l1_norm1": 1.0 + nrm((d,), 0.02),
        "l1_ada_w": nrm((d, 6 * d), 0.5 * d ** -0.5),
        "l1_ada_b": nrm((6 * d,), 0.02),
        "l1_w_in": nrm((d, ODD_IN), d ** -0.5),
        "l1_kv_norm": 1.0 + nrm((KV_RANK,), 0.02),
        "l1_w_ukv": nrm((KV_RANK, 2 * N_DSA_HEADS * HEAD_DIM), KV_RANK ** -0.5),
        "l1_w_out": nrm((ODD_OUT, d), ODD_OUT ** -0.5),
        "l1_norm2": 1.0 + nrm((d,), 0.02),
        "l1_router_w": nrm((d, N_EXPERTS), d ** -0.5),
        "l1_router_b": nrm((N_EXPERTS,), 0.01),
        "l1_w1": nrm((N_EXPERTS, d, 2 * D_EXPERT), d ** -0.5),
        "l1_b1": nrm((N_EXPERTS, 2 * D_EXPERT), 0.01),
        "l1_w2": nrm((N_EXPERTS, D_EXPERT, d), D_EXPERT ** -0.5),
        "l1_b2": nrm((N_EXPERTS, d), 0.01),
        "final_norm": 1.0 + nrm((d,), 0.02),
    }


def reference(x, c, rel_bias,
              l0_norm1, l0_ada_w, l0_ada_b, l0_w_in, l0_fox_fb, l0_w_out,
              l0_norm2, l0_router_w, l0_router_b, l0_w1, l0_b1, l0_w2, l0_b2,
              l1_norm1, l1_ada_w, l1_ada_b, l1_w_in, l1_kv_norm, l1_w_ukv, l1_w_out,
              l1_norm2, l1_router_w, l1_router_b, l1_w1, l1_b1, l1_w2, l1_b2,
              final_norm):
    even_params = (l0_w_in, l0_fox_fb, l0_w_out)
    odd_params = (l1_w_in, l1_kv_norm, l1_w_ukv, l1_w_out)
    common = (
        (l0_norm1, l0_ada_w, l0_ada_b, l0_norm2, l0_router_w, l0_router_b, l0_w1, l0_b1, l0_w2, l0_b2),
        (l1_norm1, l1_ada_w, l1_ada_b, l1_norm2, l1_router_w, l1_router_b, l1_w1, l1_b1, l1_w2, l1_b2),
    )
    for i in range(DEPTH):
        norm1, ada_w, ada_b, norm2, rw, rb, w1, b1, w2, b2 = common[i]
        mods = (jax.nn.silu(c) @ ada_w + ada_b)[:, None, :]
        shift1, scale1, gate1, shift2, scale2, gate2 = jnp.split(mods, 6, axis=-1)
        h = rmsnorm(x, norm1) * (1.0 + scale1) + shift1
        if i % 2 == 0:
            mix = even_mixer(h, *even_params, rel_bias)
        else:
            mix = odd_mixer(h, *odd_params, rel_bias)
        x = x + gate1 * mix
        h = rmsnorm(x, norm2) * (1.0 + scale2) + shift2
        x = x + gate2 * moe_ffn(h, rw, rb, w1, b1, w2, b2)
    return rmsnorm(x, final_norm)
```

```python
import math
from contextlib import ExitStack
import numpy as np
import concourse.bass as bass
import concourse.mybir as mybir
from concourse.bass_utils import run_bass_kernel_spmd

F32, BF16 = mybir.dt.float32, mybir.dt.bfloat16
ALU = mybir.AluOpType
AF = mybir.ActivationFunctionType
AX = mybir.AxisListType

NDS = 12
ROT = 24000
NEG = -30000.0
D = 1024
NE = 32
EPS = 1e-6


class Dep:
    __slots__ = ("lw", "rd")

    def __init__(self):
        self.lw = None
        self.rd = {}


class Tl:
    __slots__ = ("t", "d")

    def __init__(self, t):
        self.t = t
        self.d = Dep()


class KB:
    def __init__(self, nc, es):
        self.nc = nc
        self.es = es
        self.eng = {"pe": nc.tensor, "dve": nc.vector, "act": nc.scalar, "pool": nc.gpsimd, "sp": nc.sync}
        self.sem = {}
        self.cnt = {}
        self.nsem = 0
        self.final_val = {}
        for e in self.eng:
            self.sem[e] = self._newsem("e_" + e)
            self.cnt[e] = 0
        self.waited = {e: {} for e in self.eng}
        self.dq = ("sp", "pool")
        self.dsem = {q: [self._newsem(f"d_{q}{i}") for i in range(NDS)] for q in self.dq}
        self.dval = {q: [0] * NDS for q in self.dq}
        self.dnext = {q: 0 for q in self.dq}
        self.ninst = 0
        self.nt = 0

    def _newsem(self, name):
        self.nsem += 1
        s = self.es.enter_context(self.nc.semaphore(f"{name}_{self.nsem}"))
        self.final_val[s] = 0
        return s

    def _waitv(self, e, s, v):
        if v <= 0 or self.waited[e].get(s, 0) >= v:
            return
        self.eng[e].wait_ge(s, v)
        self.waited[e][s] = v

    def _deps(self, e, r, w):
        need = {}
        for d in r:
            if d.lw is not None:
                s, v = d.lw
                if need.get(s, 0) < v:
                    need[s] = v
        for d in w:
            if d.lw is not None:
                s, v = d.lw
                if need.get(s, 0) < v:
                    need[s] = v
            for s, v in d.rd.items():
                if need.get(s, 0) < v:
                    need[s] = v
        for s, v in need.items():
            if e == "pe" and s is self.sem["pe"]:
                continue
            self._waitv(e, s, v)

    @staticmethod
    def _dl(x):
        return [a.d if isinstance(a, Tl) else a for a in x]

    def op(self, e, fn, r=(), w=()):
        r = self._dl(r)
        w = self._dl(w)
        if self.cnt[e] >= ROT:
            self.sem[e] = self._newsem("e_" + e)
            self.cnt[e] = 0
        self._deps(e, r, w)
        ins = fn(self.eng[e])
        self.cnt[e] += 1
        s = self.sem[e]
        ins.then_inc(s, 1)
        self.final_val[s] = self.cnt[e]
        self.ninst += 1
        for d in r:
            d.rd[s] = self.cnt[e]
        for d in w:
            d.lw = (s, self.cnt[e])
            d.rd = {}

    def dma(self, q, out, in_, r=(), w=()):
        r = self._dl(r)
        w = self._dl(w)
        i = self.dnext[q]
        self.dnext[q] = (i + 1) % NDS
        s = self.dsem[q][i]
        self._waitv(q, s, self.dval[q][i])
        self._deps(q, r, w)
        ins = self.eng[q].dma_start(out=out, in_=in_)
        self.dval[q][i] += 16
        v = self.dval[q][i]
        ins.then_inc(s, 16)
        self.final_val[s] = v
        self.ninst += 1
        for d in r:
            d.rd[s] = v
        for d in w:
            d.lw = (s, v)
            d.rd = {}

    def barrier(self):
        for e in self.eng:
            for s, v in self.final_val.items():
                self._waitv(e, s, v)

    def T(self, es, shape, dt, name=None):
        self.nt += 1
        t = es.enter_context(self.nc.sbuf_tensor(f"{name or 't'}_{self.nt}", list(shape), dt))
        return Tl(t)


class Rot:
    def __init__(self, items):
        self.items = items
        self.i = 0

    def next(self):
        x = self.items[self.i % len(self.items)]
        self.i += 1
        return x


def t5_bucket_np(dist):
    nb, md = 32, 2048
    me = nb // 2
    d = np.maximum(dist, 0)
    df = np.maximum(d, 1).astype(np.float32)
    large = me + (np.log(df / me) / math.log(md / me) * (nb - me)).astype(np.int32)
    large = np.minimum(large, nb - 1)
    return np.where(d < me, d, large)


DIL_PAIRS = ((128, 1), (512, 4), (2048, 16))


def toep_geom(emin, emax):
    dmin = 256 * emin - 127 - 128
    dmax = 256 * (emax + 3) + 127 + 128
    return dmin, dmax - dmin + 1


def dil_evals(window, part_shift_opts=(-128, 128)):
    own, oth = [], []
    for e in range(-3, 16):
        ok_own = ok_oth = False
        for a in range(4):
            lo = 256 * (a + e) - 127
            hi = 256 * (a + e) + 127
            if hi >= 0 and lo <= window:
                ok_own = True
            for sh in part_shift_opts:
                if hi + sh >= 0 and lo + sh <= window:
                    ok_oth = True
        if ok_own:
            own.append(e)
        if ok_oth:
            oth.append(e)
    return own, oth


FOX_E = [-3, -2, -1, 0]
DSA_NEAR_E = list(range(-3, 10))


def true_tile(st, hf, NH):
    return 2 * st + hf if st < NH else 2 * (st - NH) + 1 - hf


def sigma_perm(S, hf):
    NT = S // 128
    NH = NT // 2
    idx = []
    for st in range(NT):
        T = true_tile(st, hf, NH)
        idx.extend(range(T * 128, T * 128 + 128))
    return np.array(idx)


def fox_R(hf):
    tpos = np.zeros(1024, np.int64)
    for tt in range(8):
        T = (2 * tt + hf) if tt < 4 else (2 * (tt - 4) + 1 - hf)
        tpos[tt * 128:(tt + 1) * 128] = T * 128 + np.arange(128)
    R = (tpos[:, None] <= tpos[None, :]).astype(np.float32)
    return R.reshape(8, 128, 1024)


def build_layer(layer, S, dbg=False, stop_after=99, cut='', ctx=None, tag='', shared=None, xT_in=None, out_ap=None,
                pervar=()):
    NT = S // 128
    NH = NT // 2
    NQ = NH // 4
    NG = S // 512
    SO = S // 2
    if ctx is None:
        nc = bass.Bass("TRN2", target_bir_lowering=False)
        es = ExitStack()
        kb = KB(nc, es)
        PS = [Tl(es.enter_context(nc.psum_tensor(f"ps{i}", [128, 512], F32))) for i in range(7)]
    else:
        nc, es, kb, PS = ctx
    if shared is None:
        shared = {}

    def din(name, shape):
        key = (tag + name) if (name in pervar or not tag) else ("w%d_" % layer + name)
        if key not in shared:
            shared[key] = nc.dram_tensor(key, list(shape), F32, kind="ExternalInput").ap()
        return shared[key]

    def dscr(name, shape, dt):
        return nc.dram_tensor(tag + name, list(shape), dt, kind=("ExternalOutput" if dbg else "Internal")).ap()

    WIN = 3848 if layer == 0 else 1864
    xT = xT_in if xT_in is not None else din("xT", [D, S])
    ccol = din("ccol", [128, 8])
    ada_w = din("ada_w", [D, 6 * D])
    adab = din("adab", [128, 48])
    n1col = din("n1col", [128, 8])
    n2col = din("n2col", [128, 8])
    w_in = din("w_in", [D, WIN])
    NMO = 768 if layer == 0 else 1024
    w_out = din("w_out", [NMO, D])
    rw = din("rw", [D, NE])
    rbb = din("rbb", [128, NE])
    w1 = din("w1", [NE, D, 2 * D])
    b1col = din("b1col", [128, NE * 16])
    w2 = din("w2", [NE, D, D])
    b2 = din("b2", [NE, D])
    identD = din("ident", [128, 128])
    selD = din("sel", [NE, NE * 128])
    rbflat = din("rbflat", [128, 512])
    if layer == 0:
        fbb = din("fbb", [128, 8])
        RD = din("R", [8, 128, 1024])
        dgeo = []
        for (w_, r_) in DIL_PAIRS:
            eo, et = dil_evals(w_)
            dgeo.append((eo, et) + toep_geom(min(eo + et), max(eo + et)))
        fgeo = toep_geom(-3, 0)
        FcD = din("Fc", [2, fgeo[1]])
        FdD = [din(f"Fd{g}", [8, dgeo[g][3]]) for g in range(3)]
        FcR = dscr("FcR", [2, 128, fgeo[1]], F32)
        FdR = [dscr(f"FdR{g}", [8, 128, dgeo[g][3]], F32) for g in range(3)]
    else:
        kvn = din("kvn", [128, 2])
        w_ukv = din("w_ukv", [256, 2048])
        fnc = din("fncol", [128, 8])
        trimD = din("trim", [128, 128])
        othdD = din("othd", [128, 128])
        pow2D = din("pow2", [128, 32])
        sgeo = toep_geom(-3, DSA_NEAR_E[-1])
        FsD = din("Fs", [32, sgeo[1]])
        FsR = dscr("FsR", [32, 128, sgeo[1]], F32)
        rbfar = din("rbfar", [128, 16])
    outD = out_ap if out_ap is not None else nc.dram_tensor(tag + "out", [D, SO], F32, kind="ExternalOutput").ap()

    MT = dscr("MT", [NMO, SO], BF16)
    X1 = dscr("X1", [D, SO], F32)
    H2 = dscr("H2", [D, SO], BF16)
    GTs = dscr("GTs", [NE, SO], F32)
    if layer == 0:
        KAF = dscr("KAF", [8, 72, S], BF16)
        QAF = dscr("QAF", [8, 72, SO], BF16)
        VF = dscr("VF", [8, 128, NT, 128], BF16)
        KAD = dscr("KAD", [12, 72, S], BF16)
        QAD = dscr("QAD", [12, 72, SO], BF16)
        VD = dscr("VD", [12, 128, NT, 128], BF16)
    else:
        KAS = dscr("KAS", [16, 72, S], BF16)
        QAS = dscr("QAS", [16, 72, SO], BF16)
        VS = dscr("VS", [16, 128, NT, 128], BF16)
        KI = dscr("KI", [128, S], BF16)
        QI = dscr("QI", [512, SO], BF16)
        WI = dscr("WI", [SO, 128], F32)
        NM = dscr("NM", [NQ, NT, 128, 512], BF16)


    kb.PS = PS
    kb.shared = shared
    if not hasattr(kb, "P0t"):
        P0 = ExitStack()
        es.enter_context(P0)
        identf = kb.T(P0, [128, 128], F32)
        identb = kb.T(P0, [128, 128], BF16)
        onesf = kb.T(P0, [128, 128], F32)
        onesb = kb.T(P0, [128, 512], BF16)
        ind2 = kb.T(P0, [128, 2], BF16)
        mods = kb.T(P0, [128, 48], F32)
        gs1 = kb.T(P0, [128, 8], F32)
        gs2 = kb.T(P0, [128, 8], F32)
        bmax = kb.T(P0, [128, 1], F32)
        zcol = kb.T(P0, [128, 1], F32)
        kb.op("dve", lambda e: e.memset(zcol.t[:], 0.0), w=[zcol])
        kb.dma("sp", identf.t[:], identD[:, :], w=[identf])
        kb.op("dve", lambda e: e.tensor_copy(out=identb.t[:], in_=identf.t[:]), r=[identf], w=[identb])
        kb.op("dve", lambda e: e.memset(onesf.t[:], 1.0), w=[onesf])
        kb.op("dve", lambda e: e.memset(onesb.t[:], 1.0), w=[onesb])
        kb.op("dve", lambda e: e.memset(ind2.t[:], 0.0), w=[ind2])
        kb.op("dve", lambda e: e.memset(ind2.t[0:64, 0:1], 1.0), w=[ind2])
        kb.op("dve", lambda e: e.memset(ind2.t[64:128, 1:2], 1.0), w=[ind2])
        kb.P0t = (identf, identb, onesf, onesb, ind2, mods, gs1, gs2, bmax, zcol)
    identf, identb, onesf, onesb, ind2, mods, gs1, gs2, bmax, zcol = kb.P0t

    with ExitStack() as ph:
        cc = kb.T(ph, [128, 8], F32)
        sc = kb.T(ph, [128, 8], F32)
        ab = kb.T(ph, [128, 48], F32)
        n1 = kb.T(ph, [128, 8], F32)
        n2 = kb.T(ph, [128, 8], F32)
        rbf = kb.T(ph, [128, 512], F32)
        kb.dma("sp", cc.t[:], ccol[:, :], w=[cc])
        kb.dma("sp", ab.t[:], adab[:, :], w=[ab])
        kb.dma("sp", n1.t[:], n1col[:, :], w=[n1])
        kb.dma("sp", n2.t[:], n2col[:, :], w=[n2])
        kb.dma("sp", rbf.t[:], rbflat[:, :], w=[rbf])
        kb.op("dve", lambda e: e.reduce_max(out=bmax.t[:], in_=rbf.t[:], axis=AX.X), r=[rbf], w=[bmax])
        kb.op("act", lambda e: e.activation(out=sc.t[:], in_=cc.t[:], func=AF.Silu), r=[cc], w=[sc])
        aw = Rot([kb.T(ph, [128, 8, 1024], F32) for _ in range(2)])
        awv = ada_w.rearrange("(k p) f -> p k f", p=128)
        psm = PS[0]
        for j in range(6):
            a = aw.next()
            kb.dma("sp", a.t[:], awv[:, :, j * 1024:(j + 1) * 1024], w=[a])
            for fc in range(8):
                for k in range(8):
                    kb.op("pe", lambda e, a=a, fc=fc, k=k, j=j: e.matmul(
                        psm.t[:, j * 8 + fc:j * 8 + fc + 1], lhsT=a.t[:, k, fc * 128:(fc + 1) * 128],
                        rhs=sc.t[:, k:k + 1], start=(k == 0), stop=(k == 7)), r=[a, sc], w=[psm])
        kb.op("dve", lambda e: e.tensor_tensor(out=mods.t[:], in0=psm.t[:, 0:48], in1=ab.t[:], op=ALU.add),
              r=[psm, ab], w=[mods])
        kb.op("dve", lambda e: e.scalar_tensor_tensor(out=gs1.t[:], in0=mods.t[:, 8:16], scalar=1.0, in1=n1.t[:],
                                                     op0=ALU.add, op1=ALU.mult), r=[mods, n1], w=[gs1])
        kb.op("dve", lambda e: e.scalar_tensor_tensor(out=gs2.t[:], in0=mods.t[:, 32:40], scalar=1.0, in1=n2.t[:],
                                                     op0=ALU.add, op1=ALU.mult), r=[mods, n2], w=[gs2])
        kb.barrier()
    SH1, GT1, SH2, GT2 = 0, 16, 24, 40
    if stop_after <= 0:
        return nc, es, kb

    def rms_mod(ph_tiles, xin, gs, sh_off, hout, hf32=None):
        sqr, rstd, tmpr, pss = ph_tiles
        ps = pss
        for k in range(8):
            sq = sqr.next()
            kb.op("act", lambda e, sq=sq, k=k: e.activation(out=sq.t[:], in_=xin.t[:, k, :], func=AF.Square),
                  r=[xin], w=[sq])
            kb.op("pe", lambda e, sq=sq, k=k: e.matmul(ps.t[:], lhsT=onesf.t[:], rhs=sq.t[:], start=(k == 0),
                                                      stop=(k == 7)), r=[sq, onesf], w=[ps])
        kb.op("dve", lambda e: e.tensor_scalar(out=rstd.t[:], in0=ps.t[:], scalar1=1.0 / D, scalar2=EPS,
                                               op0=ALU.mult, op1=ALU.add), r=[ps], w=[rstd])
        kb.op("act", lambda e: e.activation(out=rstd.t[:], in_=rstd.t[:], func=AF.Sqrt), r=[rstd], w=[rstd])
        kb.op("dve", lambda e: e.reciprocal(out=rstd.t[:], in_=rstd.t[:]), r=[rstd], w=[rstd])
        for k in range(8):
            tm = tmpr.next()
            kb.op("dve", lambda e, tm=tm, k=k: e.tensor_tensor(out=tm.t[:], in0=xin.t[:, k, :], in1=rstd.t[:],
                                                               op=ALU.mult), r=[xin, rstd], w=[tm])
            if hf32 is not None:
                kb.op("act", lambda e, tm=tm, k=k: e.activation(
                    out=hf32.t[:, k, :], in_=tm.t[:], func=AF.Identity, scale=gs.t[:, k:k + 1],
                    bias=mods.t[:, sh_off + k:sh_off + k + 1]), r=[tm, gs, mods], w=[hf32])
                kb.op("pool", lambda e, k=k: e.tensor_copy(out=hout.t[:, k, :], in_=hf32.t[:, k, :]),
                      r=[hf32], w=[hout])
            else:
                kb.op("act", lambda e, tm=tm, k=k: e.activation(
                    out=hout.t[:, k, :], in_=tm.t[:], func=AF.Identity, scale=gs.t[:, k:k + 1],
                    bias=mods.t[:, sh_off + k:sh_off + k + 1]), r=[tm, gs, mods], w=[hout])

    xTv = xT.rearrange("(k p) t -> p k t", p=128)

    def projT(h, w, c0, ps, ncols=128):
        for k in range(8):
            kb.op("pe", lambda e, k=k: e.matmul(ps.t[0:ncols, :], lhsT=w.t[:, k, c0:c0 + ncols], rhs=h.t[:, k, :],
                                               start=(k == 0), stop=(k == 7)), r=[h, w], w=[ps])

    def projTok(h, j, w, c0, n, ps):
        for k in range(8):
            kb.op("pe", lambda e, k=k: e.matmul(ps.t[:, 0:n], lhsT=h.t[:, k, j * 128:(j + 1) * 128],
                                               rhs=w.t[:, k, c0:c0 + n], start=(k == 0), stop=(k == 7)),
                  r=[h, w], w=[ps])

    with ExitStack() as ph:
        wsb = kb.T(ph, [128, 8, WIN], BF16)
        wv = w_in.rearrange("(k p) n -> p k n", p=128)
        for k in range(8):
            kb.dma("pool", wsb.t[:, k, :], wv[:, k, :], w=[wsb])
        xgr = Rot([kb.T(ph, [128, 8, 512], F32) for _ in range(2)])
        hr = Rot([kb.T(ph, [128, 8, 512], BF16) for _ in range(2)])
        sqr = Rot([kb.T(ph, [128, 512], F32) for _ in range(2)])
        tmpr = Rot([kb.T(ph, [128, 512], F32) for _ in range(2)])
        rstd = kb.T(ph, [128, 512], F32)
        rmt = (sqr, rstd, tmpr, PS[0])
        stg = Rot([kb.T(ph, [128, 512], BF16) for _ in range(3)])
        sqb = Rot([kb.T(ph, [128, 512], BF16) for _ in range(2)])
        NVH = 20 if layer == 0 else 16
        psA = Rot([PS[1], PS[2]])
        psN = PS[3]
        psV = Rot([PS[4], PS[5]])
        psC = PS[6]
        NKN = 16
        kn = kb.T(ph, [128, NKN], F32)
        kms = kb.T(ph, [128, NKN], F32)
        tm2 = kb.T(ph, [128, 1], F32)
        kb.op("dve", lambda e: e.memset(kn.t[:], 0.0), w=[kn])

        def head_pair_K(h, c0, KA, hd0, col0, knc, scale=None):
            ps = psA.next()
            projT(h, wsb, c0, ps)
            s = stg.next()
            kb.op("act", lambda e: e.activation(out=s.t[:], in_=ps.t[:], func=AF.Copy,
                                                scale=(1.0 if scale is None else scale)), r=[ps], w=[s])
            q = sqb.next()
            kb.op("act", lambda e: e.activation(out=q.t[:], in_=ps.t[:], func=AF.Square), r=[ps], w=[q])
            kb.dma("sp", KA[hd0, 0:64, col0:col0 + 512], s.t[0:64, :], r=[s])
            kb.dma("sp", KA[hd0 + 1, 0:64, col0:col0 + 512], s.t[64:128, :], r=[s])
            kb.op("pe", lambda e: e.matmul(psN.t[0:2, :], lhsT=ind2.t[:, 0:2], rhs=q.t[:], start=True, stop=True),
                  r=[q, ind2], w=[psN])
            return ps

        def kn_update(knc):
            kb.op("dve", lambda e: e.reduce_max(out=tm2.t[0:2, :], in_=psN.t[0:2, :], axis=AX.X), r=[psN], w=[tm2])
            kb.op("dve", lambda e: e.tensor_max(out=kn.t[0:2, knc:knc + 1], in0=kn.t[0:2, knc:knc + 1],
                                                in1=tm2.t[0:2, :]), r=[kn, tm2], w=[kn])

        def tokV(h, c0, n, sv, h0, j):
            for (o, m) in ([(0, min(512, n))] + ([(512, n - 512)] if n > 512 else [])):
                ps = psV.next()
                projTok(h, j, wsb, c0 + o, m, ps)
                nh = m // 64
                hh = h0 + o // 64
                kb.op("dve", lambda e, ps=ps, m=m, nh=nh, hh=hh: e.tensor_copy(
                    out=sv.t[:, hh:hh + nh, j, 0:64], in_=ps.t[:, 0:m].rearrange("p (h c) -> p h c", h=nh)),
                    r=[ps], w=[sv])

        def flushV(sv, VDst, h0, nh, sg):
            for hh in range(nh):
                kb.dma("sp", VDst[hh, :, sg * 4:(sg + 1) * 4, :], sv.t[:, h0 + hh, :, :], r=[sv])

        if layer == 0:
            Lall = kb.T(ph, [128, NT, 8], F32)
            fb = kb.T(ph, [128, 8], F32)
            kb.dma("sp", fb.t[:], fbb[:, :], w=[fb])
            zt = kb.T(ph, [128, 8], F32)
            for p_ in range(2):
                kb.dma("sp", FcR[p_, :, :], bass.AP(tensor=FcD.tensor, offset=p_ * fgeo[1], ap=[[0, 128], [1, fgeo[1]]]))
            for g in range(3):
                W_ = dgeo[g][3]
                for q_ in range(8):
                    kb.dma("sp", FdR[g][q_, :, :], bass.AP(tensor=FdD[g].tensor, offset=q_ * W_, ap=[[0, 128], [1, W_]]))
            p1 = ExitStack()
            stv = Rot([kb.T(p1, [128, NVH, 4, 128], BF16) for _ in range(1)])
            for v_ in stv.items:
                kb.op("pool", lambda e, v_=v_: e.memset(v_.t[:, :, :, 64:128], 1.0), w=[v_])
            for sg in range(NG):
                xg = xgr.next()
                kb.dma("sp", xg.t[:], xTv[:, :, sg * 512:(sg + 1) * 512], w=[xg])
                h = hr.next()
                rms_mod(rmt, xg, gs1, SH1, h)
                for c4 in range(4):
                    head_pair_K(h, 512 + 128 * c4, KAF, 2 * c4, sg * 512, c4)
                    kn_update(c4)
                for c6 in range(6):
                    head_pair_K(h, 2312 + 128 * c6, KAD, 2 * c6, sg * 512, 4 + c6)
                    kn_update(4 + c6)
                sv = stv.next()
                for j in range(4):
                    tokV(h, 1024, 512, sv, 0, j)
                    tokV(h, 3080, 768, sv, 8, j)
                flushV(sv, VF, 0, 8, sg)
                flushV(sv, VD, 8, 12, sg)
                for j in range(4):
                    projTok(h, j, wsb, 1536, 8, psC)
                    kb.op("dve", lambda e: e.tensor_tensor(out=zt.t[:], in0=psC.t[:, 0:8], in1=fb.t[:], op=ALU.add),
                          r=[psC, fb], w=[zt])
                    kb.op("act", lambda e: e.activation(out=zt.t[:], in_=zt.t[:], func=AF.Exp, scale=-1.0), r=[zt], w=[zt])
                    kb.op("act", lambda e, j=j, sg=sg: e.activation(out=Lall.t[:, sg * 4 + j, :], in_=zt.t[:], func=AF.Ln,
                                                                    bias=1.0), r=[zt], w=[Lall])
                kb.dma("sp", KAF[0:8, 64, sg * 512:(sg + 1) * 512], onesb.t[0:8, :], r=[onesb])
                kb.dma("sp", KAF[0:8, 65, sg * 512:(sg + 1) * 512], onesb.t[0:8, :], r=[onesb])
                kb.dma("sp", KAF[0:8, 66, sg * 512:(sg + 1) * 512], onesb.t[0:8, :], r=[onesb])
                kb.dma("sp", KAF[0:8, 70, sg * 512:(sg + 1) * 512], onesb.t[0:8, :], r=[onesb])
                kb.dma("sp", KAD[0:12, 64, sg * 512:(sg + 1) * 512], onesb.t[0:12, :], r=[onesb])
                if sg < NQ:
                    for rr in (67, 68, 69):
                        kb.dma("sp", QAF[0:8, rr, sg * 512:(sg + 1) * 512], onesb.t[0:8, :], r=[onesb])
            kb.barrier()
            p1.close()
            p1b = ExitStack()
            Rsb = kb.T(p1b, [128, 8, 1024], F32)
            kb.dma("sp", Rsb.t[:], RD.rearrange("a p c -> p a c"), w=[Rsb])
            carry = kb.T(p1b, [128, 1], F32)
            kb.op("dve", lambda e: e.memset(carry.t[:], 0.0), w=[carry])
            Cg = kb.T(p1b, [128, 512], F32)
            r1 = kb.T(p1b, [128, 512], F32)
            cbr = Rot([kb.T(p1b, [128, 512], BF16) for _ in range(4)])
            for i in range(NQ):
                tiles = [4 * i + a for a in range(4)] + [NH + 4 * i + a for a in range(4)]
                for half in range(2):
                    for tt in range(8):
                        kb.op("pe", lambda e, tt=tt, half=half: e.matmul(
                            psC.t[0:8, :], lhsT=Lall.t[:, tiles[tt], :], rhs=Rsb.t[:, tt, half * 512:(half + 1) * 512],
                            start=(tt == 0), stop=(tt == 7)), r=[Lall, Rsb], w=[psC])
                    kb.op("dve", lambda e: e.tensor_scalar(out=Cg.t[0:8, :], in0=psC.t[0:8, :], scalar1=carry.t[0:8, 0:1],
                                                           scalar2=None, op0=ALU.add), r=[psC, carry], w=[Cg])
                    cur = Cg
                    scol = (i * 512) if half == 0 else (SO + i * 512)
                    for p3 in range(3):
                        cb = cbr.next()
                        kb.op("dve", lambda e, cur=cur, cb=cb: e.tensor_copy(out=cb.t[0:8, :], in_=cur.t[0:8, :]),
                              r=[cur], w=[cb])
                        kb.dma("sp", KAF[0:8, 67 + p3, scol:scol + 512], cb.t[0:8, :], r=[cb])
                        if half == 0:
                            nb = cbr.next()
                            kb.op("dve", lambda e, cb=cb, nb=nb: e.tensor_scalar(
                                out=nb.t[0:8, :], in0=cb.t[0:8, :], scalar1=-1.0, scalar2=None, op0=ALU.mult),
                                r=[cb], w=[nb])
                            kb.dma("sp", QAF[0:8, 64 + p3, i * 512:(i + 1) * 512], nb.t[0:8, :], r=[nb])
                        if p3 < 2:
                            kb.op("dve", lambda e, cur=cur, cb=cb: e.tensor_tensor(
                                out=r1.t[0:8, :], in0=cur.t[0:8, :], in1=cb.t[0:8, :], op=ALU.subtract),
                                r=[cur, cb], w=[r1])
                            cur = r1
                for tt in range(8):
                    kb.op("pe", lambda e, tt=tt: e.matmul(psC.t[0:8, 0:1], lhsT=Lall.t[:, tiles[tt], :],
                                                         rhs=onesf.t[:, 0:1], start=(tt == 0), stop=(tt == 7)),
                          r=[Lall, onesf], w=[psC])
                kb.op("dve", lambda e: e.tensor_tensor(out=carry.t[0:8, :], in0=carry.t[0:8, :], in1=psC.t[0:8, 0:1],
                                                       op=ALU.add), r=[carry, psC], w=[carry])
            kb.barrier()
            p1b.close()
        else:
            wu = kb.T(ph, [128, 2, 2048], BF16)
            wuv = w_ukv.rearrange("(k p) n -> p k n", p=128)
            for k in range(0 if 'U' in cut else 2):
                kb.dma("pool", wu.t[:, k, :], wuv[:, k, :], w=[wu])
            kvns = kb.T(ph, [128, 2], F32)
            if 'N' not in cut:
                kb.dma("sp", kvns.t[:], kvn[:, :], w=[kvns])
            ckf = Rot([kb.T(ph, [128, 2, 512], F32) for _ in range(2)])
            ckb = Rot([kb.T(ph, [128, 2, 512], BF16) for _ in range(2)])
            wki = kb.T(ph, [128, 8, 128], BF16)
            if 'W' not in cut:
                kb.op("dve", lambda e: e.tensor_copy(out=wki.t[:, :, 0:64], in_=wsb.t[:, :, 1792:1856]), r=[wsb], w=[wki])
                kb.op("dve", lambda e: e.tensor_copy(out=wki.t[:, :, 64:128], in_=wsb.t[:, :, 1792:1856]), r=[wsb], w=[wki])
            p1 = ExitStack()
            stv = Rot([kb.T(p1, [128, NVH, 4, 128], BF16) for _ in range(1)])
            for v_ in stv.items:
                kb.op("pool", lambda e, v_=v_: e.memset(v_.t[:, :, :, 64:128], 1.0), w=[v_])
            for sg in range(NG):
                xg = xgr.next()
                kb.dma("sp", xg.t[:], xTv[:, :, sg * 512:(sg + 1) * 512], w=[xg])
                h = hr.next()
                rms_mod(rmt, xg, gs1, SH1, h)
                cf = ckf.next()
                cb_ = ckb.next()
                pss = PS[0]
                if 'B' in cut:
                    continue
                qs_ = []
                for k2 in range(2):
                    ps = psA.next()
                    projT(h, wsb, 1024 + 128 * k2, ps)
                    kb.op("act", lambda e, ps=ps, k2=k2: e.activation(out=cf.t[:, k2, :], in_=ps.t[:], func=AF.Copy), r=[ps], w=[cf])
                    q = sqr.next()
                    kb.op("act", lambda e, q=q, ps=ps: e.activation(out=q.t[:], in_=ps.t[:], func=AF.Square), r=[ps], w=[q])
                    qs_.append(q)
                if 'P' in cut:
                    continue
                for k2 in range(2):
                    kb.op("pe", lambda e, k2=k2: e.matmul(pss.t[:], lhsT=onesf.t[:], rhs=qs_[k2].t[:], start=(k2 == 0),
                                                          stop=(k2 == 1)), r=[qs_[k2], onesf], w=[pss])
                if 'R' in cut:
                    continue
                kb.op("dve", lambda e: e.tensor_scalar(out=rstd.t[:], in0=pss.t[:], scalar1=1.0 / 256, scalar2=EPS,
                                                       op0=ALU.mult, op1=ALU.add), r=[pss], w=[rstd])
                kb.op("act", lambda e: e.activation(out=rstd.t[:], in_=rstd.t[:], func=AF.Sqrt), r=[rstd], w=[rstd])
                kb.op("dve", lambda e: e.reciprocal(out=rstd.t[:], in_=rstd.t[:]), r=[rstd], w=[rstd])
                if 'T' in cut:
                    continue
                for k2 in range(2):
                    tm = tmpr.next()
                    kb.op("dve", lambda e, tm=tm, k2=k2: e.tensor_tensor(out=tm.t[:], in0=cf.t[:, k2, :], in1=rstd.t[:],
                                                                         op=ALU.mult), r=[cf, rstd], w=[tm])
                    kb.op("act", lambda e, tm=tm, k2=k2: e.activation(out=cb_.t[:, k2, :], in_=tm.t[:], func=AF.Identity,
                                                                      scale=kvns.t[:, k2:k2 + 1], bias=zcol.t[:, 0:1]),
                          r=[tm, kvns, zcol], w=[cb_])
                for c8 in range(0 if 'C' in cut else 8):
                    ps = psA.next()
                    for k2 in range(2):
                        kb.op("pe", lambda e, k2=k2, c8=c8, ps=ps: e.matmul(
                            ps.t[:], lhsT=wu.t[:, k2, c8 * 128:(c8 + 1) * 128], rhs=cb_.t[:, k2, :],
                            start=(k2 == 0), stop=(k2 == 1)), r=[wu, cb_], w=[ps])
                    s = stg.next()
                    kb.op("act", lambda e, s=s, ps=ps: e.activation(out=s.t[:], in_=ps.t[:], func=AF.Copy), r=[ps], w=[s])
                    q = sqb.next()
                    kb.op("act", lambda e, q=q, ps=ps: e.activation(out=q.t[:], in_=ps.t[:], func=AF.Square), r=[ps], w=[q])
                    kb.dma("sp", KAS[2 * c8, 0:64, sg * 512:(sg + 1) * 512], s.t[0:64, :], r=[s])
                    kb.dma("sp", KAS[2 * c8 + 1, 0:64, sg * 512:(sg + 1) * 512], s.t[64:128, :], r=[s])
                    kb.op("pe", lambda e, q=q: e.matmul(psN.t[0:2, :], lhsT=ind2.t[:, 0:2], rhs=q.t[:], start=True,
                                                       stop=True), r=[q, ind2], w=[psN])
                    kn_update(c8)
                sv = stv.next()
                for j in range(0 if 'D' in cut else 4):
                    for o in (0, 512):
                        ps = psV.next()
                        for k2 in range(2):
                            kb.op("pe", lambda e, k2=k2, o=o, ps=ps, j=j: e.matmul(
                                ps.t[:], lhsT=cb_.t[:, k2, j * 128:(j + 1) * 128], rhs=wu.t[:, k2, 1024 + o:1536 + o],
                                start=(k2 == 0), stop=(k2 == 1)), r=[wu, cb_], w=[ps])
                        kb.op("dve", lambda e, o=o, ps=ps, j=j: e.tensor_copy(
                            out=sv.t[:, o // 64:o // 64 + 8, j, 0:64], in_=ps.t[:, :].rearrange("p (h c) -> p h c", h=8)),
                            r=[ps], w=[sv])
                if 'D' not in cut:
                    flushV(sv, VS, 0, 16, sg)
                if 'E' in cut:
                    continue
                ps = psA.next()
                projT(h, wki, 0, ps)
                s = stg.next()
                kb.op("act", lambda e, s=s, ps=ps: e.activation(out=s.t[:], in_=ps.t[:], func=AF.Copy), r=[ps], w=[s])
                kb.dma("sp", KI[:, sg * 512:(sg + 1) * 512], s.t[:, :], r=[s])
                kb.dma("sp", KAS[0:16, 64, sg * 512:(sg + 1) * 512], onesb.t[0:16, :], r=[onesb])
            kb.barrier()
            p1.close()

        kb.op("act", lambda e: e.activation(out=kms.t[0:2, :], in_=kn.t[0:2, :], func=AF.Sqrt), r=[kn], w=[kms])
        kb.op("dve", lambda e: e.tensor_scalar(out=kms.t[0:2, :], in0=kms.t[0:2, :], scalar1=0.125 * 1.05, scalar2=None,
                                               op0=ALU.mult), r=[kms], w=[kms])
        nqr = Rot([kb.T(ph, [128, 512], F32) for _ in range(2)])
        mgr = Rot([kb.T(ph, [128, 512], F32) for _ in range(4)])
        mrr = Rot([kb.T(ph, [128, 512], BF16) for _ in range(2)])

        def q_pair(h, c0, QA, hd0, col0):
            ps = psA.next()
            projT(h, wsb, c0, ps)
            s = stg.next()
            kb.op("act", lambda e: e.activation(out=s.t[:], in_=ps.t[:], func=AF.Copy, scale=0.125), r=[ps], w=[s])
            q = sqb.next()
            kb.op("act", lambda e: e.activation(out=q.t[:], in_=ps.t[:], func=AF.Square), r=[ps], w=[q])
            kb.dma("sp", QA[hd0, 0:64, col0:col0 + 512], s.t[0:64, :], r=[s])
            kb.dma("sp", QA[hd0 + 1, 0:64, col0:col0 + 512], s.t[64:128, :], r=[s])
            kb.op("pe", lambda e: e.matmul(psN.t[0:2, :], lhsT=ind2.t[:, 0:2], rhs=q.t[:], start=True, stop=True),
                  r=[q, ind2], w=[psN])
            nq = nqr.next()
            kb.op("act", lambda e: e.activation(out=nq.t[0:2, :], in_=psN.t[0:2, :], func=AF.Sqrt), r=[psN], w=[nq])
            return nq

        for i in range(NQ):
            xg = xgr.next()
            kb.dma("sp", xg.t[:], xTv[:, :, i * 512:(i + 1) * 512], w=[xg])
            h = hr.next()
            rms_mod(rmt, xg, gs1, SH1, h)
            c0s = i * 512
            if layer == 0:
                for c4 in range(4):
                    nq = q_pair(h, 128 * c4, QAF, 2 * c4, c0s)
                    mr = mrr.next()
                    kb.op("dve", lambda e, nq=nq, mr=mr, c4=c4: e.tensor_scalar(
                        out=mr.t[0:2, :], in0=nq.t[0:2, :], scalar1=kms.t[0:2, c4:c4 + 1], scalar2=-1.0,
                        op0=ALU.mult, op1=ALU.mult), r=[nq, kms], w=[mr])
                    kb.dma("sp", QAF[2 * c4, 70, c0s:c0s + 512], mr.t[0:1, :], r=[mr])
                    kb.dma("sp", QAF[2 * c4 + 1, 70, c0s:c0s + 512], mr.t[1:2, :], r=[mr])
                for sp in range(2):
                    mgs = []
                    for gp in range(3):
                        c6 = 2 * gp + sp
                        nq = q_pair(h, 1544 + 128 * c6, QAD, 2 * c6, c0s)
                        mg = mgr.next()
                        kb.op("dve", lambda e, nq=nq, mg=mg, c6=c6: e.tensor_scalar(
                            out=mg.t[0:2, :], in0=nq.t[0:2, :], scalar1=kms.t[0:2, 4 + c6:5 + c6], scalar2=None,
                            op0=ALU.mult), r=[nq, kms], w=[mg])
                        mgs.append(mg)
                    kb.op("dve", lambda e: e.tensor_max(out=mgs[0].t[0:2, :], in0=mgs[0].t[0:2, :], in1=mgs[1].t[0:2, :]),
                          r=[mgs[0], mgs[1]], w=[mgs[0]])
                    kb.op("dve", lambda e: e.tensor_max(out=mgs[0].t[0:2, :], in0=mgs[0].t[0:2, :], in1=mgs[2].t[0:2, :]),
                          r=[mgs[0], mgs[2]], w=[mgs[0]])
                    mr = mrr.next()
                    kb.op("dve", lambda e, mr=mr: e.tensor_scalar(out=mr.t[0:2, :], in0=mgs[0].t[0:2, :],
                                                                  scalar1=bmax.t[0:2, 0:1], scalar2=-1.0, op0=ALU.add,
                                                                  op1=ALU.mult), r=[mgs[0], bmax], w=[mr])
                    for gp in range(3):
                        c6 = 2 * gp + sp
                        kb.dma("sp", QAD[2 * c6, 64, c0s:c0s + 512], mr.t[0:1, :], r=[mr])
                        kb.dma("sp", QAD[2 * c6 + 1, 64, c0s:c0s + 512], mr.t[1:2, :], r=[mr])
            else:
                for c8 in range(0 if 'F' in cut else 8):
                    nq = q_pair(h, 128 * c8, QAS, 2 * c8, c0s)
                    mg = mgr.next()
                    kb.op("dve", lambda e, nq=nq, mg=mg, c8=c8: e.tensor_scalar(
                        out=mg.t[0:2, :], in0=nq.t[0:2, :], scalar1=kms.t[0:2, c8:c8 + 1], scalar2=None,
                        op0=ALU.mult), r=[nq, kms], w=[mg])
                    mr2 = mrr.next()
                    kb.op("dve", lambda e, mg=mg, mr2=mr2: e.tensor_scalar(
                        out=mr2.t[0:2, :], in0=mg.t[0:2, :], scalar1=bmax.t[0:2, 0:1], scalar2=-1.0, op0=ALU.add,
                        op1=ALU.mult), r=[mg, bmax], w=[mr2])
                    kb.dma("sp", QAS[2 * c8, 64, c0s:c0s + 512], mr2.t[0:1, :], r=[mr2])
                    kb.dma("sp", QAS[2 * c8 + 1, 64, c0s:c0s + 512], mr2.t[1:2, :], r=[mr2])
                for c4 in range(0 if 'G' in cut else 4):
                    ps = psA.next()
                    projT(h, wsb, 1280 + 128 * c4, ps)
                    s = stg.next()
                    kb.op("act", lambda e, s=s, ps=ps: e.activation(out=s.t[:], in_=ps.t[:], func=AF.Copy), r=[ps], w=[s])
                    kb.dma("sp", QI[c4 * 128:(c4 + 1) * 128, c0s:c0s + 512], s.t[:, :], r=[s])
                for j in range(0 if 'H' in cut else 4):
                    projTok(h, j, wsb, 1856, 8, psC)
                    wt = tmpr.next()
                    kb.op("dve", lambda e, wt=wt: e.tensor_copy(out=wt.t[:, 0:8], in_=psC.t[:, 0:8]), r=[psC], w=[wt])
                    kb.dma("sp", WI[c0s + j * 128:c0s + (j + 1) * 128, :], wt.t[:, 0:128], r=[wt])
        kb.barrier()

    if stop_after <= 1:
        return nc, es, kb
    def attention_head(ph, kaD, qaD, vD, vc0, Kd, blocks_fn, finalize, bufs, expbias=None, tail_fn=None):
        ka, qa, vp, psS, psO, pTr = bufs
        kb.dma("sp", ka.t[0:Kd, :], kaD[0:Kd, :], w=[ka])
        kb.dma("sp", qa.t[0:Kd, :], qaD[0:Kd, :], w=[qa])
        kb.dma("sp", vp.t[:], vD, w=[vp])
        for i in range(NQ):
            blocks = blocks_fn(i)
            po = psO.next()
            nb = len(blocks)
            pend = []
            for n in range(nb + 2):
                if n < nb:
                    kt, masks, far = blocks[n]
                    if callable(masks):
                        masks = masks()
                    ps = psS.next()
                    nm_ = len(masks)
                    kb.op("pe", lambda e, kt=kt, ps=ps, nm_=nm_: e.matmul(
                        ps.t[:], lhsT=ka.t[0:Kd, kt * 128:(kt + 1) * 128], rhs=qa.t[0:Kd, i * 512:(i + 1) * 512],
                        start=True, stop=(nm_ == 0)), r=[ka, qa], w=[ps])
                    for mi, (lt, mk, mkap) in enumerate(masks):
                        kb.op("pe", lambda e, lt=lt, mkap=mkap, ps=ps, mi=mi, nm_=nm_: e.matmul(
                            ps.t[:], lhsT=lt.t[:], rhs=mkap, start=False, stop=(mi == nm_ - 1)), r=[lt, mk], w=[ps])
                    pT = pTr.next()
                    if far and expbias is not None:
                        kb.op("act", lambda e, pT=pT, ps=ps: e.activation(out=pT.t[:], in_=ps.t[:], func=AF.Exp,
                                                                          bias=expbias[1]), r=[ps, expbias[0]], w=[pT])
                    else:
                        kb.op("act", lambda e, pT=pT, ps=ps: e.activation(out=pT.t[:], in_=ps.t[:], func=AF.Exp),
                              r=[ps], w=[pT])
                    pend.append((kt, pT))
                if n >= 2:
                    kt, pT = pend[n - 2]
                    kb.op("pe", lambda e, kt=kt, pT=pT, n=n: e.matmul(
                        po.t[:], lhsT=vp.t[:, kt, :], rhs=pT.t[:], start=(n == 2),
                        stop=(n == nb + 1 and tail_fn is None)), r=[vp, pT], w=[po])
            if tail_fn is not None:
                tail_fn(i, po)
            finalize(i, po)

    def norm_write(ph_t, src_ps_or_sb, srcdeps, row0, i):
        rz, on = ph_t
        kb.op("dve", lambda e: e.reciprocal(out=rz.t[64:128, :], in_=src_ps_or_sb.t[64:128, :]), r=srcdeps, w=[rz])
        o = on.next()
        kb.op("dve", lambda e: e.tensor_tensor(out=o.t[0:64, :], in0=src_ps_or_sb.t[0:64, :], in1=rz.t[64:128, :],
                                               op=ALU.mult), r=srcdeps + [rz], w=[o])
        kb.dma("sp", MT[row0:row0 + 64, i * 512:(i + 1) * 512], o.t[0:64, :], r=[o])

    def toep_tile(mk, FR, idx, W_, dmin, e_):
        src = bass.AP(tensor=FR.tensor, offset=idx * 128 * W_ + 256 * e_ - dmin, ap=[[W_ - 1, 128], [256, 4], [1, 128]])
        kb.dma("pool", mk.t[:].rearrange("p (a c) -> p a c", a=4), src, w=[mk])

    with ExitStack() as ph:
        kar = Rot([kb.T(ph, [128, S], BF16) for _ in range(2)])
        qar = Rot([kb.T(ph, [128, SO], BF16) for _ in range(2)])
        vpr = Rot([kb.T(ph, [128, NT, 128], BF16) for _ in range(2)])
        psS = Rot([PS[0], PS[1], PS[2], PS[3]])
        psO = Rot([PS[4], PS[5]])
        pTr = Rot([kb.T(ph, [128, 512], BF16) for _ in range(4)])
        rz = kb.T(ph, [128, 512], F32)
        onr = Rot([kb.T(ph, [128, 512], BF16) for _ in range(2)])
        if layer == 0:
            cm = {}
            for part in range(2):
                for e_ in FOX_E:
                    mk = kb.T(ph, [128, 512], BF16)
                    toep_tile(mk, FcR, part, fgeo[1], fgeo[0], e_)
                    cm[(part, e_)] = mk

            def fox_blocks(i):
                bl = []
                for part in range(2):
                    for b in range(0, 4 * i + 4):
                        e_ = 4 * i - b
                        ms = [] if e_ >= 1 else [(identb, cm[(part, e_)], cm[(part, e_)].t[:])]
                        bl.append((b + part * NH, ms, False))
                return bl

            for hd in range(8):
                bufs = (kar.next(), qar.next(), vpr.next(), psS, psO, pTr)
                attention_head(ph, KAF[hd], QAF[hd], VF[hd], 0, 71, fox_blocks,
                               lambda i, po, hd=hd: norm_write((rz, onr), po, [po], hd * 64, i), bufs)
            oacc = kb.T(ph, [128, SO], F32)
            tot = kb.T(ph, [128, 512], F32)
            for j in range(4):
                for g in range(3):
                    eo, et, dmin, W_ = dgeo[g]
                    hd = g * 4 + j
                    dm = {}
                    with ExitStack() as ph2:
                        for part, el in ((0, eo), (1, et)):
                            for e_ in el:
                                mk = kb.T(ph2, [128, 512], BF16)
                                toep_tile(mk, FdR[g], j * 2 + part, W_, dmin, e_)
                                dm[(part, e_)] = mk

                        def dil_blocks(i, eo=eo, et=et, dm=dm):
                            bl = []
                            for part, el in ((0, eo), (1, et)):
                                for e_ in el:
                                    b = 4 * i - e_
                                    if 0 <= b < NH:
                                        bl.append((b + part * NH, [(identb, dm[(part, e_)], dm[(part, e_)].t[:])], False))
                            return bl

                        def dil_fin(i, po, g=g, j=j):
                            sl = oacc.t[:, i * 512:(i + 1) * 512]
                            if g == 0:
                                kb.op("act", lambda e: e.activation(out=sl, in_=po.t[:], func=AF.Copy), r=[po], w=[oacc])
                            elif g == 1:
                                kb.op("dve", lambda e: e.tensor_tensor(out=sl, in0=po.t[:], in1=sl, op=ALU.add),
                                      r=[po, oacc], w=[oacc])
                            else:
                                norm_write((rz, onr), po, [po], 512 + j * 64, i)

                        def dil_tail(i, po):
                            kb.op("pe", lambda e: e.matmul(po.t[:], lhsT=identf.t[:], rhs=oacc.t[:, i * 512:(i + 1) * 512],
                                                           start=False, stop=True), r=[identf, oacc], w=[po])

                        bufs = (kar.next(), qar.next(), vpr.next(), psS, psO, pTr)
                        attention_head(ph2, KAD[hd], QAD[hd], VD[hd], 0, 65, dil_blocks, dil_fin, bufs,
                                       tail_fn=(dil_tail if g == 2 else None))
                        kb.barrier()
        if layer == 1:
            pass
        kb.barrier()
    if layer == 1:
        NIT = 26
        with ExitStack() as ph:
            pst = Tl(ph.enter_context(nc.psum_tensor(tag + "pst", [128, 1024], BF16)))
            qis = kb.T(ph, [128, 4, SO], BF16)
            kb.dma("sp", qis.t[:], QI.rearrange("(c p) t -> p c t", p=128), w=[qis])
            kis = kb.T(ph, [128, S], BF16)
            kb.dma("sp", kis.t[:], KI[:, :], w=[kis])
            sc = kb.T(ph, [128, S], F32)
            junk = kb.T(ph, [128, S], BF16)
            nmb = kb.T(ph, [128, S], BF16)
            stage = kb.T(ph, [128, NT, 512], BF16)
            rlr = Rot([kb.T(ph, [128, 512], F32) for _ in range(3)])
            wq = kb.T(ph, [128, 8], F32)
            trim = kb.T(ph, [128, 128], F32)
            othd = kb.T(ph, [128, 128], F32)
            pw2 = kb.T(ph, [128, 32], F32)
            kb.dma("sp", trim.t[:], trimD[:, :], w=[trim])
            kb.dma("sp", othd.t[:], othdD[:, :], w=[othd])
            kb.dma("sp", pw2.t[:], pow2D[:, :], w=[pw2])
            dhs = kb.T(ph, [128, 32], F32)
            cnt = kb.T(ph, [128, 32], F32)
            lo = kb.T(ph, [128, 1], F32)
            mid = kb.T(ph, [128, 1], F32)
            st4 = kb.T(ph, [128, 4], F32)
            psI = Rot([PS[0], PS[1], PS[2], PS[3]])
            for i in range(NQ):
                NCH = i + 1
                RW = 2 * NCH * 512
                for a4 in range(4):
                    aq = 4 * i + a4
                    kb.dma("sp", wq.t[:], WI[aq * 128:(aq + 1) * 128, 0:8], w=[wq])
                    for kc in range(NCH):
                        for part in range(2):
                            c0 = (2 * kc + part) * 512
                            k0 = part * SO + kc * 512
                            for hh in range(8):
                                pb = (hh % 2) * 64
                                ps = psI.next()
                                kb.op("pe", lambda e, hh=hh, pb=pb, ps=ps, k0=k0: e.matmul(
                                    ps.t[:], lhsT=qis.t[pb:pb + 64, hh // 2, aq * 128:(aq + 1) * 128],
                                    rhs=kis.t[pb:pb + 64, k0:k0 + 512], start=True, stop=True), r=[qis, kis], w=[ps])
                                rl = rlr.next()
                                kb.op("act", lambda e, rl=rl, ps=ps: e.activation(out=rl.t[:], in_=ps.t[:], func=AF.Relu),
                                      r=[ps], w=[rl])
                                if hh == 0:
                                    kb.op("dve", lambda e, rl=rl, c0=c0: e.tensor_scalar(
                                        out=sc.t[:, c0:c0 + 512], in0=rl.t[:], scalar1=wq.t[:, 0:1], scalar2=None,
                                        op0=ALU.mult), r=[rl, wq], w=[sc])
                                else:
                                    kb.op("dve", lambda e, rl=rl, c0=c0, hh=hh: e.scalar_tensor_tensor(
                                        out=sc.t[:, c0:c0 + 512], in0=rl.t[:], scalar=wq.t[:, hh:hh + 1],
                                        in1=sc.t[:, c0:c0 + 512], op0=ALU.mult, op1=ALU.add), r=[rl, wq, sc], w=[sc])
                    kb.op("dve", lambda e: e.tensor_reduce(out=st4.t[:, 0:1], in_=sc.t[:, 0:RW], axis=AX.X, op=ALU.max),
                          r=[sc], w=[st4])
                    kb.op("dve", lambda e: e.tensor_reduce(out=st4.t[:, 1:2], in_=sc.t[:, 0:RW], axis=AX.X, op=ALU.min),
                          r=[sc], w=[st4])
                    for part in range(2):
                        base = (2 * i + part) * 512
                        for j in range(4):
                            blk = sc.t[:, base + j * 128:base + (j + 1) * 128]
                            if j > a4:
                                kb.op("pool", lambda e, blk=blk: e.memset(blk, -1.0e9), w=[sc])
                            elif j == a4:
                                mt_ = trim if part == 0 else othd
                                kb.op("dve", lambda e, blk=blk, mt_=mt_: e.tensor_tensor(out=blk, in0=blk, in1=mt_.t[:],
                                                                                         op=ALU.add), r=[sc, mt_], w=[sc])
                    if aq == 0:
                        kb.op("dve", lambda e: e.memset(lo.t[:], -1.0e8), w=[lo])
                    else:
                        kb.op("dve", lambda e: e.tensor_tensor(out=st4.t[:, 2:3], in0=st4.t[:, 0:1], in1=st4.t[:, 1:2],
                                                               op=ALU.subtract), r=[st4], w=[st4])
                        kb.op("dve", lambda e: e.tensor_scalar(out=st4.t[:, 2:3], in0=st4.t[:, 2:3], scalar1=2.0, scalar2=None,
                                                               op0=ALU.add), r=[st4], w=[st4])
                        kb.op("dve", lambda e: e.tensor_scalar(out=dhs.t[:], in0=pw2.t[:], scalar1=st4.t[:, 2:3], scalar2=None,
                                                               op0=ALU.mult), r=[pw2, st4], w=[dhs])
                        kb.op("dve", lambda e: e.tensor_scalar(out=lo.t[:], in0=st4.t[:, 1:2], scalar1=-1.0, scalar2=None,
                                                               op0=ALU.add), r=[st4], w=[lo])
                        kb.op("dve", lambda e: e.memset(cnt.t[:], 0.0), w=[cnt])
                        for it in range(NIT):
                            kb.op("dve", lambda e, it=it: e.tensor_tensor(out=mid.t[:], in0=lo.t[:], in1=dhs.t[:, it:it + 1],
                                                                          op=ALU.add), r=[lo, dhs], w=[mid])
                            kb.op("dve", lambda e, it=it: e.tensor_scalar(
                                out=junk.t[:, 0:RW], in0=sc.t[:, 0:RW], scalar1=mid.t[:, 0:1], scalar2=0.0, op0=ALU.is_ge,
                                op1=ALU.add, accum_out=cnt.t[:, it:it + 1]), r=[sc, mid], w=[junk, cnt])
                            kb.op("dve", lambda e, it=it: e.scalar_tensor_tensor(
                                out=mid.t[:], in0=cnt.t[:, it:it + 1], scalar=255.5, in1=dhs.t[:, it:it + 1], op0=ALU.is_ge,
                                op1=ALU.mult), r=[cnt, dhs], w=[mid])
                            kb.op("dve", lambda e: e.tensor_tensor(out=lo.t[:], in0=lo.t[:], in1=mid.t[:], op=ALU.add),
                                  r=[lo, mid], w=[lo])
                    kb.op("dve", lambda e: e.tensor_scalar(out=nmb.t[:, 0:RW], in0=sc.t[:, 0:RW], scalar1=lo.t[:, 0:1],
                                                           scalar2=1.0, op0=ALU.is_ge, op1=ALU.subtract), r=[sc, lo], w=[nmb])
                    nblk = RW // 128
                    for b0 in range(0, nblk, 8):
                        nb_ = min(8, nblk - b0)
                        for bb in range(nb_):
                            kb.op("pe", lambda e, bb=bb, b0=b0: e.transpose(
                                out=pst.t[:, bb * 128:(bb + 1) * 128], in_=nmb.t[:, (b0 + bb) * 128:(b0 + bb + 1) * 128],
                                identity=identb.t[:]), r=[nmb, identb], w=[pst])
                        for half in range(nb_ // 4):
                            cb = (b0 // 4) + half
                            kt0 = (cb % 2) * NH + (cb // 2) * 4
                            kb.op("act", lambda e, half=half, kt0=kt0: e.activation(
                                out=stage.t[:, kt0:kt0 + 4, a4 * 128:(a4 + 1) * 128],
                                in_=pst.t[:, half * 512:(half + 1) * 512].rearrange("p (j c) -> p j c", j=4), func=AF.Copy),
                                r=[pst], w=[stage])
                for part in range(2):
                    kb.dma("sp", NM[i, part * NH:part * NH + 4 * NCH].rearrange("k p c -> p k c"),
                           stage.t[:, part * NH:part * NH + 4 * NCH, :], r=[stage])
            kb.barrier()

        with ExitStack() as ph:
            kar = Rot([kb.T(ph, [128, S], BF16) for _ in range(2)])
            qar = Rot([kb.T(ph, [128, SO], BF16) for _ in range(2)])
            vpr = Rot([kb.T(ph, [128, NT, 128], BF16) for _ in range(2)])
            psS = Rot([PS[0], PS[1], PS[2], PS[3]])
            psO = Rot([PS[4], PS[5]])
            pTr = Rot([kb.T(ph, [128, 512], BF16) for _ in range(4)])
            rz = kb.T(ph, [128, 512], F32)
            onr = Rot([kb.T(ph, [128, 512], BF16) for _ in range(2)])
            i3b = kb.T(ph, [128, 128], BF16)
            kb.op("dve", lambda e: e.tensor_scalar(out=i3b.t[:], in0=identf.t[:], scalar1=-NEG, scalar2=None, op0=ALU.mult),
                  r=[identf], w=[i3b])
            rbf_ = kb.T(ph, [128, 16], F32)
            kb.dma("sp", rbf_.t[:], rbfar[:, :], w=[rbf_])
            for q_ in range(32):
                W_ = sgeo[1]
                kb.dma("sp", FsR[q_, :, :], bass.AP(tensor=FsD.tensor, offset=q_ * W_, ap=[[0, 128], [1, W_]]))
            kb.barrier()
            nmr = Rot([kb.T(ph, [128, 4, 512], BF16) for _ in range(6)])
            bt = {}
            for part in range(2):
                for e_ in DSA_NEAR_E:
                    bt[(part, e_)] = kb.T(ph, [128, 512], BF16)
            for hd in range(16):
                for part in range(2):
                    for e_ in DSA_NEAR_E:
                        toep_tile(bt[(part, e_)], FsR, hd * 2 + part, sgeo[1], sgeo[0], e_)

                def dsa_blocks(i, hd=hd):
                    pieces = [(part, kc) for part in range(2) for kc in range(i + 1)]
                    tiles = {}

                    def load(pi):
                        if pi >= len(pieces) or pi in tiles:
                            return
                        part, kc = pieces[pi]
                        nt_ = nmr.next()
                        kb.dma("sp", nt_.t[:], NM[i, part * NH + 4 * kc:part * NH + 4 * kc + 4].rearrange("k p c -> p k c"),
                               w=[nt_])
                        tiles[pi] = nt_

                    bl = []
                    for pi, (part, kc) in enumerate(pieces):
                        for j in range(4):
                            b = 4 * kc + j
                            e_ = 4 * i - b
                            far = e_ > DSA_NEAR_E[-1]

                            def mk(pi=pi, j=j, part=part, e_=e_, far=far):
                                load(pi)
                                if j == 0:
                                    load(pi + 1)
                                    load(pi + 2)
                                nt_ = tiles[pi]
                                ms = [(i3b, nt_, nt_.t[:, j, :])]
                                if not far:
                                    ms.append((identb, bt[(part, e_)], bt[(part, e_)].t[:]))
                                return ms
                            bl.append((b + part * NH, mk, far))
                    return bl

                bufs = (kar.next(), qar.next(), vpr.next(), psS, psO, pTr)
                attention_head(ph, KAS[hd], QAS[hd], VS[hd], 0, 65, dsa_blocks,
                               lambda i, po, hd=hd: norm_write((rz, onr), po, [po], hd * 64, i), bufs,
                               expbias=(rbf_, rbf_.t[:, hd:hd + 1]))
            kb.barrier()
    if stop_after <= 2:
        return nc, es, kb

    KC = NMO // 128
    with ExitStack() as ph:
        wo = kb.T(ph, [128, KC, 1024], BF16)
        for kc in range(KC):
            kb.dma("pool", wo.t[:, kc, :], w_out[kc * 128:(kc + 1) * 128, :], w=[wo])
        rwf = kb.T(ph, [128, 8, NE], F32)
        kb.dma("sp", rwf.t[:], rw.rearrange("(k p) e -> p k e", p=128), w=[rwf])
        rbs = kb.T(ph, [128, NE], F32)
        kb.dma("sp", rbs.t[:], rbb[:, :], w=[rbs])
        xgr = Rot([kb.T(ph, [128, 8, 512], F32) for _ in range(2)])
        mtr = Rot([kb.T(ph, [128, KC, 512], BF16) for _ in range(2)])
        x1r = Rot([kb.T(ph, [128, 8, 512], F32) for _ in range(2)])
        h2f = kb.T(ph, [128, 8, 512], F32)
        h2r = Rot([kb.T(ph, [128, 8, 512], BF16) for _ in range(2)])
        sqr = Rot([kb.T(ph, [128, 512], F32) for _ in range(2)])
        tmpr = Rot([kb.T(ph, [128, 512], F32) for _ in range(2)])
        rstd = kb.T(ph, [128, 512], F32)
        rmt = (sqr, rstd, tmpr, PS[0])
        psA = Rot([PS[1], PS[2]])
        psL = Rot([PS[3], PS[4]])
        psT = PS[5]
        lg = kb.T(ph, [128, NE], F32)
        ex = kb.T(ph, [128, NE], F32)
        mk_ = kb.T(ph, [128, NE], F32)
        gg = kb.T(ph, [128, NE], F32)
        mx8 = kb.T(ph, [128, 8], F32)
        sm1 = kb.T(ph, [128, 2], F32)
        gTr = Rot([kb.T(ph, [128, 512], F32) for _ in range(2)])
        MTv = MT.rearrange("(k p) t -> p k t", p=128)
        X1v = X1.rearrange("(k p) t -> p k t", p=128)
        H2v = H2.rearrange("(k p) t -> p k t", p=128)
        for i in range(NQ):
            cs = slice(i * 512, (i + 1) * 512)
            xg = xgr.next()
            kb.dma("sp", xg.t[:], xTv[:, :, cs], w=[xg])
            mt = mtr.next()
            kb.dma("sp", mt.t[:], MTv[:, :, cs], w=[mt])
            x1 = x1r.next()
            for dc in range(8):
                ps = psA.next()
                for kc in range(KC):
                    kb.op("pe", lambda e, kc=kc, dc=dc, ps=ps: e.matmul(
                        ps.t[:], lhsT=wo.t[:, kc, dc * 128:(dc + 1) * 128], rhs=mt.t[:, kc, :], start=(kc == 0),
                        stop=(kc == KC - 1)), r=[wo, mt], w=[ps])
                kb.op("dve", lambda e, dc=dc, ps=ps: e.scalar_tensor_tensor(
                    out=x1.t[:, dc, :], in0=ps.t[:], scalar=mods.t[:, GT1 + dc:GT1 + dc + 1], in1=xg.t[:, dc, :],
                    op0=ALU.mult, op1=ALU.add), r=[ps, mods, xg], w=[x1])
            kb.dma("sp", X1v[:, :, cs], x1.t[:], r=[x1])
            h2 = h2r.next()
            rms_mod(rmt, x1, gs2, SH2, h2, hf32=h2f)
            kb.dma("sp", H2v[:, :, cs], h2.t[:], r=[h2])
            gT = gTr.next()
            for j in range(4):
                pl = psL.next()
                for k in range(8):
                    kb.op("pe", lambda e, k=k, j=j, pl=pl: e.matmul(
                        pl.t[:, 0:NE], lhsT=h2f.t[:, k, j * 128:(j + 1) * 128], rhs=rwf.t[:, k, :], start=(k == 0),
                        stop=(k == 7)), r=[h2f, rwf], w=[pl])
                kb.op("dve", lambda e, pl=pl: e.tensor_tensor(out=lg.t[:], in0=pl.t[:, 0:NE], in1=rbs.t[:], op=ALU.add),
                      r=[pl, rbs], w=[lg])
                kb.op("dve", lambda e: e.max(out=mx8.t[:], in_=lg.t[:]), r=[lg], w=[mx8])
                kb.op("dve", lambda e: e.tensor_scalar(out=sm1.t[:, 0:1], in0=mx8.t[:, 0:1], scalar1=-1.0, scalar2=None,
                                                       op0=ALU.mult), r=[mx8], w=[sm1])
                kb.op("act", lambda e: e.activation(out=ex.t[:], in_=lg.t[:], func=AF.Exp, bias=sm1.t[:, 0:1]),
                      r=[lg, sm1], w=[ex])
                kb.op("dve", lambda e: e.tensor_scalar(out=mk_.t[:], in0=lg.t[:], scalar1=mx8.t[:, 3:4], scalar2=None,
                                                       op0=ALU.is_ge), r=[lg, mx8], w=[mk_])
                kb.op("dve", lambda e: e.tensor_tensor(out=gg.t[:], in0=ex.t[:], in1=mk_.t[:], op=ALU.mult),
                      r=[ex, mk_], w=[gg])
                kb.op("dve", lambda e: e.reduce_sum(out=sm1.t[:, 1:2], in_=gg.t[:], axis=AX.X), r=[gg], w=[sm1])
                kb.op("dve", lambda e: e.reciprocal(out=sm1.t[:, 1:2], in_=sm1.t[:, 1:2]), r=[sm1], w=[sm1])
                kb.op("dve", lambda e: e.tensor_scalar(out=gg.t[:], in0=gg.t[:], scalar1=sm1.t[:, 1:2], scalar2=None,
                                                       op0=ALU.mult), r=[gg, sm1], w=[gg])
                kb.op("pe", lambda e: e.transpose(out=psT.t[0:NE, 0:128], in_=gg.t[:], identity=identf.t[:]),
                      r=[gg, identf], w=[psT])
                kb.op("act", lambda e, j=j: e.activation(out=gT.t[0:NE, j * 128:(j + 1) * 128], in_=psT.t[0:NE, 0:128],
                                                         func=AF.Copy), r=[psT], w=[gT])
            kb.dma("sp", GTs[:, cs], gT.t[0:NE, :], r=[gT])
        kb.barrier()
    if stop_after <= 3:
        return nc, es, kb

    P = min(1024, SO)
    NPG = P // 512
    outv = outD.rearrange("(k p) t -> p k t", p=128)
    with ExitStack() as ph:
        PS7 = Tl(ph.enter_context(nc.psum_tensor(tag + "ps7", [128, 512], F32)))
        b1c = kb.T(ph, [128, NE * 16], F32)
        kb.dma("sp", b1c.t[:], b1col[:, :], w=[b1c])
        selb = kb.T(ph, [128, NE * 128], BF16)
        kb.dma("pool", selb.t[0:NE, :], selD[:, :], w=[selb])
        b2f = kb.T(ph, [128, D], F32)
        kb.dma("sp", b2f.t[0:NE, :], b2[:, :], w=[b2f])
        H2p = kb.T(ph, [128, 8, P], BF16)
        acc = kb.T(ph, [128, 8, P], F32)
        gtf = kb.T(ph, [128, P], F32)
        gtr_ = kb.T(ph, [128, P], F32)
        gth = kb.T(ph, [128, P], BF16)
        gtl = kb.T(ph, [128, P], BF16)
        if layer == 1:
            fns = kb.T(ph, [128, 8], F32)
            kb.dma("sp", fns.t[:], fnc[:, :], w=[fns])
        X1v = X1.rearrange("(k p) t -> p k t", p=128)
        H2v = H2.rearrange("(k p) t -> p k t", p=128)
        for pz in range(SO // P):
            c0 = pz * P
            kb.dma("sp", H2p.t[:], H2v[:, :, c0:c0 + P], w=[H2p])
            kb.dma("sp", gtf.t[0:NE, :], GTs[:, c0:c0 + P], w=[gtf])
            kb.op("dve", lambda e: e.tensor_copy(out=gth.t[0:NE, :], in_=gtf.t[0:NE, :]), r=[gtf], w=[gth])
            kb.op("dve", lambda e: e.tensor_tensor(out=gtr_.t[0:NE, :], in0=gtf.t[0:NE, :], in1=gth.t[0:NE, :],
                                                   op=ALU.subtract), r=[gtf, gth], w=[gtr_])
            kb.op("dve", lambda e: e.tensor_copy(out=gtl.t[0:NE, :], in_=gtr_.t[0:NE, :]), r=[gtr_], w=[gtl])
            with ExitStack() as pe_:
                w1r = Rot([kb.T(pe_, [128, 8, 2048], BF16) for _ in range(2)])
                w2r = Rot([kb.T(pe_, [128, 8, 1024], BF16) for _ in range(2)])
                glr = Rot([kb.T(pe_, [128, 512], F32) for _ in range(2)])
                sgr = Rot([kb.T(pe_, [128, 512], F32) for _ in range(2)])
                lir = Rot([kb.T(pe_, [128, 512], F32) for _ in range(2)])
                t1r = Rot([kb.T(pe_, [128, 512], F32) for _ in range(2)])
                t2r = Rot([kb.T(pe_, [128, 512], F32) for _ in range(2)])
                Gsr = Rot([kb.T(pe_, [128, 512], F32) for _ in range(2)])
                abr = Rot([kb.T(pe_, [128, 512], BF16) for _ in range(10)])
                psg = Rot([PS[0], PS[1]])
                psl = Rot([PS[2], PS[3]])
                psG = PS[4]
                psy = Rot([PS[5], PS[6]])
                psB = PS7
                for gi in range(NPG):
                    cs = slice(gi * 512, (gi + 1) * 512)
                    for dc in range(8):
                        kb.op("pe", lambda e, dc=dc, cs=cs: e.matmul(
                            psB.t[:], lhsT=b2f.t[0:NE, dc * 128:(dc + 1) * 128], rhs=gtf.t[0:NE, cs], start=True,
                            stop=True), r=[b2f, gtf], w=[psB])
                        kb.op("act", lambda e, dc=dc, cs=cs: e.activation(out=acc.t[:, dc, cs], in_=psB.t[:], func=AF.Copy),
                              r=[psB], w=[acc])
                for ex_ in range(NE):
                    w1e = w1r.next()
                    w2e = w2r.next()
                    kb.dma("pool", w1e.t[:], w1[ex_].rearrange("(k p) n -> p k n", p=128), w=[w1e])
                    kb.dma("pool", w2e.t[:], w2[ex_].rearrange("(k p) n -> p k n", p=128), w=[w2e])
                    for gi in range(NPG):
                        cs = slice(gi * 512, (gi + 1) * 512)
                        kb.op("pe", lambda e, cs=cs: e.matmul(psG.t[:], lhsT=selb.t[0:NE, ex_ * 128:(ex_ + 1) * 128],
                                                              rhs=gth.t[0:NE, cs], start=True, stop=False),
                              r=[selb, gth], w=[psG])
                        kb.op("pe", lambda e, cs=cs: e.matmul(psG.t[:], lhsT=selb.t[0:NE, ex_ * 128:(ex_ + 1) * 128],
                                                              rhs=gtl.t[0:NE, cs], start=False, stop=True),
                              r=[selb, gtl], w=[psG])
                        Gs = Gsr.next()
                        kb.op("act", lambda e, Gs=Gs: e.activation(out=Gs.t[:], in_=psG.t[:], func=AF.Copy), r=[psG], w=[Gs])
                        acts = []
                        for hc in range(8):
                            pg = psg.next()
                            pl = psl.next()
                            for k in range(8):
                                kb.op("pe", lambda e, k=k, hc=hc, pg=pg, cs=cs: e.matmul(
                                    pg.t[:], lhsT=w1e.t[:, k, hc * 128:(hc + 1) * 128], rhs=H2p.t[:, k, cs],
                                    start=(k == 0), stop=(k == 7)), r=[w1e, H2p], w=[pg])
                            for k in range(8):
                                kb.op("pe", lambda e, k=k, hc=hc, pl=pl, cs=cs: e.matmul(
                                    pl.t[:], lhsT=w1e.t[:, k, 1024 + hc * 128:1024 + (hc + 1) * 128], rhs=H2p.t[:, k, cs],
                                    start=(k == 0), stop=(k == 7)), r=[w1e, H2p], w=[pl])
                            gl = glr.next()
                            sg_ = sgr.next()
                            li = lir.next()
                            t1 = t1r.next()
                            t2 = t2r.next()
                            ab_ = abr.next()
                            bc = ex_ * 16 + hc
                            kb.op("dve", lambda e, gl=gl, pg=pg, bc=bc: e.tensor_scalar(
                                out=gl.t[:], in0=pg.t[:], scalar1=b1c.t[:, bc:bc + 1], scalar2=7.0, op0=ALU.add,
                                op1=ALU.min), r=[pg, b1c], w=[gl])
                            kb.op("act", lambda e, gl=gl, sg_=sg_: e.activation(out=sg_.t[:], in_=gl.t[:], func=AF.Sigmoid,
                                                                                scale=1.702), r=[gl], w=[sg_])
                            kb.op("dve", lambda e, li=li, pl=pl, bc=bc: e.tensor_scalar(
                                out=li.t[:], in0=pl.t[:], scalar1=b1c.t[:, bc + 8:bc + 9], scalar2=7.0, op0=ALU.add,
                                op1=ALU.min), r=[pl, b1c], w=[li])
                            kb.op("pool", lambda e, li=li: e.tensor_scalar(out=li.t[:], in0=li.t[:], scalar1=-7.0, scalar2=1.0,
                                                                           op0=ALU.max, op1=ALU.add), r=[li], w=[li])
                            kb.op("pool", lambda e, t1=t1, gl=gl, sg_=sg_: e.tensor_tensor(out=t1.t[:], in0=gl.t[:], in1=sg_.t[:],
                                                                                           op=ALU.mult), r=[gl, sg_], w=[t1])
                            kb.op("pool", lambda e, t2=t2, li=li, Gs=Gs: e.tensor_tensor(out=t2.t[:], in0=li.t[:], in1=Gs.t[:],
                                                                                         op=ALU.mult), r=[li, Gs], w=[t2])
                            kb.op("dve", lambda e, ab_=ab_, t1=t1, t2=t2: e.tensor_tensor(out=ab_.t[:], in0=t1.t[:], in1=t2.t[:],
                                                                                          op=ALU.mult), r=[t1, t2], w=[ab_])
                            acts.append(ab_)
                        for dc in range(8):
                            py = psy.next()
                            for hc in range(8):
                                kb.op("pe", lambda e, hc=hc, dc=dc, py=py: e.matmul(
                                    py.t[:], lhsT=w2e.t[:, hc, dc * 128:(dc + 1) * 128], rhs=acts[hc].t[:],
                                    start=(hc == 0), stop=(hc == 7)), r=[w2e, acts[hc]], w=[py])
                            kb.op("dve", lambda e, dc=dc, py=py, cs=cs: e.tensor_tensor(
                                out=acc.t[:, dc, cs], in0=py.t[:], in1=acc.t[:, dc, cs], op=ALU.add), r=[py, acc], w=[acc])
                kb.barrier()
            with ExitStack() as pf:
                x1r = Rot([kb.T(pf, [128, 8, 512], F32) for _ in range(2)])
                x2r = Rot([kb.T(pf, [128, 8, 512], F32) for _ in range(2)])
                sqr = Rot([kb.T(pf, [128, 512], F32) for _ in range(2)])
                rstd = kb.T(pf, [128, 512], F32)
                for gi in range(NPG):
                    cs = slice(gi * 512, (gi + 1) * 512)
                    gs_ = slice(c0 + gi * 512, c0 + (gi + 1) * 512)
                    x1 = x1r.next()
                    kb.dma("sp", x1.t[:], X1v[:, :, gs_], w=[x1])
                    x2 = x2r.next()
                    for dc in range(8):
                        kb.op("dve", lambda e, dc=dc, cs=cs: e.scalar_tensor_tensor(
                            out=x2.t[:, dc, :], in0=acc.t[:, dc, cs], scalar=mods.t[:, GT2 + dc:GT2 + dc + 1],
                            in1=x1.t[:, dc, :], op0=ALU.mult, op1=ALU.add), r=[acc, mods, x1], w=[x2])
                    if layer == 1:
                        ps = PS7
                        for k in range(8):
                            sq = sqr.next()
                            kb.op("act", lambda e, sq=sq, k=k: e.activation(out=sq.t[:], in_=x2.t[:, k, :], func=AF.Square),
                                  r=[x2], w=[sq])
                            kb.op("pe", lambda e, sq=sq, k=k: e.matmul(ps.t[:], lhsT=onesf.t[:], rhs=sq.t[:], start=(k == 0),
                                                                      stop=(k == 7)), r=[sq, onesf], w=[ps])
                        kb.op("dve", lambda e: e.tensor_scalar(out=rstd.t[:], in0=ps.t[:], scalar1=1.0 / D, scalar2=EPS,
                                                               op0=ALU.mult, op1=ALU.add), r=[ps], w=[rstd])
                        kb.op("act", lambda e: e.activation(out=rstd.t[:], in_=rstd.t[:], func=AF.Sqrt), r=[rstd], w=[rstd])
                        kb.op("dve", lambda e: e.reciprocal(out=rstd.t[:], in_=rstd.t[:]), r=[rstd], w=[rstd])
                        for k in range(8):
                            kb.op("dve", lambda e, k=k: e.scalar_tensor_tensor(
                                out=x2.t[:, k, :], in0=x2.t[:, k, :], scalar=fns.t[:, k:k + 1], in1=rstd.t[:],
                                op0=ALU.mult, op1=ALU.mult), r=[x2, fns, rstd], w=[x2])
                    kb.dma("sp", outv[:, :, gs_], x2.t[:], r=[x2])
                kb.barrier()
    kb.barrier()
    return nc, es, kb


def col8(v):
    return np.ascontiguousarray(np.asarray(v, np.float32).reshape(-1, 128).T)


def toep_vec(fn, dmin, W, shift):
    d = np.arange(W) + dmin + shift
    return fn(d).astype(np.float32)


def prep_common(p, pre, xb, cb, hf, S):
    perm = sigma_perm(S, hf)
    m = {}
    if xb is not None:
        m["xT"] = np.ascontiguousarray(xb[perm].T)
    m["ccol"] = col8(cb)
    m["ada_w"] = p[pre + "ada_w"]
    m["adab"] = col8(p[pre + "ada_b"])
    m["n1col"] = col8(p[pre + "norm1"])
    m["n2col"] = col8(p[pre + "norm2"])
    m["w_in"] = p[pre + "w_in"]
    m["w_out"] = p[pre + "w_out"]
    m["rw"] = p[pre + "router_w"]
    m["rbb"] = np.ascontiguousarray(np.tile(p[pre + "router_b"][None, :], (128, 1)))
    m["w1"] = p[pre + "w1"]
    m["b1col"] = np.ascontiguousarray(p[pre + "b1"].reshape(NE, 16, 128).transpose(2, 0, 1).reshape(128, NE * 16))
    m["w2"] = p[pre + "w2"]
    m["b2"] = p[pre + "b2"]
    m["ident"] = np.eye(128, dtype=np.float32)
    sel = np.zeros((NE, NE * 128), np.float32)
    for e in range(NE):
        sel[e, e * 128:(e + 1) * 128] = 1.0
    m["sel"] = sel
    m["rbflat"] = np.ascontiguousarray(np.tile(p["rel_bias"].reshape(1, 512), (128, 1)))
    return m


def prep_l0(p, xb, cb, hf, S):
    m = prep_common(p, "l0_", xb, cb, hf, S)
    rb = p["rel_bias"]
    m["fbb"] = np.ascontiguousarray(np.tile(p["l0_fox_fb"][None, :], (128, 1)))
    m["R"] = fox_R(hf)
    sh = 128 * (2 * hf - 1)
    dmin, W = toep_geom(-3, 0)
    causal = lambda d: np.where(d >= 0, 0.0, NEG)
    m["Fc"] = np.stack([toep_vec(causal, dmin, W, 0), toep_vec(causal, dmin, W, sh)])
    for g, (win, r) in enumerate(DIL_PAIRS):
        eo, et = dil_evals(win)
        dmin, W = toep_geom(min(eo + et), max(eo + et))
        rows = []
        for j in range(4):
            tab = rb[:, g * 4 + j]

            def fn(d, tab=tab, win=win, r=r):
                ok = (d >= 0) & (d <= win) & (d % r == 0)
                return np.where(ok, tab[t5_bucket_np(np.clip(d, 0, None))], NEG)
            rows.append(toep_vec(fn, dmin, W, 0))
            rows.append(toep_vec(fn, dmin, W, sh))
        m[f"Fd{g}"] = np.stack(rows)
    return m


def prep_l1(p, xb, cb, hf, S):
    m = prep_common(p, "l1_", xb, cb, hf, S)
    rb = p["rel_bias"]
    m["kvn"] = col8(p["l1_kv_norm"])
    m["w_ukv"] = p["l1_w_ukv"]
    m["fncol"] = col8(p["final_norm"])
    tri = np.where(np.arange(128)[None, :] <= np.arange(128)[:, None], 0.0, -1.0e9).astype(np.float32)
    m["trim"] = tri
    m["othd"] = np.full((128, 128), 0.0 if hf == 1 else -1.0e9, np.float32)
    m["pow2"] = np.ascontiguousarray(np.tile((2.0 ** -(np.arange(32) + 1.0))[None, :], (128, 1)).astype(np.float32))
    sh = 128 * (2 * hf - 1)
    dmin, W = toep_geom(-3, DSA_NEAR_E[-1])
    rows = []
    for h in range(16):
        tab = rb[:, h]
        fn = lambda d, tab=tab: np.where(d >= 0, tab[t5_bucket_np(np.clip(d, 0, None))], 0.0)
        rows.append(toep_vec(fn, dmin, W, 0))
        rows.append(toep_vec(fn, dmin, W, sh))
    m["Fs"] = np.stack(rows)
    m["rbfar"] = np.ascontiguousarray(np.tile(rb[31:32, :], (128, 1)))
    return m


_PROG = {}
PV0 = ("xT", "R", "Fc", "Fd0", "Fd1", "Fd2")


def build_fused(S):
    SO = S // 2
    nc = bass.Bass("TRN2", target_bir_lowering=False)
    es = ExitStack()
    kb = KB(nc, es)
    PS = [Tl(es.enter_context(nc.psum_tensor(f"ps{i}", [128, 512], F32))) for i in range(7)]
    ctx = (nc, es, kb, PS)
    shared = {}
    X1F = nc.dram_tensor("X1F", [D, S], F32, kind="Internal").ap()
    build_layer(0, S, ctx=ctx, tag="a_", shared=shared, pervar=PV0, out_ap=X1F[:, 0:SO])
    build_layer(0, S, ctx=ctx, tag="b_", shared=shared, pervar=PV0, out_ap=X1F[:, SO:S])
    build_layer(1, S, ctx=ctx, tag="c_", shared=shared, xT_in=X1F)
    es.close()
    return nc


def _get_prog(S):
    if S not in _PROG:
        _PROG[S] = build_fused(S)
    return _PROG[S]


def kernel(**inputs):
    p = {k: np.ascontiguousarray(np.asarray(v, dtype=np.float32)) for k, v in inputs.items()}
    x = p["x"]
    B, S = x.shape[0], x.shape[1]
    SO = S // 2
    n = 2 * B
    nc = _get_prog(S)
    maps = []
    for c in range(n):
        b, hf = c // 2, c % 2
        ma = prep_l0(p, x[b], p["c"][b], hf, S)
        mb = prep_l0(p, x[b], p["c"][b], 1 - hf, S)
        m1 = prep_l1(p, None, p["c"][b], hf, S)
        m = {}
        for k, v in ma.items():
            m[("a_" + k) if k in PV0 else ("w0_" + k)] = v
        for k in PV0:
            m["b_" + k] = mb[k]
        for k, v in m1.items():
            m["w1_" + k] = v
        maps.append(m)
    res = run_bass_kernel_spmd(nc, maps, core_ids=list(range(n)))
    out = np.empty_like(x)
    for c in range(n):
        own = sigma_perm(S, c % 2)[:SO]
        out[c // 2][own] = np.asarray(res.results[c]["c_out"], np.float32).T
    return out
```

```python
import math
from contextlib import ExitStack
import numpy as np
import concourse.bass as bass
import concourse.mybir as mybir
from concourse.bass_utils import run_bass_kernel_spmd

F32, BF16 = mybir.dt.float32, mybir.dt.bfloat16
ALU = mybir.AluOpType
AF = mybir.ActivationFunctionType
AX = mybir.AxisListType

NDS = 12
ROT = 24000
NEG = -30000.0
D = 1024
NE = 32
EPS = 1e-6


class Dep:
    __slots__ = ("lw", "rd")

    def __init__(self):
        self.lw = None
        self.rd = {}


class Tl:
    __slots__ = ("t", "d")

    def __init__(self, t):
        self.t = t
        self.d = Dep()


class KB:
    def __init__(self, nc, es):
        self.nc = nc
        self.es = es
        self.eng = {"pe": nc.tensor, "dve": nc.vector, "act": nc.scalar, "pool": nc.gpsimd, "sp": nc.sync}
        self.sem = {}
        self.cnt = {}
        self.nsem = 0
        self.final_val = {}
        for e in self.eng:
            self.sem[e] = self._newsem("e_" + e)
            self.cnt[e] = 0
        self.waited = {e: {} for e in self.eng}
        self.dq = ("sp", "pool")
        self.dsem = {q: [self._newsem(f"d_{q}{i}") for i in range(NDS)] for q in self.dq}
        self.dval = {q: [0] * NDS for q in self.dq}
        self.dnext = {q: 0 for q in self.dq}
        self.ninst = 0
        self.nt = 0

    def _newsem(self, name):
        self.nsem += 1
        s = self.es.enter_context(self.nc.semaphore(f"{name}_{self.nsem}"))
        self.final_val[s] = 0
        return s

    def _waitv(self, e, s, v):
        if v <= 0 or self.waited[e].get(s, 0) >= v:
            return
        self.eng[e].wait_ge(s, v)
        self.waited[e][s] = v

    def _deps(self, e, r, w):
        need = {}
        for d in r:
            if d.lw is not None:
                s, v = d.lw
                if need.get(s, 0) < v:
                    need[s] = v
        for d in w:
            if d.lw is not None:
                s, v = d.lw
                if need.get(s, 0) < v:
                    need[s] = v
            for s, v in d.rd.items():
                if need.get(s, 0) < v:
                    need[s] = v
        for s, v in need.items():
            if e == "pe" and s is self.sem["pe"]:
                continue
            self._waitv(e, s, v)

    @staticmethod
    def _dl(x):
        return [a.d if isinstance(a, Tl) else a for a in x]

    def op(self, e, fn, r=(), w=()):
        r = self._dl(r)
        w = self._dl(w)
        if self.cnt[e] >= ROT:
            self.sem[e] = self._newsem("e_" + e)
            self.cnt[e] = 0
        self._deps(e, r, w)
        ins = fn(self.eng[e])
        self.cnt[e] += 1
        s = self.sem[e]
        ins.then_inc(s, 1)
        self.final_val[s] = self.cnt[e]
        self.ninst += 1
        for d in r:
            d.rd[s] = self.cnt[e]
        for d in w:
            d.lw = (s, self.cnt[e])
            d.rd = {}

    def dma(self, q, out, in_, r=(), w=()):
        r = self._dl(r)
        w = self._dl(w)
        i = self.dnext[q]
        self.dnext[q] = (i + 1) % NDS
        s = self.dsem[q][i]
        self._waitv(q, s, self.dval[q][i])
        self._deps(q, r, w)
        ins = self.eng[q].dma_start(out=out, in_=in_)
        self.dval[q][i] += 16
        v = self.dval[q][i]
        ins.then_inc(s, 16)
        self.final_val[s] = v
        self.ninst += 1
        for d in r:
            d.rd[s] = v
        for d in w:
            d.lw = (s, v)
            d.rd = {}

    def barrier(self):
        for e in self.eng:
            for s, v in self.final_val.items():
                self._waitv(e, s, v)

    def T(self, es, shape, dt, name=None):
        self.nt += 1
        t = es.enter_context(self.nc.sbuf_tensor(f"{name or 't'}_{self.nt}", list(shape), dt))
        return Tl(t)


class Rot:
    def __init__(self, items):
        self.items = items
        self.i = 0

    def next(self):
        x = self.items[self.i % len(self.items)]
        self.i += 1
        return x


def t5_bucket_np(dist):
    nb, md = 32, 2048
    me = nb // 2
    d = np.maximum(dist, 0)
    df = np.maximum(d, 1).astype(np.float32)
    large = me + (np.log(df / me) / math.log(md / me) * (nb - me)).astype(np.int32)
    large = np.minimum(large, nb - 1)
    return np.where(d < me, d, large)


DIL_PAIRS = ((128, 1), (512, 4), (2048, 16))


def toep_geom(emin, emax):
    dmin = 256 * emin - 127 - 128
    dmax = 256 * (emax + 3) + 127 + 128
    return dmin, dmax - dmin + 1


def dil_evals(window, part_shift_opts=(-128, 128)):
    own, oth = [], []
    for e in range(-3, 16):
        ok_own = ok_oth = False
        for a in range(4):
            lo = 256 * (a + e) - 127
            hi = 256 * (a + e) + 127
            if hi >= 0 and lo <= window:
                ok_own = True
            for sh in part_shift_opts:
                if hi + sh >= 0 and lo + sh <= window:
                    ok_oth = True
        if ok_own:
            own.append(e)
        if ok_oth:
            oth.append(e)
    return own, oth


FOX_E = [-3, -2, -1, 0]
DSA_NEAR_E = list(range(-3, 10))


def true_tile(st, hf, NH):
    return 2 * st + hf if st < NH else 2 * (st - NH) + 1 - hf


def sigma_perm(S, hf):
    NT = S // 128
    NH = NT // 2
    idx = []
    for st in range(NT):
        T = true_tile(st, hf, NH)
        idx.extend(range(T * 128, T * 128 + 128))
    return np.array(idx)


def fox_R(hf):
    tpos = np.zeros(1024, np.int64)
    for tt in range(8):
        T = (2 * tt + hf) if tt < 4 else (2 * (tt - 4) + 1 - hf)
        tpos[tt * 128:(tt + 1) * 128] = T * 128 + np.arange(128)
    R = (tpos[:, None] <= tpos[None, :]).astype(np.float32)
    return R.reshape(8, 128, 1024)


def build_layer(layer, S, dbg=False, stop_after=99, cut='', ctx=None, tag='', shared=None, xT_in=None, out_ap=None,
                pervar=()):
    NT = S // 128
    NH = NT // 2
    NQ = NH // 4
    NG = S // 512
    SO = S // 2
    if ctx is None:
        nc = bass.Bass("TRN2", target_bir_lowering=False)
        es = ExitStack()
        kb = KB(nc, es)
        PS = [Tl(es.enter_context(nc.psum_tensor(f"ps{i}", [128, 512], F32))) for i in range(7)]
    else:
        nc, es, kb, PS = ctx
    if shared is None:
        shared = {}

    def din(name, shape):
        key = (tag + name) if (name in pervar or not tag) else ("w%d_" % layer + name)
        if key not in shared:
            shared[key] = nc.dram_tensor(key, list(shape), F32, kind="ExternalInput").ap()
        return shared[key]

    def dscr(name, shape, dt):
        return nc.dram_tensor(tag + name, list(shape), dt, kind=("ExternalOutput" if dbg else "Internal")).ap()

    WIN = 3848 if layer == 0 else 1864
    xT = xT_in if xT_in is not None else din("xT", [D, S])
    ccol = din("ccol", [128, 8])
    ada_w = din("ada_w", [D, 6 * D])
    adab = din("adab", [128, 48])
    n1col = din("n1col", [128, 8])
    n2col = din("n2col", [128, 8])
    w_in = din("w_in", [D, WIN])
    NMO = 768 if layer == 0 else 1024
    w_out = din("w_out", [NMO, D])
    rw = din("rw", [D, NE])
    rbb = din("rbb", [128, NE])
    w1 = din("w1", [NE, D, 2 * D])
    b1col = din("b1col", [128, NE * 16])
    w2 = din("w2", [NE, D, D])
    b2 = din("b2", [NE, D])
    identD = din("ident", [128, 128])
    selD = din("sel", [NE, NE * 128])
    rbflat = din("rbflat", [128, 512])
    if layer == 0:
        fbb = din("fbb", [128, 8])
        RD = din("R", [8, 128, 1024])
        dgeo = []
        for (w_, r_) in DIL_PAIRS:
            eo, et = dil_evals(w_)
            dgeo.append((eo, et) + toep_geom(min(eo + et), max(eo + et)))
        fgeo = toep_geom(-3, 0)
        FcD = din("Fc", [2, fgeo[1]])
        FdD = [din(f"Fd{g}", [8, dgeo[g][3]]) for g in range(3)]
        FcR = dscr("FcR", [2, 128, fgeo[1]], F32)
        FdR = [dscr(f"FdR{g}", [8, 128, dgeo[g][3]], F32) for g in range(3)]
    else:
        kvn = din("kvn", [128, 2])
        w_ukv = din("w_ukv", [256, 2048])
        fnc = din("fncol", [128, 8])
        trimD = din("trim", [128, 128])
        othdD = din("othd", [128, 128])
        pow2D = din("pow2", [128, 32])
        sgeo = toep_geom(-3, DSA_NEAR_E[-1])
        FsD = din("Fs", [32, sgeo[1]])
        FsR = dscr("FsR", [32, 128, sgeo[1]], F32)
        rbfar = din("rbfar", [128, 16])
    outD = out_ap if out_ap is not None else nc.dram_tensor(tag + "out", [D, SO], F32, kind="ExternalOutput").ap()

    MT = dscr("MT", [NMO, SO], BF16)
    X1 = dscr("X1", [D, SO], F32)
    H2 = dscr("H2", [D, SO], BF16)
    GTs = dscr("GTs", [NE, SO], F32)
    if layer == 0:
        KAF = dscr("KAF", [8, 72, S], BF16)
        QAF = dscr("QAF", [8, 72, SO], BF16)
        VF = dscr("VF", [8, 128, NT, 128], BF16)
        KAD = dscr("KAD", [12, 72, S], BF16)
        QAD = dscr("QAD", [12, 72, SO], BF16)
        VD = dscr("VD", [12, 128, NT, 128], BF16)
    else:
        KAS = dscr("KAS", [16, 72, S], BF16)
        QAS = dscr("QAS", [16, 72, SO], BF16)
        VS = dscr("VS", [16, 128, NT, 128], BF16)
        KI = dscr("KI", [128, S], BF16)
        QI = dscr("QI", [512, SO], BF16)
        WI = dscr("WI", [SO, 128], F32)
        NM = dscr("NM", [NQ, NT, 128, 512], BF16)


    kb.PS = PS
    kb.shared = shared
    if not hasattr(kb, "P0t"):
        P0 = ExitStack()
        es.enter_context(P0)
        identf = kb.T(P0, [128, 128], F32)
        identb = kb.T(P0, [128, 128], BF16)
        onesf = kb.T(P0, [128, 128], F32)
        onesb = kb.T(P0, [128, 512], BF16)
        ind2 = kb.T(P0, [128, 2], BF16)
        mods = kb.T(P0, [128, 48], F32)
        gs1 = kb.T(P0, [128, 8], F32)
        gs2 = kb.T(P0, [128, 8], F32)
        bmax = kb.T(P0, [128, 1], F32)
        zcol = kb.T(P0, [128, 1], F32)
        kb.op("dve", lambda e: e.memset(zcol.t[:], 0.0), w=[zcol])
        kb.dma("sp", identf.t[:], identD[:, :], w=[identf])
        kb.op("dve", lambda e: e.tensor_copy(out=identb.t[:], in_=identf.t[:]), r=[identf], w=[identb])
        kb.op("dve", lambda e: e.memset(onesf.t[:], 1.0), w=[onesf])
        kb.op("dve", lambda e: e.memset(onesb.t[:], 1.0), w=[onesb])
        kb.op("dve", lambda e: e.memset(ind2.t[:], 0.0), w=[ind2])
        kb.op("dve", lambda e: e.memset(ind2.t[0:64, 0:1], 1.0), w=[ind2])
        kb.op("dve", lambda e: e.memset(ind2.t[64:128, 1:2], 1.0), w=[ind2])
        kb.P0t = (identf, identb, onesf, onesb, ind2, mods, gs1, gs2, bmax, zcol)
    identf, identb, onesf, onesb, ind2, mods, gs1, gs2, bmax, zcol = kb.P0t

    with ExitStack() as ph:
        cc = kb.T(ph, [128, 8], F32)
        sc = kb.T(ph, [128, 8], F32)
        ab = kb.T(ph, [128, 48], F32)
        n1 = kb.T(ph, [128, 8], F32)
        n2 = kb.T(ph, [128, 8], F32)
        rbf = kb.T(ph, [128, 512], F32)
        kb.dma("sp", cc.t[:], ccol[:, :], w=[cc])
        kb.dma("sp", ab.t[:], adab[:, :], w=[ab])
        kb.dma("sp", n1.t[:], n1col[:, :], w=[n1])
        kb.dma("sp", n2.t[:], n2col[:, :], w=[n2])
        kb.dma("sp", rbf.t[:], rbflat[:, :], w=[rbf])
        kb.op("dve", lambda e: e.reduce_max(out=bmax.t[:], in_=rbf.t[:], axis=AX.X), r=[rbf], w=[bmax])
        kb.op("act", lambda e: e.activation(out=sc.t[:], in_=cc.t[:], func=AF.Silu), r=[cc], w=[sc])
        aw = Rot([kb.T(ph, [128, 8, 1024], F32) for _ in range(2)])
        awv = ada_w.rearrange("(k p) f -> p k f", p=128)
        psm = PS[0]
        for j in range(6):
            a = aw.next()
            kb.dma("sp", a.t[:], awv[:, :, j * 1024:(j + 1) * 1024], w=[a])
            for fc in range(8):
                for k in range(8):
                    kb.op("pe", lambda e, a=a, fc=fc, k=k, j=j: e.matmul(
                        psm.t[:, j * 8 + fc:j * 8 + fc + 1], lhsT=a.t[:, k, fc * 128:(fc + 1) * 128],
                        rhs=sc.t[:, k:k + 1], start=(k == 0), stop=(k == 7)), r=[a, sc], w=[psm])
        kb.op("dve", lambda e: e.tensor_tensor(out=mods.t[:], in0=psm.t[:, 0:48], in1=ab.t[:], op=ALU.add),
              r=[psm, ab], w=[mods])
        kb.op("dve", lambda e: e.scalar_tensor_tensor(out=gs1.t[:], in0=mods.t[:, 8:16], scalar=1.0, in1=n1.t[:],
                                                     op0=ALU.add, op1=ALU.mult), r=[mods, n1], w=[gs1])
        kb.op("dve", lambda e: e.scalar_tensor_tensor(out=gs2.t[:], in0=mods.t[:, 32:40], scalar=1.0, in1=n2.t[:],
                                                     op0=ALU.add, op1=ALU.mult), r=[mods, n2], w=[gs2])
        kb.barrier()
    SH1, GT1, SH2, GT2 = 0, 16, 24, 40
    if stop_after <= 0:
        return nc, es, kb

    def rms_mod(ph_tiles, xin, gs, sh_off, hout, hf32=None):
        sqr, rstd, tmpr, pss = ph_tiles
        ps = pss
        for k in range(8):
            sq = sqr.next()
            kb.op("act", lambda e, sq=sq, k=k: e.activation(out=sq.t[:], in_=xin.t[:, k, :], func=AF.Square),
                  r=[xin], w=[sq])
            kb.op("pe", lambda e, sq=sq, k=k: e.matmul(ps.t[:], lhsT=onesf.t[:], rhs=sq.t[:], start=(k == 0),
                                                      stop=(k == 7)), r=[sq, onesf], w=[ps])
        kb.op("dve", lambda e: e.tensor_scalar(out=rstd.t[:], in0=ps.t[:], scalar1=1.0 / D, scalar2=EPS,
                                               op0=ALU.mult, op1=ALU.add), r=[ps], w=[rstd])
        kb.op("act", lambda e: e.activation(out=rstd.t[:], in_=rstd.t[:], func=AF.Sqrt), r=[rstd], w=[rstd])
        kb.op("dve", lambda e: e.reciprocal(out=rstd.t[:], in_=rstd.t[:]), r=[rstd], w=[rstd])
        for k in range(8):
            tm = tmpr.next()
            kb.op("dve", lambda e, tm=tm, k=k: e.tensor_tensor(out=tm.t[:], in0=xin.t[:, k, :], in1=rstd.t[:],
                                                               op=ALU.mult), r=[xin, rstd], w=[tm])
            if hf32 is not None:
                kb.op("act", lambda e, tm=tm, k=k: e.activation(
                    out=hf32.t[:, k, :], in_=tm.t[:], func=AF.Identity, scale=gs.t[:, k:k + 1],
                    bias=mods.t[:, sh_off + k:sh_off + k + 1]), r=[tm, gs, mods], w=[hf32])
                kb.op("pool", lambda e, k=k: e.tensor_copy(out=hout.t[:, k, :], in_=hf32.t[:, k, :]),
                      r=[hf32], w=[hout])
            else:
                kb.op("act", lambda e, tm=tm, k=k: e.activation(
                    out=hout.t[:, k, :], in_=tm.t[:], func=AF.Identity, scale=gs.t[:, k:k + 1],
                    bias=mods.t[:, sh_off + k:sh_off + k + 1]), r=[tm, gs, mods], w=[hout])

    xTv = xT.rearrange("(k p) t -> p k t", p=128)

    def projT(h, w, c0, ps, ncols=128):
        for k in range(8):
            kb.op("pe", lambda e, k=k: e.matmul(ps.t[0:ncols, :], lhsT=w.t[:, k, c0:c0 + ncols], rhs=h.t[:, k, :],
                                               start=(k == 0), stop=(k == 7)), r=[h, w], w=[ps])

    def projTok(h, j, w, c0, n, ps):
        for k in range(8):
            kb.op("pe", lambda e, k=k: e.matmul(ps.t[:, 0:n], lhsT=h.t[:, k, j * 128:(j + 1) * 128],
                                               rhs=w.t[:, k, c0:c0 + n], start=(k == 0), stop=(k == 7)),
                  r=[h, w], w=[ps])

    with ExitStack() as ph:
        wsb = kb.T(ph, [128, 8, WIN], BF16)
        wv = w_in.rearrange("(k p) n -> p k n", p=128)
        for k in range(8):
            kb.dma("pool", wsb.t[:, k, :], wv[:, k, :], w=[wsb])
        xgr = Rot([kb.T(ph, [128, 8, 512], F32) for _ in range(2)])
        hr = Rot([kb.T(ph, [128, 8, 512], BF16) for _ in range(2)])
        sqr = Rot([kb.T(ph, [128, 512], F32) for _ in range(2)])
        tmpr = Rot([kb.T(ph, [128, 512], F32) for _ in range(2)])
        rstd = kb.T(ph, [128, 512], F32)
        rmt = (sqr, rstd, tmpr, PS[0])
        stg = Rot([kb.T(ph, [128, 512], BF16) for _ in range(3)])
        sqb = Rot([kb.T(ph, [128, 512], BF16) for _ in range(2)])
        NVH = 20 if layer == 0 else 16
        psA = Rot([PS[1], PS[2]])
        psN = PS[3]
        psV = Rot([PS[4], PS[5]])
        psC = PS[6]
        NKN = 16
        kn = kb.T(ph, [128, NKN], F32)
        kms = kb.T(ph, [128, NKN], F32)
        tm2 = kb.T(ph, [128, 1], F32)
        kb.op("dve", lambda e: e.memset(kn.t[:], 0.0), w=[kn])

        def head_pair_K(h, c0, KA, hd0, col0, knc, scale=None):
            ps = psA.next()
            projT(h, wsb, c0, ps)
            s = stg.next()
            kb.op("act", lambda e: e.activation(out=s.t[:], in_=ps.t[:], func=AF.Copy,
                                                scale=(1.0 if scale is None else scale)), r=[ps], w=[s])
            q = sqb.next()
            kb.op("act", lambda e: e.activation(out=q.t[:], in_=ps.t[:], func=AF.Square), r=[ps], w=[q])
            kb.dma("sp", KA[hd0, 0:64, col0:col0 + 512], s.t[0:64, :], r=[s])
            kb.dma("sp", KA[hd0 + 1, 0:64, col0:col0 + 512], s.t[64:128, :], r=[s])
            kb.op("pe", lambda e: e.matmul(psN.t[0:2, :], lhsT=ind2.t[:, 0:2], rhs=q.t[:], start=True, stop=True),
                  r=[q, ind2], w=[psN])
            return ps

        def kn_update(knc):
            kb.op("dve", lambda e: e.reduce_max(out=tm2.t[0:2, :], in_=psN.t[0:2, :], axis=AX.X), r=[psN], w=[tm2])
            kb.op("dve", lambda e: e.tensor_max(out=kn.t[0:2, knc:knc + 1], in0=kn.t[0:2, knc:knc + 1],
                                                in1=tm2.t[0:2, :]), r=[kn, tm2], w=[kn])

        def tokV(h, c0, n, sv, h0, j):
            for (o, m) in ([(0, min(512, n))] + ([(512, n - 512)] if n > 512 else [])):
                ps = psV.next()
                projTok(h, j, wsb, c0 + o, m, ps)
                nh = m // 64
                hh = h0 + o // 64
                kb.op("dve", lambda e, ps=ps, m=m, nh=nh, hh=hh: e.tensor_copy(
                    out=sv.t[:, hh:hh + nh, j, 0:64], in_=ps.t[:, 0:m].rearrange("p (h c) -> p h c", h=nh)),
                    r=[ps], w=[sv])

        def flushV(sv, VDst, h0, nh, sg):
            for hh in range(nh):
                kb.dma("sp", VDst[hh, :, sg * 4:(sg + 1) * 4, :], sv.t[:, h0 + hh, :, :], r=[sv])

        if layer == 0:
            Lall = kb.T(ph, [128, NT, 8], F32)
            fb = kb.T(ph, [128, 8], F32)
            kb.dma("sp", fb.t[:], fbb[:, :], w=[fb])
            zt = kb.T(ph, [128, 8], F32)
            for p_ in range(2):
                kb.dma("sp", FcR[p_, :, :], bass.AP(tensor=FcD.tensor, offset=p_ * fgeo[1], ap=[[0, 128], [1, fgeo[1]]]))
            for g in range(3):
                W_ = dgeo[g][3]
                for q_ in range(8):
                    kb.dma("sp", FdR[g][q_, :, :], bass.AP(tensor=FdD[g].tensor, offset=q_ * W_, ap=[[0, 128], [1, W_]]))
            p1 = ExitStack()
            stv = Rot([kb.T(p1, [128, NVH, 4, 128], BF16) for _ in range(1)])
            for v_ in stv.items:
                kb.op("pool", lambda e, v_=v_: e.memset(v_.t[:, :, :, 64:128], 1.0), w=[v_])
            for sg in range(NG):
                xg = xgr.next()
                kb.dma("sp", xg.t[:], xTv[:, :, sg * 512:(sg + 1) * 512], w=[xg])
                h = hr.next()
                rms_mod(rmt, xg, gs1, SH1, h)
                for c4 in range(4):
                    head_pair_K(h, 512 + 128 * c4, KAF, 2 * c4, sg * 512, c4)
                    kn_update(c4)
                for c6 in range(6):
                    head_pair_K(h, 2312 + 128 * c6, KAD, 2 * c6, sg * 512, 4 + c6)
                    kn_update(4 + c6)
                sv = stv.next()
                for j in range(4):
                    tokV(h, 1024, 512, sv, 0, j)
                    tokV(h, 3080, 768, sv, 8, j)
                flushV(sv, VF, 0, 8, sg)
                flushV(sv, VD, 8, 12, sg)
                for j in range(4):
                    projTok(h, j, wsb, 1536, 8, psC)
                    kb.op("dve", lambda e: e.tensor_tensor(out=zt.t[:], in0=psC.t[:, 0:8], in1=fb.t[:], op=ALU.add),
                          r=[psC, fb], w=[zt])
                    kb.op("act", lambda e: e.activation(out=zt.t[:], in_=zt.t[:], func=AF.Exp, scale=-1.0), r=[zt], w=[zt])
                    kb.op("act", lambda e, j=j, sg=sg: e.activation(out=Lall.t[:, sg * 4 + j, :], in_=zt.t[:], func=AF.Ln,
                                                                    bias=1.0), r=[zt], w=[Lall])
                kb.dma("sp", KAF[0:8, 64, sg * 512:(sg + 1) * 512], onesb.t[0:8, :], r=[onesb])
                kb.dma("sp", KAF[0:8, 65, sg * 512:(sg + 1) * 512], onesb.t[0:8, :], r=[onesb])
                kb.dma("sp", KAF[0:8, 66, sg * 512:(sg + 1) * 512], onesb.t[0:8, :], r=[onesb])
                kb.dma("sp", KAF[0:8, 70, sg * 512:(sg + 1) * 512], onesb.t[0:8, :], r=[onesb])
                kb.dma("sp", KAD[0:12, 64, sg * 512:(sg + 1) * 512], onesb.t[0:12, :], r=[onesb])
                if sg < NQ:
                    for rr in (67, 68, 69):
                        kb.dma("sp", QAF[0:8, rr, sg * 512:(sg + 1) * 512], onesb.t[0:8, :], r=[onesb])
            kb.barrier()
            p1.close()
            p1b = ExitStack()
            Rsb = kb.T(p1b, [128, 8, 1024], F32)
            kb.dma("sp", Rsb.t[:], RD.rearrange("a p c -> p a c"), w=[Rsb])
            carry = kb.T(p1b, [128, 1], F32)
            kb.op("dve", lambda e: e.memset(carry.t[:], 0.0), w=[carry])
            Cg = kb.T(p1b, [128, 512], F32)
            r1 = kb.T(p1b, [128, 512], F32)
            cbr = Rot([kb.T(p1b, [128, 512], BF16) for _ in range(4)])
            for i in range(NQ):
                tiles = [4 * i + a for a in range(4)] + [NH + 4 * i + a for a in range(4)]
                for half in range(2):
                    for tt in range(8):
                        kb.op("pe", lambda e, tt=tt, half=half: e.matmul(
                            psC.t[0:8, :], lhsT=Lall.t[:, tiles[tt], :], rhs=Rsb.t[:, tt, half * 512:(half + 1) * 512],
                            start=(tt == 0), stop=(tt == 7)), r=[Lall, Rsb], w=[psC])
                    kb.op("dve", lambda e: e.tensor_scalar(out=Cg.t[0:8, :], in0=psC.t[0:8, :], scalar1=carry.t[0:8, 0:1],
                                                           scalar2=None, op0=ALU.add), r=[psC, carry], w=[Cg])
                    cur = Cg
                    scol = (i * 512) if half == 0 else (SO + i * 512)
                    for p3 in range(3):
                        cb = cbr.next()
                        kb.op("dve", lambda e, cur=cur, cb=cb: e.tensor_copy(out=cb.t[0:8, :], in_=cur.t[0:8, :]),
                              r=[cur], w=[cb])
                        kb.dma("sp", KAF[0:8, 67 + p3, scol:scol + 512], cb.t[0:8, :], r=[cb])
                        if half == 0:
                            nb = cbr.next()
                            kb.op("dve", lambda e, cb=cb, nb=nb: e.tensor_scalar(
                                out=nb.t[0:8, :], in0=cb.t[0:8, :], scalar1=-1.0, scalar2=None, op0=ALU.mult),
                                r=[cb], w=[nb])
                            kb.dma("sp", QAF[0:8, 64 + p3, i * 512:(i + 1) * 512], nb.t[0:8, :], r=[nb])
                        if p3 < 2:
                            kb.op("dve", lambda e, cur=cur, cb=cb: e.tensor_tensor(
                                out=r1.t[0:8, :], in0=cur.t[0:8, :], in1=cb.t[0:8, :], op=ALU.subtract),
                                r=[cur, cb], w=[r1])
                            cur = r1
                for tt in range(8):
                    kb.op("pe", lambda e, tt=tt: e.matmul(psC.t[0:8, 0:1], lhsT=Lall.t[:, tiles[tt], :],
                                                         rhs=onesf.t[:, 0:1], start=(tt == 0), stop=(tt == 7)),
                          r=[Lall, onesf], w=[psC])
                kb.op("dve", lambda e: e.tensor_tensor(out=carry.t[0:8, :], in0=carry.t[0:8, :], in1=psC.t[0:8, 0:1],
                                                       op=ALU.add), r=[carry, psC], w=[carry])
            kb.barrier()
            p1b.close()
        else:
            wu = kb.T(ph, [128, 2, 2048], BF16)
            wuv = w_ukv.rearrange("(k p) n -> p k n", p=128)
            for k in range(0 if 'U' in cut else 2):
                kb.dma("pool", wu.t[:, k, :], wuv[:, k, :], w=[wu])
            kvns = kb.T(ph, [128, 2], F32)
            if 'N' not in cut:
                kb.dma("sp", kvns.t[:], kvn[:, :], w=[kvns])
            ckf = Rot([kb.T(ph, [128, 2, 512], F32) for _ in range(2)])
            ckb = Rot([kb.T(ph, [128, 2, 512], BF16) for _ in range(2)])
            wki = kb.T(ph, [128, 8, 128], BF16)
            if 'W' not in cut:
                kb.op("dve", lambda e: e.tensor_copy(out=wki.t[:, :, 0:64], in_=wsb.t[:, :, 1792:1856]), r=[wsb], w=[wki])
                kb.op("dve", lambda e: e.tensor_copy(out=wki.t[:, :, 64:128], in_=wsb.t[:, :, 1792:1856]), r=[wsb], w=[wki])
            p1 = ExitStack()
            stv = Rot([kb.T(p1, [128, NVH, 4, 128], BF16) for _ in range(1)])
            for v_ in stv.items:
                kb.op("pool", lambda e, v_=v_: e.memset(v_.t[:, :, :, 64:128], 1.0), w=[v_])
            for sg in range(NG):
                xg = xgr.next()
                kb.dma("sp", xg.t[:], xTv[:, :, sg * 512:(sg + 1) * 512], w=[xg])
                h = hr.next()
                rms_mod(rmt, xg, gs1, SH1, h)
                cf = ckf.next()
                cb_ = ckb.next()
                pss = PS[0]
                if 'B' in cut:
                    continue
                qs_ = []
                for k2 in range(2):
                    ps = psA.next()
                    projT(h, wsb, 1024 + 128 * k2, ps)
                    kb.op("act", lambda e, ps=ps, k2=k2: e.activation(out=cf.t[:, k2, :], in_=ps.t[:], func=AF.Copy), r=[ps], w=[cf])
                    q = sqr.next()
                    kb.op("act", lambda e, q=q, ps=ps: e.activation(out=q.t[:], in_=ps.t[:], func=AF.Square), r=[ps], w=[q])
                    qs_.append(q)
                if 'P' in cut:
                    continue
                for k2 in range(2):
                    kb.op("pe", lambda e, k2=k2: e.matmul(pss.t[:], lhsT=onesf.t[:], rhs=qs_[k2].t[:], start=(k2 == 0),
                                                          stop=(k2 == 1)), r=[qs_[k2], onesf], w=[pss])
                if 'R' in cut:
                    continue
                kb.op("dve", lambda e: e.tensor_scalar(out=rstd.t[:], in0=pss.t[:], scalar1=1.0 / 256, scalar2=EPS,
                                                       op0=ALU.mult, op1=ALU.add), r=[pss], w=[rstd])
                kb.op("act", lambda e: e.activation(out=rstd.t[:], in_=rstd.t[:], func=AF.Sqrt), r=[rstd], w=[rstd])
                kb.op("dve", lambda e: e.reciprocal(out=rstd.t[:], in_=rstd.t[:]), r=[rstd], w=[rstd])
                if 'T' in cut:
                    continue
                for k2 in range(2):
                    tm = tmpr.next()
                    kb.op("dve", lambda e, tm=tm, k2=k2: e.tensor_tensor(out=tm.t[:], in0=cf.t[:, k2, :], in1=rstd.t[:],
                                                                         op=ALU.mult), r=[cf, rstd], w=[tm])
                    kb.op("act", lambda e, tm=tm, k2=k2: e.activation(out=cb_.t[:, k2, :], in_=tm.t[:], func=AF.Identity,
                                                                      scale=kvns.t[:, k2:k2 + 1], bias=zcol.t[:, 0:1]),
                          r=[tm, kvns, zcol], w=[cb_])
                for c8 in range(0 if 'C' in cut else 8):
                    ps = psA.next()
                    for k2 in range(2):
                        kb.op("pe", lambda e, k2=k2, c8=c8, ps=ps: e.matmul(
                            ps.t[:], lhsT=wu.t[:, k2, c8 * 128:(c8 + 1) * 128], rhs=cb_.t[:, k2, :],
                            start=(k2 == 0), stop=(k2 == 1)), r=[wu, cb_], w=[ps])
                    s = stg.next()
                    kb.op("act", lambda e, s=s, ps=ps: e.activation(out=s.t[:], in_=ps.t[:], func=AF.Copy), r=[ps], w=[s])
                    q = sqb.next()
                    kb.op("act", lambda e, q=q, ps=ps: e.activation(out=q.t[:], in_=ps.t[:], func=AF.Square), r=[ps], w=[q])
                    kb.dma("sp", KAS[2 * c8, 0:64, sg * 512:(sg + 1) * 512], s.t[0:64, :], r=[s])
                    kb.dma("sp", KAS[2 * c8 + 1, 0:64, sg * 512:(sg + 1) * 512], s.t[64:128, :], r=[s])
                    kb.op("pe", lambda e, q=q: e.matmul(psN.t[0:2, :], lhsT=ind2.t[:, 0:2], rhs=q.t[:], start=True,
                                                       stop=True), r=[q, ind2], w=[psN])
                    kn_update(c8)
                sv = stv.next()
                for j in range(0 if 'D' in cut else 4):
                    for o in (0, 512):
                        ps = psV.next()
                        for k2 in range(2):
                            kb.op("pe", lambda e, k2=k2, o=o, ps=ps, j=j: e.matmul(
                                ps.t[:], lhsT=cb_.t[:, k2, j * 128:(j + 1) * 128], rhs=wu.t[:, k2, 1024 + o:1536 + o],
                                start=(k2 == 0), stop=(k2 == 1)), r=[wu, cb_], w=[ps])
                        kb.op("dve", lambda e, o=o, ps=ps, j=j: e.tensor_copy(
                            out=sv.t[:, o // 64:o // 64 + 8, j, 0:64], in_=ps.t[:, :].rearrange("p (h c) -> p h c", h=8)),
                            r=[ps], w=[sv])
                if 'D' not in cut:
                    flushV(sv, VS, 0, 16, sg)
                if 'E' in cut:
                    continue
                ps = psA.next()
                projT(h, wki, 0, ps)
                s = stg.next()
                kb.op("act", lambda e, s=s, ps=ps: e.activation(out=s.t[:], in_=ps.t[:], func=AF.Copy), r=[ps], w=[s])
                kb.dma("sp", KI[:, sg * 512:(sg + 1) * 512], s.t[:, :], r=[s])
                kb.dma("sp", KAS[0:16, 64, sg * 512:(sg + 1) * 512], onesb.t[0:16, :], r=[onesb])
            kb.barrier()
            p1.close()

        kb.op("act", lambda e: e.activation(out=kms.t[0:2, :], in_=kn.t[0:2, :], func=AF.Sqrt), r=[kn], w=[kms])
        kb.op("dve", lambda e: e.tensor_scalar(out=kms.t[0:2, :], in0=kms.t[0:2, :], scalar1=0.125 * 1.05, scalar2=None,
                                               op0=ALU.mult), r=[kms], w=[kms])
        nqr = Rot([kb.T(ph, [128, 512], F32) for _ in range(2)])
        mgr = Rot([kb.T(ph, [128, 512], F32) for _ in range(4)])
        mrr = Rot([kb.T(ph, [128, 512], BF16) for _ in range(2)])

        def q_pair(h, c0, QA, hd0, col0):
            ps = psA.next()
            projT(h, wsb, c0, ps)
            s = stg.next()
            kb.op("act", lambda e: e.activation(out=s.t[:], in_=ps.t[:], func=AF.Copy, scale=0.125), r=[ps], w=[s])
            q = sqb.next()
            kb.op("act", lambda e: e.activation(out=q.t[:], in_=ps.t[:], func=AF.Square), r=[ps], w=[q])
            kb.dma("sp", QA[hd0, 0:64, col0:col0 + 512], s.t[0:64, :], r=[s])
            kb.dma("sp", QA[hd0 + 1, 0:64, col0:col0 + 512], s.t[64:128, :], r=[s])
            kb.op("pe", lambda e: e.matmul(psN.t[0:2, :], lhsT=ind2.t[:, 0:2], rhs=q.t[:], start=True, stop=True),
                  r=[q, ind2], w=[psN])
            nq = nqr.next()
            kb.op("act", lambda e: e.activation(out=nq.t[0:2, :], in_=psN.t[0:2, :], func=AF.Sqrt), r=[psN], w=[nq])
            return nq

        for i in range(NQ):
            xg = xgr.next()
            kb.dma("sp", xg.t[:], xTv[:, :, i * 512:(i + 1) * 512], w=[xg])
            h = hr.next()
            rms_mod(rmt, xg, gs1, SH1, h)
            c0s = i * 512
            if layer == 0:
                for c4 in range(4):
                    nq = q_pair(h, 128 * c4, QAF, 2 * c4, c0s)
                    mr = mrr.next()
                    kb.op("dve", lambda e, nq=nq, mr=mr, c4=c4: e.tensor_scalar(
                        out=mr.t[0:2, :], in0=nq.t[0:2, :], scalar1=kms.t[0:2, c4:c4 + 1], scalar2=-1.0,
                        op0=ALU.mult, op1=ALU.mult), r=[nq, kms], w=[mr])
                    kb.dma("sp", QAF[2 * c4, 70, c0s:c0s + 512], mr.t[0:1, :], r=[mr])
                    kb.dma("sp", QAF[2 * c4 + 1, 70, c0s:c0s + 512], mr.t[1:2, :], r=[mr])
                for sp in range(2):
                    mgs = []
                    for gp in range(3):
                        c6 = 2 * gp + sp
                        nq = q_pair(h, 1544 + 128 * c6, QAD, 2 * c6, c0s)
                        mg = mgr.next()
                        kb.op("dve", lambda e, nq=nq, mg=mg, c6=c6: e.tensor_scalar(
                            out=mg.t[0:2, :], in0=nq.t[0:2, :], scalar1=kms.t[0:2, 4 + c6:5 + c6], scalar2=None,
                            op0=ALU.mult), r=[nq, kms], w=[mg])
                        mgs.append(mg)
                    kb.op("dve", lambda e: e.tensor_max(out=mgs[0].t[0:2, :], in0=mgs[0].t[0:2, :], in1=mgs[1].t[0:2, :]),
                          r=[mgs[0], mgs[1]], w=[mgs[0]])
                    kb.op("dve", lambda e: e.tensor_max(out=mgs[0].t[0:2, :], in0=mgs[0].t[0:2, :], in1=mgs[2].t[0:2, :]),
                          r=[mgs[0], mgs[2]], w=[mgs[0]])
                    mr = mrr.next()
                    kb.op("dve", lambda e, mr=mr: e.tensor_scalar(out=mr.t[0:2, :], in0=mgs[0].t[0:2, :],
                                                                  scalar1=bmax.t[0:2, 0:1], scalar2=-1.0, op0=ALU.add,
                                                                  op1=ALU.mult), r=[mgs[0], bmax], w=[mr])
                    for gp in range(3):
                        c6 = 2 * gp + sp
                        kb.dma("sp", QAD[2 * c6, 64, c0s:c0s + 512], mr.t[0:1, :], r=[mr])
                        kb.dma("sp", QAD[2 * c6 + 1, 64, c0s:c0s + 512], mr.t[1:2, :], r=[mr])
            else:
                for c8 in range(0 if 'F' in cut else 8):
                    nq = q_pair(h, 128 * c8, QAS, 2 * c8, c0s)
                    mg = mgr.next()
                    kb.op("dve", lambda e, nq=nq, mg=mg, c8=c8: e.tensor_scalar(
                        out=mg.t[0:2, :], in0=nq.t[0:2, :], scalar1=kms.t[0:2, c8:c8 + 1], scalar2=None,
                        op0=ALU.mult), r=[nq, kms], w=[mg])
                    mr2 = mrr.next()
                    kb.op("dve", lambda e, mg=mg, mr2=mr2: e.tensor_scalar(
                        out=mr2.t[0:2, :], in0=mg.t[0:2, :], scalar1=bmax.t[0:2, 0:1], scalar2=-1.0, op0=ALU.add,
                        op1=ALU.mult), r=[mg, bmax], w=[mr2])
                    kb.dma("sp", QAS[2 * c8, 64, c0s:c0s + 512], mr2.t[0:1, :], r=[mr2])
                    kb.dma("sp", QAS[2 * c8 + 1, 64, c0s:c0s + 512], mr2.t[1:2, :], r=[mr2])
                for c4 in range(0 if 'G' in cut else 4):
                    ps = psA.next()
                    projT(h, wsb, 1280 + 128 * c4, ps)
                    s = stg.next()
                    kb.op("act", lambda e, s=s, ps=ps: e.activation(out=s.t[:], in_=ps.t[:], func=AF.Copy), r=[ps], w=[s])
                    kb.dma("sp", QI[c4 * 128:(c4 + 1) * 128, c0s:c0s + 512], s.t[:, :], r=[s])
                for j in range(0 if 'H' in cut else 4):
                    projTok(h, j, wsb, 1856, 8, psC)
                    wt = tmpr.next()
                    kb.op("dve", lambda e, wt=wt: e.tensor_copy(out=wt.t[:, 0:8], in_=psC.t[:, 0:8]), r=[psC], w=[wt])
                    kb.dma("sp", WI[c0s + j * 128:c0s + (j + 1) * 128, :], wt.t[:, 0:128], r=[wt])
        kb.barrier()

    if stop_after <= 1:
        return nc, es, kb
    def attention_head(ph, kaD, qaD, vD, vc0, Kd, blocks_fn, finalize, bufs, expbias=None, tail_fn=None):
        ka, qa, vp, psS, psO, pTr = bufs
        kb.dma("sp", ka.t[0:Kd, :], kaD[0:Kd, :], w=[ka])
        kb.dma("sp", qa.t[0:Kd, :], qaD[0:Kd, :], w=[qa])
        kb.dma("sp", vp.t[:], vD, w=[vp])
        for i in range(NQ):
            blocks = blocks_fn(i)
            po = psO.next()
            nb = len(blocks)
            pend = []
            for n in range(nb + 2):
                if n < nb:
                    kt, masks, far = blocks[n]
                    if callable(masks):
                        masks = masks()
                    ps = psS.next()
                    nm_ = len(masks)
                    kb.op("pe", lambda e, kt=kt, ps=ps, nm_=nm_: e.matmul(
                        ps.t[:], lhsT=ka.t[0:Kd, kt * 128:(kt + 1) * 128], rhs=qa.t[0:Kd, i * 512:(i + 1) * 512],
                        start=True, stop=(nm_ == 0)), r=[ka, qa], w=[ps])
                    for mi, (lt, mk, mkap) in enumerate(masks):
                        kb.op("pe", lambda e, lt=lt, mkap=mkap, ps=ps, mi=mi, nm_=nm_: e.matmul(
                            ps.t[:], lhsT=lt.t[:], rhs=mkap, start=False, stop=(mi == nm_ - 1)), r=[lt, mk], w=[ps])
                    pT = pTr.next()
                    if far and expbias is not None:
                        kb.op("act", lambda e, pT=pT, ps=ps: e.activation(out=pT.t[:], in_=ps.t[:], func=AF.Exp,
                                                                          bias=expbias[1]), r=[ps, expbias[0]], w=[pT])
                    else:
                        kb.op("act", lambda e, pT=pT, ps=ps: e.activation(out=pT.t[:], in_=ps.t[:], func=AF.Exp),
                              r=[ps], w=[pT])
                    pend.append((kt, pT))
                if n >= 2:
                    kt, pT = pend[n - 2]
                    kb.op("pe", lambda e, kt=kt, pT=pT, n=n: e.matmul(
                        po.t[:], lhsT=vp.t[:, kt, :], rhs=pT.t[:], start=(n == 2),
                        stop=(n == nb + 1 and tail_fn is None)), r=[vp, pT], w=[po])
            if tail_fn is not None:
                tail_fn(i, po)
            finalize(i, po)

    def norm_write(ph_t, src_ps_or_sb, srcdeps, row0, i):
        rz, on = ph_t
        kb.op("dve", lambda e: e.reciprocal(out=rz.t[64:128, :], in_=src_ps_or_sb.t[64:128, :]), r=srcdeps, w=[rz])
        o = on.next()
        kb.op("dve", lambda e: e.tensor_tensor(out=o.t[0:64, :], in0=src_ps_or_sb.t[0:64, :], in1=rz.t[64:128, :],
                                               op=ALU.mult), r=srcdeps + [rz], w=[o])
        kb.dma("sp", MT[row0:row0 + 64, i * 512:(i + 1) * 512], o.t[0:64, :], r=[o])

    def toep_tile(mk, FR, idx, W_, dmin, e_):
        src = bass.AP(tensor=FR.tensor, offset=idx * 128 * W_ + 256 * e_ - dmin, ap=[[W_ - 1, 128], [256, 4], [1, 128]])
        kb.dma("pool", mk.t[:].rearrange("p (a c) -> p a c", a=4), src, w=[mk])

    with ExitStack() as ph:
        kar = Rot([kb.T(ph, [128, S], BF16) for _ in range(2)])
        qar = Rot([kb.T(ph, [128, SO], BF16) for _ in range(2)])
        vpr = Rot([kb.T(ph, [128, NT, 128], BF16) for _ in range(2)])
        psS = Rot([PS[0], PS[1], PS[2], PS[3]])
        psO = Rot([PS[4], PS[5]])
        pTr = Rot([kb.T(ph, [128, 512], BF16) for _ in range(4)])
        rz = kb.T(ph, [128, 512], F32)
        onr = Rot([kb.T(ph, [128, 512], BF16) for _ in range(2)])
        if layer == 0:
            cm = {}
            for part in range(2):
                for e_ in FOX_E:
                    mk = kb.T(ph, [128, 512], BF16)
                    toep_tile(mk, FcR, part, fgeo[1], fgeo[0], e_)
                    cm[(part, e_)] = mk

            def fox_blocks(i):
                bl = []
                for part in range(2):
                    for b in range(0, 4 * i + 4):
                        e_ = 4 * i - b
                        ms = [] if e_ >= 1 else [(identb, cm[(part, e_)], cm[(part, e_)].t[:])]
                        bl.append((b + part * NH, ms, False))
                return bl

            for hd in range(8):
                bufs = (kar.next(), qar.next(), vpr.next(), psS, psO, pTr)
                attention_head(ph, KAF[hd], QAF[hd], VF[hd], 0, 71, fox_blocks,
                               lambda i, po, hd=hd: norm_write((rz, onr), po, [po], hd * 64, i), bufs)
            oacc = kb.T(ph, [128, SO], F32)
            tot = kb.T(ph, [128, 512], F32)
            for j in range(4):
                for g in range(3):
                    eo, et, dmin, W_ = dgeo[g]
                    hd = g * 4 + j
                    dm = {}
                    with ExitStack() as ph2:
                        for part, el in ((0, eo), (1, et)):
                            for e_ in el:
                                mk = kb.T(ph2, [128, 512], BF16)
                                toep_tile(mk, FdR[g], j * 2 + part, W_, dmin, e_)
                                dm[(part, e_)] = mk

                        def dil_blocks(i, eo=eo, et=et, dm=dm):
                            bl = []
                            for part, el in ((0, eo), (1, et)):
                                for e_ in el:
                                    b = 4 * i - e_
                                    if 0 <= b < NH:
                                        bl.append((b + part * NH, [(identb, dm[(part, e_)], dm[(part, e_)].t[:])], False))
                            return bl

                        def dil_fin(i, po, g=g, j=j):
                            sl = oacc.t[:, i * 512:(i + 1) * 512]
                            if g == 0:
                                kb.op("act", lambda e: e.activation(out=sl, in_=po.t[:], func=AF.Copy), r=[po], w=[oacc])
                            elif g == 1:
                                kb.op("dve", lambda e: e.tensor_tensor(out=sl, in0=po.t[:], in1=sl, op=ALU.add),
                                      r=[po, oacc], w=[oacc])
                            else:
                                norm_write((rz, onr), po, [po], 512 + j * 64, i)

                        def dil_tail(i, po):
                            kb.op("pe", lambda e: e.matmul(po.t[:], lhsT=identf.t[:], rhs=oacc.t[:, i * 512:(i + 1) * 512],
                                                           start=False, stop=True), r=[identf, oacc], w=[po])

                        bufs = (kar.next(), qar.next(), vpr.next(), psS, psO, pTr)
                        attention_head(ph2, KAD[hd], QAD[hd], VD[hd], 0, 65, dil_blocks, dil_fin, bufs,
                                       tail_fn=(dil_tail if g == 2 else None))
                        kb.barrier()
        if layer == 1:
            pass
        kb.barrier()
    if layer == 1:
        NIT = 26
        with ExitStack() as ph:
            pst = Tl(ph.enter_context(nc.psum_tensor(tag + "pst", [128, 1024], BF16)))
            qis = kb.T(ph, [128, 4, SO], BF16)
            kb.dma("sp", qis.t[:], QI.rearrange("(c p) t -> p c t", p=128), w=[qis])
            kis = kb.T(ph, [128, S], BF16)
            kb.dma("sp", kis.t[:], KI[:, :], w=[kis])
            sc = kb.T(ph, [128, S], F32)
            junk = kb.T(ph, [128, S], BF16)
            nmb = kb.T(ph, [128, S], BF16)
            stage = kb.T(ph, [128, NT, 512], BF16)
            rlr = Rot([kb.T(ph, [128, 512], F32) for _ in range(3)])
            wq = kb.T(ph, [128, 8], F32)
            trim = kb.T(ph, [128, 128], F32)
            othd = kb.T(ph, [128, 128], F32)
            pw2 = kb.T(ph, [128, 32], F32)
            kb.dma("sp", trim.t[:], trimD[:, :], w=[trim])
            kb.dma("sp", othd.t[:], othdD[:, :], w=[othd])
            kb.dma("sp", pw2.t[:], pow2D[:, :], w=[pw2])
            dhs = kb.T(ph, [128, 32], F32)
            cnt = kb.T(ph, [128, 32], F32)
            lo = kb.T(ph, [128, 1], F32)
            mid = kb.T(ph, [128, 1], F32)
            st4 = kb.T(ph, [128, 4], F32)
            psI = Rot([PS[0], PS[1], PS[2], PS[3]])
            for i in range(NQ):
                NCH = i + 1
                RW = 2 * NCH * 512
                for a4 in range(4):
                    aq = 4 * i + a4
                    kb.dma("sp", wq.t[:], WI[aq * 128:(aq + 1) * 128, 0:8], w=[wq])
                    for kc in range(NCH):
                        for part in range(2):
                            c0 = (2 * kc + part) * 512
                            k0 = part * SO + kc * 512
                            for hh in range(8):
                                pb = (hh % 2) * 64
                                ps = psI.next()
                                kb.op("pe", lambda e, hh=hh, pb=pb, ps=ps, k0=k0: e.matmul(
                                    ps.t[:], lhsT=qis.t[pb:pb + 64, hh // 2, aq * 128:(aq + 1) * 128],
                                    rhs=kis.t[pb:pb + 64, k0:k0 + 512], start=True, stop=True), r=[qis, kis], w=[ps])
                                rl = rlr.next()
                                kb.op("act", lambda e, rl=rl, ps=ps: e.activation(out=rl.t[:], in_=ps.t[:], func=AF.Relu),
                                      r=[ps], w=[rl])
                                if hh == 0:
                                    kb.op("dve", lambda e, rl=rl, c0=c0: e.tensor_scalar(
                                        out=sc.t[:, c0:c0 + 512], in0=rl.t[:], scalar1=wq.t[:, 0:1], scalar2=None,
                                        op0=ALU.mult), r=[rl, wq], w=[sc])
                                else:
                                    kb.op("dve", lambda e, rl=rl, c0=c0, hh=hh: e.scalar_tensor_tensor(
                                        out=sc.t[:, c0:c0 + 512], in0=rl.t[:], scalar=wq.t[:, hh:hh + 1],
                                        in1=sc.t[:, c0:c0 + 512], op0=ALU.mult, op1=ALU.add), r=[rl, wq, sc], w=[sc])
                    kb.op("dve", lambda e: e.tensor_reduce(out=st4.t[:, 0:1], in_=sc.t[:, 0:RW], axis=AX.X, op=ALU.max),
                          r=[sc], w=[st4])
                    kb.op("dve", lambda e: e.tensor_reduce(out=st4.t[:, 1:2], in_=sc.t[:, 0:RW], axis=AX.X, op=ALU.min),
                          r=[sc], w=[st4])
                    for part in range(2):
                        base = (2 * i + part) * 512
                        for j in range(4):
                            blk = sc.t[:, base + j * 128:base + (j + 1) * 128]
                            if j > a4:
                                kb.op("pool", lambda e, blk=blk: e.memset(blk, -1.0e9), w=[sc])
                            elif j == a4:
                                mt_ = trim if part == 0 else othd
                                kb.op("dve", lambda e, blk=blk, mt_=mt_: e.tensor_tensor(out=blk, in0=blk, in1=mt_.t[:],
                                                                                         op=ALU.add), r=[sc, mt_], w=[sc])
                    if aq == 0:
                        kb.op("dve", lambda e: e.memset(lo.t[:], -1.0e8), w=[lo])
                    else:
                        kb.op("dve", lambda e: e.tensor_tensor(out=st4.t[:, 2:3], in0=st4.t[:, 0:1], in1=st4.t[:, 1:2],
                                                               op=ALU.subtract), r=[st4], w=[st4])
                        kb.op("dve", lambda e: e.tensor_scalar(out=st4.t[:, 2:3], in0=st4.t[:, 2:3], scalar1=2.0, scalar2=None,
                                                               op0=ALU.add), r=[st4], w=[st4])
                        kb.op("dve", lambda e: e.tensor_scalar(out=dhs.t[:], in0=pw2.t[:], scalar1=st4.t[:, 2:3], scalar2=None,
                                                               op0=ALU.mult), r=[pw2, st4], w=[dhs])
                        kb.op("dve", lambda e: e.tensor_scalar(out=lo.t[:], in0=st4.t[:, 1:2], scalar1=-1.0, scalar2=None,
                                                               op0=ALU.add), r=[st4], w=[lo])
                        kb.op("dve", lambda e: e.memset(cnt.t[:], 0.0), w=[cnt])
                        for it in range(NIT):
                            kb.op("dve", lambda e, it=it: e.tensor_tensor(out=mid.t[:], in0=lo.t[:], in1=dhs.t[:, it:it + 1],
                                                                          op=ALU.add), r=[lo, dhs], w=[mid])
                            kb.op("dve", lambda e, it=it: e.tensor_scalar(
                                out=junk.t[:, 0:RW], in0=sc.t[:, 0:RW], scalar1=mid.t[:, 0:1], scalar2=0.0, op0=ALU.is_ge,
                                op1=ALU.add, accum_out=cnt.t[:, it:it + 1]), r=[sc, mid], w=[junk, cnt])
                            kb.op("dve", lambda e, it=it: e.scalar_tensor_tensor(
                                out=mid.t[:], in0=cnt.t[:, it:it + 1], scalar=255.5, in1=dhs.t[:, it:it + 1], op0=ALU.is_ge,
                                op1=ALU.mult), r=[cnt, dhs], w=[mid])
                            kb.op("dve", lambda e: e.tensor_tensor(out=lo.t[:], in0=lo.t[:], in1=mid.t[:], op=ALU.add),
                                  r=[lo, mid], w=[lo])
                    kb.op("dve", lambda e: e.tensor_scalar(out=nmb.t[:, 0:RW], in0=sc.t[:, 0:RW], scalar1=lo.t[:, 0:1],
                                                           scalar2=1.0, op0=ALU.is_ge, op1=ALU.subtract), r=[sc, lo], w=[nmb])
                    nblk = RW // 128
                    for b0 in range(0, nblk, 8):
                        nb_ = min(8, nblk - b0)
                        for bb in range(nb_):
                            kb.op("pe", lambda e, bb=bb, b0=b0: e.transpose(
                                out=pst.t[:, bb * 128:(bb + 1) * 128], in_=nmb.t[:, (b0 + bb) * 128:(b0 + bb + 1) * 128],
                                identity=identb.t[:]), r=[nmb, identb], w=[pst])
                        for half in range(nb_ // 4):
                            cb = (b0 // 4) + half
                            kt0 = (cb % 2) * NH + (cb // 2) * 4
                            kb.op("act", lambda e, half=half, kt0=kt0: e.activation(
                                out=stage.t[:, kt0:kt0 + 4, a4 * 128:(a4 + 1) * 128],
                                in_=pst.t[:, half * 512:(half + 1) * 512].rearrange("p (j c) -> p j c", j=4), func=AF.Copy),
                                r=[pst], w=[stage])
                for part in range(2):
                    kb.dma("sp", NM[i, part * NH:part * NH + 4 * NCH].rearrange("k p c -> p k c"),
                           stage.t[:, part * NH:part * NH + 4 * NCH, :], r=[stage])
            kb.barrier()

        with ExitStack() as ph:
            kar = Rot([kb.T(ph, [128, S], BF16) for _ in range(2)])
            qar = Rot([kb.T(ph, [128, SO], BF16) for _ in range(2)])
            vpr = Rot([kb.T(ph, [128, NT, 128], BF16) for _ in range(2)])
            psS = Rot([PS[0], PS[1], PS[2], PS[3]])
            psO = Rot([PS[4], PS[5]])
            pTr = Rot([kb.T(ph, [128, 512], BF16) for _ in range(4)])
            rz = kb.T(ph, [128, 512], F32)
            onr = Rot([kb.T(ph, [128, 512], BF16) for _ in range(2)])
            i3b = kb.T(ph, [128, 128], BF16)
            kb.op("dve", lambda e: e.tensor_scalar(out=i3b.t[:], in0=identf.t[:], scalar1=-NEG, scalar2=None, op0=ALU.mult),
                  r=[identf], w=[i3b])
            rbf_ = kb.T(ph, [128, 16], F32)
            kb.dma("sp", rbf_.t[:], rbfar[:, :], w=[rbf_])
            for q_ in range(32):
                W_ = sgeo[1]
                kb.dma("sp", FsR[q_, :, :], bass.AP(tensor=FsD.tensor, offset=q_ * W_, ap=[[0, 128], [1, W_]]))
            kb.barrier()
            nmr = Rot([kb.T(ph, [128, 4, 512], BF16) for _ in range(6)])
            bt = {}
            for part in range(2):
                for e_ in DSA_NEAR_E:
                    bt[(part, e_)] = kb.T(ph, [128, 512], BF16)
            for hd in range(16):
                for part in range(2):
                    for e_ in DSA_NEAR_E:
                        toep_tile(bt[(part, e_)], FsR, hd * 2 + part, sgeo[1], sgeo[0], e_)

                def dsa_blocks(i, hd=hd):
                    pieces = [(part, kc) for part in range(2) for kc in range(i + 1)]
                    tiles = {}

                    def load(pi):
                        if pi >= len(pieces) or pi in tiles:
                            return
                        part, kc = pieces[pi]
                        nt_ = nmr.next()
                        kb.dma("sp", nt_.t[:], NM[i, part * NH + 4 * kc:part * NH + 4 * kc + 4].rearrange("k p c -> p k c"),
                               w=[nt_])
                        tiles[pi] = nt_

                    bl = []
                    for pi, (part, kc) in enumerate(pieces):
                        for j in range(4):
                            b = 4 * kc + j
                            e_ = 4 * i - b
                            far = e_ > DSA_NEAR_E[-1]

                            def mk(pi=pi, j=j, part=part, e_=e_, far=far):
                                load(pi)
                                if j == 0:
                                    load(pi + 1)
                                    load(pi + 2)
                                nt_ = tiles[pi]
                                ms = [(i3b, nt_, nt_.t[:, j, :])]
                                if not far:
                                    ms.append((identb, bt[(part, e_)], bt[(part, e_)].t[:]))
                                return ms
                            bl.append((b + part * NH, mk, far))
                    return bl

                bufs = (kar.next(), qar.next(), vpr.next(), psS, psO, pTr)
                attention_head(ph, KAS[hd], QAS[hd], VS[hd], 0, 65, dsa_blocks,
                               lambda i, po, hd=hd: norm_write((rz, onr), po, [po], hd * 64, i), bufs,
                               expbias=(rbf_, rbf_.t[:, hd:hd + 1]))
            kb.barrier()
    if stop_after <= 2:
        return nc, es, kb

    KC = NMO // 128
    with ExitStack() as ph:
        wo = kb.T(ph, [128, KC, 1024], BF16)
        for kc in range(KC):
            kb.dma("pool", wo.t[:, kc, :], w_out[kc * 128:(kc + 1) * 128, :], w=[wo])
        rwf = kb.T(ph, [128, 8, NE], F32)
        kb.dma("sp", rwf.t[:], rw.rearrange("(k p) e -> p k e", p=128), w=[rwf])
        rbs = kb.T(ph, [128, NE], F32)
        kb.dma("sp", rbs.t[:], rbb[:, :], w=[rbs])
        xgr = Rot([kb.T(ph, [128, 8, 512], F32) for _ in range(2)])
        mtr = Rot([kb.T(ph, [128, KC, 512], BF16) for _ in range(2)])
        x1r = Rot([kb.T(ph, [128, 8, 512], F32) for _ in range(2)])
        h2f = kb.T(ph, [128, 8, 512], F32)
        h2r = Rot([kb.T(ph, [128, 8, 512], BF16) for _ in range(2)])
        sqr = Rot([kb.T(ph, [128, 512], F32) for _ in range(2)])
        tmpr = Rot([kb.T(ph, [128, 512], F32) for _ in range(2)])
        rstd = kb.T(ph, [128, 512], F32)
        rmt = (sqr, rstd, tmpr, PS[0])
        psA = Rot([PS[1], PS[2]])
        psL = Rot([PS[3], PS[4]])
        psT = PS[5]
        lg = kb.T(ph, [128, NE], F32)
        ex = kb.T(ph, [128, NE], F32)
        mk_ = kb.T(ph, [128, NE], F32)
        gg = kb.T(ph, [128, NE], F32)
        mx8 = kb.T(ph, [128, 8], F32)
        sm1 = kb.T(ph, [128, 2], F32)
        gTr = Rot([kb.T(ph, [128, 512], F32) for _ in range(2)])
        MTv = MT.rearrange("(k p) t -> p k t", p=128)
        X1v = X1.rearrange("(k p) t -> p k t", p=128)
        H2v = H2.rearrange("(k p) t -> p k t", p=128)
        for i in range(NQ):
            cs = slice(i * 512, (i + 1) * 512)
            xg = xgr.next()
            kb.dma("sp", xg.t[:], xTv[:, :, cs], w=[xg])
            mt = mtr.next()
            kb.dma("sp", mt.t[:], MTv[:, :, cs], w=[mt])
            x1 = x1r.next()
            for dc in range(8):
                ps = psA.next()
                for kc in range(KC):
                    kb.op("pe", lambda e, kc=kc, dc=dc, ps=ps: e.matmul(
                        ps.t[:], lhsT=wo.t[:, kc, dc * 128:(dc + 1) * 128], rhs=mt.t[:, kc, :], start=(kc == 0),
                        stop=(kc == KC - 1)), r=[wo, mt], w=[ps])
                kb.op("dve", lambda e, dc=dc, ps=ps: e.scalar_tensor_tensor(
                    out=x1.t[:, dc, :], in0=ps.t[:], scalar=mods.t[:, GT1 + dc:GT1 + dc + 1], in1=xg.t[:, dc, :],
                    op0=ALU.mult, op1=ALU.add), r=[ps, mods, xg], w=[x1])
            kb.dma("sp", X1v[:, :, cs], x1.t[:], r=[x1])
            h2 = h2r.next()
            rms_mod(rmt, x1, gs2, SH2, h2, hf32=h2f)
            kb.dma("sp", H2v[:, :, cs], h2.t[:], r=[h2])
            gT = gTr.next()
            for j in range(4):
                pl = psL.next()
                for k in range(8):
                    kb.op("pe", lambda e, k=k, j=j, pl=pl: e.matmul(
                        pl.t[:, 0:NE], lhsT=h2f.t[:, k, j * 128:(j + 1) * 128], rhs=rwf.t[:, k, :], start=(k == 0),
                        stop=(k == 7)), r=[h2f, rwf], w=[pl])
                kb.op("dve", lambda e, pl=pl: e.tensor_tensor(out=lg.t[:], in0=pl.t[:, 0:NE], in1=rbs.t[:], op=ALU.add),
                      r=[pl, rbs], w=[lg])
                kb.op("dve", lambda e: e.max(out=mx8.t[:], in_=lg.t[:]), r=[lg], w=[mx8])
                kb.op("dve", lambda e: e.tensor_scalar(out=sm1.t[:, 0:1], in0=mx8.t[:, 0:1], scalar1=-1.0, scalar2=None,
                                                       op0=ALU.mult), r=[mx8], w=[sm1])
                kb.op("act", lambda e: e.activation(out=ex.t[:], in_=lg.t[:], func=AF.Exp, bias=sm1.t[:, 0:1]),
                      r=[lg, sm1], w=[ex])
                kb.op("dve", lambda e: e.tensor_scalar(out=mk_.t[:], in0=lg.t[:], scalar1=mx8.t[:, 3:4], scalar2=None,
                                                       op0=ALU.is_ge), r=[lg, mx8], w=[mk_])
                kb.op("dve", lambda e: e.tensor_tensor(out=gg.t[:], in0=ex.t[:], in1=mk_.t[:], op=ALU.mult),
                      r=[ex, mk_], w=[gg])
                kb.op("dve", lambda e: e.reduce_sum(out=sm1.t[:, 1:2], in_=gg.t[:], axis=AX.X), r=[gg], w=[sm1])
                kb.op("dve", lambda e: e.reciprocal(out=sm1.t[:, 1:2], in_=sm1.t[:, 1:2]), r=[sm1], w=[sm1])
                kb.op("dve", lambda e: e.tensor_scalar(out=gg.t[:], in0=gg.t[:], scalar1=sm1.t[:, 1:2], scalar2=None,
                                                       op0=ALU.mult), r=[gg, sm1], w=[gg])
                kb.op("pe", lambda e: e.transpose(out=psT.t[0:NE, 0:128], in_=gg.t[:], identity=identf.t[:]),
                      r=[gg, identf], w=[psT])
                kb.op("act", lambda e, j=j: e.activation(out=gT.t[0:NE, j * 128:(j + 1) * 128], in_=psT.t[0:NE, 0:128],
                                                         func=AF.Copy), r=[psT], w=[gT])
            kb.dma("sp", GTs[:, cs], gT.t[0:NE, :], r=[gT])
        kb.barrier()
    if stop_after <= 3:
        return nc, es, kb

    P = min(1024, SO)
    NPG = P // 512
    outv = outD.rearrange("(k p) t -> p k t", p=128)
    with ExitStack() as ph:
        PS7 = Tl(ph.enter_context(nc.psum_tensor(tag + "ps7", [128, 512], F32)))
        b1c = kb.T(ph, [128, NE * 16], F32)
        kb.dma("sp", b1c.t[:], b1col[:, :], w=[b1c])
        selb = kb.T(ph, [128, NE * 128], BF16)
        kb.dma("pool", selb.t[0:NE, :], selD[:, :], w=[selb])
        b2f = kb.T(ph, [128, D], F32)
        kb.dma("sp", b2f.t[0:NE, :], b2[:, :], w=[b2f])
        H2p = kb.T(ph, [128, 8, P], BF16)
        acc = kb.T(ph, [128, 8, P], F32)
        gtf = kb.T(ph, [128, P], F32)
        gtr_ = kb.T(ph, [128, P], F32)
        gth = kb.T(ph, [128, P], BF16)
        gtl = kb.T(ph, [128, P], BF16)
        if layer == 1:
            fns = kb.T(ph, [128, 8], F32)
            kb.dma("sp", fns.t[:], fnc[:, :], w=[fns])
        X1v = X1.rearrange("(k p) t -> p k t", p=128)
        H2v = H2.rearrange("(k p) t -> p k t", p=128)
        for pz in range(SO // P):
            c0 = pz * P
            kb.dma("sp", H2p.t[:], H2v[:, :, c0:c0 + P], w=[H2p])
            kb.dma("sp", gtf.t[0:NE, :], GTs[:, c0:c0 + P], w=[gtf])
            kb.op("dve", lambda e: e.tensor_copy(out=gth.t[0:NE, :], in_=gtf.t[0:NE, :]), r=[gtf], w=[gth])
            kb.op("dve", lambda e: e.tensor_tensor(out=gtr_.t[0:NE, :], in0=gtf.t[0:NE, :], in1=gth.t[0:NE, :],
                                                   op=ALU.subtract), r=[gtf, gth], w=[gtr_])
            kb.op("dve", lambda e: e.tensor_copy(out=gtl.t[0:NE, :], in_=gtr_.t[0:NE, :]), r=[gtr_], w=[gtl])
            with ExitStack() as pe_:
                w1r = Rot([kb.T(pe_, [128, 8, 2048], BF16) for _ in range(2)])
                w2r = Rot([kb.T(pe_, [128, 8, 1024], BF16) for _ in range(2)])
                glr = Rot([kb.T(pe_, [128, 512], F32) for _ in range(2)])
                sgr = Rot([kb.T(pe_, [128, 512], F32) for _ in range(2)])
                lir = Rot([kb.T(pe_, [128, 512], F32) for _ in range(2)])
                t1r = Rot([kb.T(pe_, [128, 512], F32) for _ in range(2)])
                t2r = Rot([kb.T(pe_, [128, 512], F32) for _ in range(2)])
                Gsr = Rot([kb.T(pe_, [128, 512], F32) for _ in range(2)])
                abr = Rot([kb.T(pe_, [128, 512], BF16) for _ in range(10)])
                psg = Rot([PS[0], PS[1]])
                psl = Rot([PS[2], PS[3]])
                psG = PS[4]
                psy = Rot([PS[5], PS[6]])
                psB = PS7
                for gi in range(NPG):
                    cs = slice(gi * 512, (gi + 1) * 512)
                    for dc in range(8):
                        kb.op("pe", lambda e, dc=dc, cs=cs: e.matmul(
                            psB.t[:], lhsT=b2f.t[0:NE, dc * 128:(dc + 1) * 128], rhs=gtf.t[0:NE, cs], start=True,
                            stop=True), r=[b2f, gtf], w=[psB])
                        kb.op("act", lambda e, dc=dc, cs=cs: e.activation(out=acc.t[:, dc, cs], in_=psB.t[:], func=AF.Copy),
                              r=[psB], w=[acc])
                def load_w(x_):
                    a_ = w1r.next()
                    b_ = w2r.next()
                    kb.dma("pool", a_.t[:], w1[x_].rearrange("(k p) n -> p k n", p=128), w=[a_])
                    kb.dma("pool", b_.t[:], w2[x_].rearrange("(k p) n -> p k n", p=128), w=[b_])
                    return a_, b_

                nxt_w = load_w(0)
                for ex_ in range(NE):
                    w1e, w2e = nxt_w
                    if ex_ + 1 < NE:
                        nxt_w = load_w(ex_ + 1)
                    for gi in range(NPG):
                        cs = slice(gi * 512, (gi + 1) * 512)
                        kb.op("pe", lambda e, cs=cs: e.matmul(psG.t[:], lhsT=selb.t[0:NE, ex_ * 128:(ex_ + 1) * 128],
                                                              rhs=gth.t[0:NE, cs], start=True, stop=False),
                              r=[selb, gth], w=[psG])
                        kb.op("pe", lambda e, cs=cs: e.matmul(psG.t[:], lhsT=selb.t[0:NE, ex_ * 128:(ex_ + 1) * 128],
                                                              rhs=gtl.t[0:NE, cs], start=False, stop=True),
                              r=[selb, gtl], w=[psG])
                        Gs = Gsr.next()
                        kb.op("act", lambda e, Gs=Gs: e.activation(out=Gs.t[:], in_=psG.t[:], func=AF.Copy), r=[psG], w=[Gs])
                        acts = []
                        for hc in range(8):
                            pg = psg.next()
                            pl = psl.next()
                            for k in range(8):
                                kb.op("pe", lambda e, k=k, hc=hc, pg=pg, cs=cs: e.matmul(
                                    pg.t[:], lhsT=w1e.t[:, k, hc * 128:(hc + 1) * 128], rhs=H2p.t[:, k, cs],
                                    start=(k == 0), stop=(k == 7)), r=[w1e, H2p], w=[pg])
                            for k in range(8):
                                kb.op("pe", lambda e, k=k, hc=hc, pl=pl, cs=cs: e.matmul(
                                    pl.t[:], lhsT=w1e.t[:, k, 1024 + hc * 128:1024 + (hc + 1) * 128], rhs=H2p.t[:, k, cs],
                                    start=(k == 0), stop=(k == 7)), r=[w1e, H2p], w=[pl])
                            gl = glr.next()
                            sg_ = sgr.next()
                            li = lir.next()
                            t1 = t1r.next()
                            t2 = t2r.next()
                            ab_ = abr.next()
                            bc = ex_ * 16 + hc
                            kb.op("dve", lambda e, gl=gl, pg=pg, bc=bc: e.tensor_scalar(
                                out=gl.t[:], in0=pg.t[:], scalar1=b1c.t[:, bc:bc + 1], scalar2=7.0, op0=ALU.add,
                                op1=ALU.min), r=[pg, b1c], w=[gl])
                            kb.op("act", lambda e, gl=gl, sg_=sg_: e.activation(out=sg_.t[:], in_=gl.t[:], func=AF.Sigmoid,
                                                                                scale=1.702), r=[gl], w=[sg_])
                            kb.op("dve", lambda e, li=li, pl=pl, bc=bc: e.tensor_scalar(
                                out=li.t[:], in0=pl.t[:], scalar1=b1c.t[:, bc + 8:bc + 9], scalar2=7.0, op0=ALU.add,
                                op1=ALU.min), r=[pl, b1c], w=[li])
                            kb.op("pool", lambda e, li=li: e.tensor_scalar(out=li.t[:], in0=li.t[:], scalar1=-7.0, scalar2=1.0,
                                                                           op0=ALU.max, op1=ALU.add), r=[li], w=[li])
                            kb.op("pool", lambda e, t1=t1, gl=gl, sg_=sg_: e.tensor_tensor(out=t1.t[:], in0=gl.t[:], in1=sg_.t[:],
                                                                                           op=ALU.mult), r=[gl, sg_], w=[t1])
                            kb.op("pool", lambda e, t2=t2, li=li, Gs=Gs: e.tensor_tensor(out=t2.t[:], in0=li.t[:], in1=Gs.t[:],
                                                                                         op=ALU.mult), r=[li, Gs], w=[t2])
                            kb.op("dve", lambda e, ab_=ab_, t1=t1, t2=t2: e.tensor_tensor(out=ab_.t[:], in0=t1.t[:], in1=t2.t[:],
                                                                                          op=ALU.mult), r=[t1, t2], w=[ab_])
                            acts.append(ab_)
                        for dc in range(8):
                            py = psy.next()
                            for hc in range(8):
                                kb.op("pe", lambda e, hc=hc, dc=dc, py=py: e.matmul(
                                    py.t[:], lhsT=w2e.t[:, hc, dc * 128:(dc + 1) * 128], rhs=acts[hc].t[:],
                                    start=(hc == 0), stop=(hc == 7)), r=[w2e, acts[hc]], w=[py])
                            kb.op("dve", lambda e, dc=dc, py=py, cs=cs: e.tensor_tensor(
                                out=acc.t[:, dc, cs], in0=py.t[:], in1=acc.t[:, dc, cs], op=ALU.add), r=[py, acc], w=[acc])
                kb.barrier()
            with ExitStack() as pf:
                x1r = Rot([kb.T(pf, [128, 8, 512], F32) for _ in range(2)])
                x2r = Rot([kb.T(pf, [128, 8, 512], F32) for _ in range(2)])
                sqr = Rot([kb.T(pf, [128, 512], F32) for _ in range(2)])
                rstd = kb.T(pf, [128, 512], F32)
                for gi in range(NPG):
                    cs = slice(gi * 512, (gi + 1) * 512)
                    gs_ = slice(c0 + gi * 512, c0 + (gi + 1) * 512)
                    x1 = x1r.next()
                    kb.dma("sp", x1.t[:], X1v[:, :, gs_], w=[x1])
                    x2 = x2r.next()
                    for dc in range(8):
                        kb.op("dve", lambda e, dc=dc, cs=cs: e.scalar_tensor_tensor(
                            out=x2.t[:, dc, :], in0=acc.t[:, dc, cs], scalar=mods.t[:, GT2 + dc:GT2 + dc + 1],
                            in1=x1.t[:, dc, :], op0=ALU.mult, op1=ALU.add), r=[acc, mods, x1], w=[x2])
                    if layer == 1:
                        ps = PS7
                        for k in range(8):
                            sq = sqr.next()
                            kb.op("act", lambda e, sq=sq, k=k: e.activation(out=sq.t[:], in_=x2.t[:, k, :], func=AF.Square),
                                  r=[x2], w=[sq])
                            kb.op("pe", lambda e, sq=sq, k=k: e.matmul(ps.t[:], lhsT=onesf.t[:], rhs=sq.t[:], start=(k == 0),
                                                                      stop=(k == 7)), r=[sq, onesf], w=[ps])
                        kb.op("dve", lambda e: e.tensor_scalar(out=rstd.t[:], in0=ps.t[:], scalar1=1.0 / D, scalar2=EPS,
                                                               op0=ALU.mult, op1=ALU.add), r=[ps], w=[rstd])
                        kb.op("act", lambda e: e.activation(out=rstd.t[:], in_=rstd.t[:], func=AF.Sqrt), r=[rstd], w=[rstd])
                        kb.op("dve", lambda e: e.reciprocal(out=rstd.t[:], in_=rstd.t[:]), r=[rstd], w=[rstd])
                        for k in range(8):
                            kb.op("dve", lambda e, k=k: e.scalar_tensor_tensor(
                                out=x2.t[:, k, :], in0=x2.t[:, k, :], scalar=fns.t[:, k:k + 1], in1=rstd.t[:],
                                op0=ALU.mult, op1=ALU.mult), r=[x2, fns, rstd], w=[x2])
                    kb.dma("sp", outv[:, :, gs_], x2.t[:], r=[x2])
                kb.barrier()
    kb.barrier()
    return nc, es, kb


def col8(v):
    return np.ascontiguousarray(np.asarray(v, np.float32).reshape(-1, 128).T)


def toep_vec(fn, dmin, W, shift):
    d = np.arange(W) + dmin + shift
    return fn(d).astype(np.float32)


def prep_common(p, pre, xb, cb, hf, S):
    perm = sigma_perm(S, hf)
    m = {}
    if xb is not None:
        m["xT"] = np.ascontiguousarray(xb[perm].T)
    m["ccol"] = col8(cb)
    m["ada_w"] = p[pre + "ada_w"]
    m["adab"] = col8(p[pre + "ada_b"])
    m["n1col"] = col8(p[pre + "norm1"])
    m["n2col"] = col8(p[pre + "norm2"])
    m["w_in"] = p[pre + "w_in"]
    m["w_out"] = p[pre + "w_out"]
    m["rw"] = p[pre + "router_w"]
    m["rbb"] = np.ascontiguousarray(np.tile(p[pre + "router_b"][None, :], (128, 1)))
    m["w1"] = p[pre + "w1"]
    m["b1col"] = np.ascontiguousarray(p[pre + "b1"].reshape(NE, 16, 128).transpose(2, 0, 1).reshape(128, NE * 16))
    m["w2"] = p[pre + "w2"]
    m["b2"] = p[pre + "b2"]
    m["ident"] = np.eye(128, dtype=np.float32)
    sel = np.zeros((NE, NE * 128), np.float32)
    for e in range(NE):
        sel[e, e * 128:(e + 1) * 128] = 1.0
    m["sel"] = sel
    m["rbflat"] = np.ascontiguousarray(np.tile(p["rel_bias"].reshape(1, 512), (128, 1)))
    return m


def prep_l0(p, xb, cb, hf, S):
    m = prep_common(p, "l0_", xb, cb, hf, S)
    rb = p["rel_bias"]
    m["fbb"] = np.ascontiguousarray(np.tile(p["l0_fox_fb"][None, :], (128, 1)))
    m["R"] = fox_R(hf)
    sh = 128 * (2 * hf - 1)
    dmin, W = toep_geom(-3, 0)
    causal = lambda d: np.where(d >= 0, 0.0, NEG)
    m["Fc"] = np.stack([toep_vec(causal, dmin, W, 0), toep_vec(causal, dmin, W, sh)])
    for g, (win, r) in enumerate(DIL_PAIRS):
        eo, et = dil_evals(win)
        dmin, W = toep_geom(min(eo + et), max(eo + et))
        rows = []
        for j in range(4):
            tab = rb[:, g * 4 + j]

            def fn(d, tab=tab, win=win, r=r):
                ok = (d >= 0) & (d <= win) & (d % r == 0)
                return np.where(ok, tab[t5_bucket_np(np.clip(d, 0, None))], NEG)
            rows.append(toep_vec(fn, dmin, W, 0))
            rows.append(toep_vec(fn, dmin, W, sh))
        m[f"Fd{g}"] = np.stack(rows)
    return m


def prep_l1(p, xb, cb, hf, S):
    m = prep_common(p, "l1_", xb, cb, hf, S)
    rb = p["rel_bias"]
    m["kvn"] = col8(p["l1_kv_norm"])
    m["w_ukv"] = p["l1_w_ukv"]
    m["fncol"] = col8(p["final_norm"])
    tri = np.where(np.arange(128)[None, :] <= np.arange(128)[:, None], 0.0, -1.0e9).astype(np.float32)
    m["trim"] = tri
    m["othd"] = np.full((128, 128), 0.0 if hf == 1 else -1.0e9, np.float32)
    m["pow2"] = np.ascontiguousarray(np.tile((2.0 ** -(np.arange(32) + 1.0))[None, :], (128, 1)).astype(np.float32))
    sh = 128 * (2 * hf - 1)
    dmin, W = toep_geom(-3, DSA_NEAR_E[-1])
    rows = []
    for h in range(16):
        tab = rb[:, h]
        fn = lambda d, tab=tab: np.where(d >= 0, tab[t5_bucket_np(np.clip(d, 0, None))], 0.0)
        rows.append(toep_vec(fn, dmin, W, 0))
        rows.append(toep_vec(fn, dmin, W, sh))
    m["Fs"] = np.stack(rows)
    m["rbfar"] = np.ascontiguousarray(np.tile(rb[31:32, :], (128, 1)))
    return m


_PROG = {}
PV0 = ("xT", "R", "Fc", "Fd0", "Fd1", "Fd2")


def build_fused(S):
    SO = S // 2
    nc = bass.Bass("TRN2", target_bir_lowering=False)
    es = ExitStack()
    kb = KB(nc, es)
    PS = [Tl(es.enter_context(nc.psum_tensor(f"ps{i}", [128, 512], F32))) for i in range(7)]
    ctx = (nc, es, kb, PS)
    shared = {}
    X1F = nc.dram_tensor("X1F", [D, S], F32, kind="Internal").ap()
    build_layer(0, S, ctx=ctx, tag="a_", shared=shared, pervar=PV0, out_ap=X1F[:, 0:SO])
    build_layer(0, S, ctx=ctx, tag="b_", shared=shared, pervar=PV0, out_ap=X1F[:, SO:S])
    build_layer(1, S, ctx=ctx, tag="c_", shared=shared, xT_in=X1F)
    es.close()
    return nc


def _get_prog(S):
    if S not in _PROG:
        _PROG[S] = build_fused(S)
    return _PROG[S]


def kernel(**inputs):
    p = {k: np.ascontiguousarray(np.asarray(v, dtype=np.float32)) for k, v in inputs.items()}
    x = p["x"]
    B, S = x.shape[0], x.shape[1]
    SO = S // 2
    n = 2 * B
    nc = _get_prog(S)
    maps = []
    for c in range(n):
        b, hf = c // 2, c % 2
        ma = prep_l0(p, x[b], p["c"][b], hf, S)
        mb = prep_l0(p, x[b], p["c"][b], 1 - hf, S)
        m1 = prep_l1(p, None, p["c"][b], hf, S)
        m = {}
        for k, v in ma.items():
            m[("a_" + k) if k in PV0 else ("w0_" + k)] = v
        for k in PV0:
            m["b_" + k] = mb[k]
        for k, v in m1.items():
            m["w1_" + k] = v
        maps.append(m)
    res = run_bass_kernel_spmd(nc, maps, core_ids=list(range(n)))
    out = np.empty_like(x)
    for c in range(n):
        own = sigma_perm(S, c % 2)[:SO]
        out[c // 2][own] = np.asarray(res.results[c]["c_out"], np.float32).T
    return out
```

```python
import math
from contextlib import ExitStack
import numpy as np
import concourse.bass as bass
import concourse.mybir as mybir
from concourse.bass_utils import run_bass_kernel_spmd

F32, BF16 = mybir.dt.float32, mybir.dt.bfloat16
ALU = mybir.AluOpType
AF = mybir.ActivationFunctionType
AX = mybir.AxisListType

NDS = 12
ROT = 24000
NEG = -30000.0
D = 1024
NE = 32
EPS = 1e-6


class Dep:
    __slots__ = ("lw", "rd")

    def __init__(self):
        self.lw = None
        self.rd = {}


class Tl:
    __slots__ = ("t", "d")

    def __init__(self, t):
        self.t = t
        self.d = Dep()


class KB:
    def __init__(self, nc, es):
        self.nc = nc
        self.es = es
        self.eng = {"pe": nc.tensor, "dve": nc.vector, "act": nc.scalar, "pool": nc.gpsimd, "sp": nc.sync}
        self.sem = {}
        self.cnt = {}
        self.nsem = 0
        self.final_val = {}
        for e in self.eng:
            self.sem[e] = self._newsem("e_" + e)
            self.cnt[e] = 0
        self.waited = {e: {} for e in self.eng}
        self.dq = ("sp", "pool")
        self.dsem = {q: [self._newsem(f"d_{q}{i}") for i in range(NDS)] for q in self.dq}
        self.dval = {q: [0] * NDS for q in self.dq}
        self.dnext = {q: 0 for q in self.dq}
        self.ninst = 0
        self.nt = 0

    def _newsem(self, name):
        self.nsem += 1
        s = self.es.enter_context(self.nc.semaphore(f"{name}_{self.nsem}"))
        self.final_val[s] = 0
        return s

    def _waitv(self, e, s, v):
        if v <= 0 or self.waited[e].get(s, 0) >= v:
            return
        self.eng[e].wait_ge(s, v)
        self.waited[e][s] = v

    def _deps(self, e, r, w):
        need = {}
        for d in r:
            if d.lw is not None:
                s, v = d.lw
                if need.get(s, 0) < v:
                    need[s] = v
        for d in w:
            if d.lw is not None:
                s, v = d.lw
                if need.get(s, 0) < v:
                    need[s] = v
            for s, v in d.rd.items():
                if need.get(s, 0) < v:
                    need[s] = v
        for s, v in need.items():
            if e == "pe" and s is self.sem["pe"]:
                continue
            self._waitv(e, s, v)

    @staticmethod
    def _dl(x):
        return [a.d if isinstance(a, Tl) else a for a in x]

    def op(self, e, fn, r=(), w=()):
        r = self._dl(r)
        w = self._dl(w)
        if self.cnt[e] >= ROT:
            self.sem[e] = self._newsem("e_" + e)
            self.cnt[e] = 0
        self._deps(e, r, w)
        ins = fn(self.eng[e])
        self.cnt[e] += 1
        s = self.sem[e]
        ins.then_inc(s, 1)
        self.final_val[s] = self.cnt[e]
        self.ninst += 1
        for d in r:
            d.rd[s] = self.cnt[e]
        for d in w:
            d.lw = (s, self.cnt[e])
            d.rd = {}

    def dma(self, q, out, in_, r=(), w=()):
        r = self._dl(r)
        w = self._dl(w)
        i = self.dnext[q]
        self.dnext[q] = (i + 1) % NDS
        s = self.dsem[q][i]
        self._waitv(q, s, self.dval[q][i])
        self._deps(q, r, w)
        ins = self.eng[q].dma_start(out=out, in_=in_)
        self.dval[q][i] += 16
        v = self.dval[q][i]
        ins.then_inc(s, 16)
        self.final_val[s] = v
        self.ninst += 1
        for d in r:
            d.rd[s] = v
        for d in w:
            d.lw = (s, v)
            d.rd = {}

    def barrier(self):
        for e in self.eng:
            for s, v in self.final_val.items():
                self._waitv(e, s, v)

    def T(self, es, shape, dt, name=None):
        self.nt += 1
        t = es.enter_context(self.nc.sbuf_tensor(f"{name or 't'}_{self.nt}", list(shape), dt))
        return Tl(t)


class Rot:
    def __init__(self, items):
        self.items = items
        self.i = 0

    def next(self):
        x = self.items[self.i % len(self.items)]
        self.i += 1
        return x


def t5_bucket_np(dist):
    nb, md = 32, 2048
    me = nb // 2
    d = np.maximum(dist, 0)
    df = np.maximum(d, 1).astype(np.float32)
    large = me + (np.log(df / me) / math.log(md / me) * (nb - me)).astype(np.int32)
    large = np.minimum(large, nb - 1)
    return np.where(d < me, d, large)


DIL_PAIRS = ((128, 1), (512, 4), (2048, 16))


def toep_geom(emin, emax):
    dmin = 256 * emin - 127 - 128
    dmax = 256 * (emax + 3) + 127 + 128
    return dmin, dmax - dmin + 1


def dil_evals(window, part_shift_opts=(-128, 128)):
    own, oth = [], []
    for e in range(-3, 16):
        ok_own = ok_oth = False
        for a in range(4):
            lo = 256 * (a + e) - 127
            hi = 256 * (a + e) + 127
            if hi >= 0 and lo <= window:
                ok_own = True
            for sh in part_shift_opts:
                if hi + sh >= 0 and lo + sh <= window:
                    ok_oth = True
        if ok_own:
            own.append(e)
        if ok_oth:
            oth.append(e)
    return own, oth


FOX_E = [-3, -2, -1, 0]
DSA_NEAR_E = list(range(-3, 10))


def true_tile(st, hf, NH):
    return 2 * st + hf if st < NH else 2 * (st - NH) + 1 - hf


def sigma_perm(S, hf):
    NT = S // 128
    NH = NT // 2
    idx = []
    for st in range(NT):
        T = true_tile(st, hf, NH)
        idx.extend(range(T * 128, T * 128 + 128))
    return np.array(idx)


def fox_R(hf):
    tpos = np.zeros(1024, np.int64)
    for tt in range(8):
        T = (2 * tt + hf) if tt < 4 else (2 * (tt - 4) + 1 - hf)
        tpos[tt * 128:(tt + 1) * 128] = T * 128 + np.arange(128)
    R = (tpos[:, None] <= tpos[None, :]).astype(np.float32)
    return R.reshape(8, 128, 1024)


def build_layer(layer, S, dbg=False, stop_after=99, cut='', ctx=None, tag='', shared=None, xT_in=None, out_ap=None,
                pervar=()):
    NT = S // 128
    NH = NT // 2
    NQ = NH // 4
    NG = S // 512
    SO = S // 2
    if ctx is None:
        nc = bass.Bass("TRN2", target_bir_lowering=False)
        es = ExitStack()
        kb = KB(nc, es)
        PS = [Tl(es.enter_context(nc.psum_tensor(f"ps{i}", [128, 512], F32))) for i in range(7)]
    else:
        nc, es, kb, PS = ctx
    if shared is None:
        shared = {}

    def din(name, shape):
        key = (tag + name) if (name in pervar or not tag) else ("w%d_" % layer + name)
        if key not in shared:
            shared[key] = nc.dram_tensor(key, list(shape), F32, kind="ExternalInput").ap()
        return shared[key]

    def dscr(name, shape, dt):
        return nc.dram_tensor(tag + name, list(shape), dt, kind=("ExternalOutput" if dbg else "Internal")).ap()

    WIN = 3848 if layer == 0 else 1864
    xT = xT_in if xT_in is not None else din("xT", [D, S])
    ccol = din("ccol", [128, 8])
    ada_w = din("ada_w", [D, 6 * D])
    adab = din("adab", [128, 48])
    n1col = din("n1col", [128, 8])
    n2col = din("n2col", [128, 8])
    w_in = din("w_in", [D, WIN])
    NMO = 768 if layer == 0 else 1024
    w_out = din("w_out", [NMO, D])
    rw = din("rw", [D, NE])
    rbb = din("rbb", [128, NE])
    w1 = din("w1", [NE, D, 2 * D])
    b1col = din("b1col", [128, NE * 16])
    w2 = din("w2", [NE, D, D])
    b2 = din("b2", [NE, D])
    identD = din("ident", [128, 128])
    selD = din("sel", [NE, NE * 128])
    rbflat = din("rbflat", [128, 512])
    if layer == 0:
        fbb = din("fbb", [128, 8])
        RD = din("R", [8, 128, 1024])
        dgeo = []
        for (w_, r_) in DIL_PAIRS:
            eo, et = dil_evals(w_)
            dgeo.append((eo, et) + toep_geom(min(eo + et), max(eo + et)))
        fgeo = toep_geom(-3, 0)
        FcD = din("Fc", [2, fgeo[1]])
        FdD = [din(f"Fd{g}", [8, dgeo[g][3]]) for g in range(3)]
        FcR = dscr("FcR", [2, 128, fgeo[1]], F32)
        FdR = [dscr(f"FdR{g}", [8, 128, dgeo[g][3]], F32) for g in range(3)]
    else:
        kvn = din("kvn", [128, 2])
        w_ukv = din("w_ukv", [256, 2048])
        fnc = din("fncol", [128, 8])
        trimD = din("trim", [128, 128])
        othdD = din("othd", [128, 128])
        pow2D = din("pow2", [128, 32])
        sgeo = toep_geom(-3, DSA_NEAR_E[-1])
        FsD = din("Fs", [32, sgeo[1]])
        FsR = dscr("FsR", [32, 128, sgeo[1]], F32)
        rbfar = din("rbfar", [128, 16])
    outD = out_ap if out_ap is not None else nc.dram_tensor(tag + "out", [D, SO], F32, kind="ExternalOutput").ap()

    MT = dscr("MT", [NMO, SO], BF16)
    X1 = dscr("X1", [D, SO], F32)
    H2 = dscr("H2", [D, SO], BF16)
    GTs = dscr("GTs", [NE, SO], F32)
    if layer == 0:
        KAF = dscr("KAF", [8, 72, S], BF16)
        QAF = dscr("QAF", [8, 72, SO], BF16)
        VF = dscr("VF", [8, 128, NT, 128], BF16)
        KAD = dscr("KAD", [12, 72, S], BF16)
        QAD = dscr("QAD", [12, 72, SO], BF16)
        VD = dscr("VD", [12, 128, NT, 128], BF16)
    else:
        KAS = dscr("KAS", [16, 72, S], BF16)
        QAS = dscr("QAS", [16, 72, SO], BF16)
        VS = dscr("VS", [16, 128, NT, 128], BF16)
        KI = dscr("KI", [128, S], BF16)
        QI = dscr("QI", [512, SO], BF16)
        WI = dscr("WI", [SO, 128], F32)
        NM = dscr("NM", [NQ, NT, 128, 512], BF16)


    kb.PS = PS
    kb.shared = shared
    if not hasattr(kb, "P0t"):
        P0 = ExitStack()
        es.enter_context(P0)
        identf = kb.T(P0, [128, 128], F32)
        identb = kb.T(P0, [128, 128], BF16)
        onesf = kb.T(P0, [128, 128], F32)
        onesb = kb.T(P0, [128, 512], BF16)
        ind2 = kb.T(P0, [128, 2], BF16)
        mods = kb.T(P0, [128, 48], F32)
        gs1 = kb.T(P0, [128, 8], F32)
        gs2 = kb.T(P0, [128, 8], F32)
        bmax = kb.T(P0, [128, 1], F32)
        zcol = kb.T(P0, [128, 1], F32)
        kb.op("dve", lambda e: e.memset(zcol.t[:], 0.0), w=[zcol])
        kb.dma("sp", identf.t[:], identD[:, :], w=[identf])
        kb.op("dve", lambda e: e.tensor_copy(out=identb.t[:], in_=identf.t[:]), r=[identf], w=[identb])
        kb.op("dve", lambda e: e.memset(onesf.t[:], 1.0), w=[onesf])
        kb.op("dve", lambda e: e.memset(onesb.t[:], 1.0), w=[onesb])
        kb.op("dve", lambda e: e.memset(ind2.t[:], 0.0), w=[ind2])
        kb.op("dve", lambda e: e.memset(ind2.t[0:64, 0:1], 1.0), w=[ind2])
        kb.op("dve", lambda e: e.memset(ind2.t[64:128, 1:2], 1.0), w=[ind2])
        kb.P0t = (identf, identb, onesf, onesb, ind2, mods, gs1, gs2, bmax, zcol)
    identf, identb, onesf, onesb, ind2, mods, gs1, gs2, bmax, zcol = kb.P0t

    with ExitStack() as ph:
        cc = kb.T(ph, [128, 8], F32)
        sc = kb.T(ph, [128, 8], F32)
        ab = kb.T(ph, [128, 48], F32)
        n1 = kb.T(ph, [128, 8], F32)
        n2 = kb.T(ph, [128, 8], F32)
        rbf = kb.T(ph, [128, 512], F32)
        kb.dma("sp", cc.t[:], ccol[:, :], w=[cc])
        kb.dma("sp", ab.t[:], adab[:, :], w=[ab])
        kb.dma("sp", n1.t[:], n1col[:, :], w=[n1])
        kb.dma("sp", n2.t[:], n2col[:, :], w=[n2])
        kb.dma("sp", rbf.t[:], rbflat[:, :], w=[rbf])
        kb.op("dve", lambda e: e.reduce_max(out=bmax.t[:], in_=rbf.t[:], axis=AX.X), r=[rbf], w=[bmax])
        kb.op("act", lambda e: e.activation(out=sc.t[:], in_=cc.t[:], func=AF.Silu), r=[cc], w=[sc])
        aw = Rot([kb.T(ph, [128, 8, 1024], F32) for _ in range(2)])
        awv = ada_w.rearrange("(k p) f -> p k f", p=128)
        psm = PS[0]
        for j in range(6):
            a = aw.next()
            kb.dma("sp", a.t[:], awv[:, :, j * 1024:(j + 1) * 1024], w=[a])
            for fc in range(8):
                for k in range(8):
                    kb.op("pe", lambda e, a=a, fc=fc, k=k, j=j: e.matmul(
                        psm.t[:, j * 8 + fc:j * 8 + fc + 1], lhsT=a.t[:, k, fc * 128:(fc + 1) * 128],
                        rhs=sc.t[:, k:k + 1], start=(k == 0), stop=(k == 7)), r=[a, sc], w=[psm])
        kb.op("dve", lambda e: e.tensor_tensor(out=mods.t[:], in0=psm.t[:, 0:48], in1=ab.t[:], op=ALU.add),
              r=[psm, ab], w=[mods])
        kb.op("dve", lambda e: e.scalar_tensor_tensor(out=gs1.t[:], in0=mods.t[:, 8:16], scalar=1.0, in1=n1.t[:],
                                                     op0=ALU.add, op1=ALU.mult), r=[mods, n1], w=[gs1])
        kb.op("dve", lambda e: e.scalar_tensor_tensor(out=gs2.t[:], in0=mods.t[:, 32:40], scalar=1.0, in1=n2.t[:],
                                                     op0=ALU.add, op1=ALU.mult), r=[mods, n2], w=[gs2])
        kb.barrier()
    SH1, GT1, SH2, GT2 = 0, 16, 24, 40
    if stop_after <= 0:
        return nc, es, kb

    def rms_mod(ph_tiles, xin, gs, sh_off, hout, hf32=None):
        sqr, rstd, tmpr, pss = ph_tiles
        ps = pss
        for k in range(8):
            sq = sqr.next()
            kb.op("act", lambda e, sq=sq, k=k: e.activation(out=sq.t[:], in_=xin.t[:, k, :], func=AF.Square),
                  r=[xin], w=[sq])
            kb.op("pe", lambda e, sq=sq, k=k: e.matmul(ps.t[:], lhsT=onesf.t[:], rhs=sq.t[:], start=(k == 0),
                                                      stop=(k == 7)), r=[sq, onesf], w=[ps])
        kb.op("dve", lambda e: e.tensor_scalar(out=rstd.t[:], in0=ps.t[:], scalar1=1.0 / D, scalar2=EPS,
                                               op0=ALU.mult, op1=ALU.add), r=[ps], w=[rstd])
        kb.op("act", lambda e: e.activation(out=rstd.t[:], in_=rstd.t[:], func=AF.Sqrt), r=[rstd], w=[rstd])
        kb.op("dve", lambda e: e.reciprocal(out=rstd.t[:], in_=rstd.t[:]), r=[rstd], w=[rstd])
        for k in range(8):
            tm = tmpr.next()
            kb.op("dve", lambda e, tm=tm, k=k: e.tensor_tensor(out=tm.t[:], in0=xin.t[:, k, :], in1=rstd.t[:],
                                                               op=ALU.mult), r=[xin, rstd], w=[tm])
            if hf32 is not None:
                kb.op("act", lambda e, tm=tm, k=k: e.activation(
                    out=hf32.t[:, k, :], in_=tm.t[:], func=AF.Identity, scale=gs.t[:, k:k + 1],
                    bias=mods.t[:, sh_off + k:sh_off + k + 1]), r=[tm, gs, mods], w=[hf32])
                kb.op("pool", lambda e, k=k: e.tensor_copy(out=hout.t[:, k, :], in_=hf32.t[:, k, :]),
                      r=[hf32], w=[hout])
            else:
                kb.op("act", lambda e, tm=tm, k=k: e.activation(
                    out=hout.t[:, k, :], in_=tm.t[:], func=AF.Identity, scale=gs.t[:, k:k + 1],
                    bias=mods.t[:, sh_off + k:sh_off + k + 1]), r=[tm, gs, mods], w=[hout])

    xTv = xT.rearrange("(k p) t -> p k t", p=128)

    def projT(h, w, c0, ps, ncols=128):
        for k in range(8):
            kb.op("pe", lambda e, k=k: e.matmul(ps.t[0:ncols, :], lhsT=w.t[:, k, c0:c0 + ncols], rhs=h.t[:, k, :],
                                               start=(k == 0), stop=(k == 7)), r=[h, w], w=[ps])

    def projTok(h, j, w, c0, n, ps):
        for k in range(8):
            kb.op("pe", lambda e, k=k: e.matmul(ps.t[:, 0:n], lhsT=h.t[:, k, j * 128:(j + 1) * 128],
                                               rhs=w.t[:, k, c0:c0 + n], start=(k == 0), stop=(k == 7)),
                  r=[h, w], w=[ps])

    with ExitStack() as ph:
        wsb = kb.T(ph, [128, 8, WIN], BF16)
        wv = w_in.rearrange("(k p) n -> p k n", p=128)
        for k in range(8):
            kb.dma("pool", wsb.t[:, k, :], wv[:, k, :], w=[wsb])
        xgr = Rot([kb.T(ph, [128, 8, 512], F32) for _ in range(2)])
        hr = Rot([kb.T(ph, [128, 8, 512], BF16) for _ in range(2)])
        sqr = Rot([kb.T(ph, [128, 512], F32) for _ in range(2)])
        tmpr = Rot([kb.T(ph, [128, 512], F32) for _ in range(2)])
        rstd = kb.T(ph, [128, 512], F32)
        rmt = (sqr, rstd, tmpr, PS[0])
        stg = Rot([kb.T(ph, [128, 512], BF16) for _ in range(3)])
        sqb = Rot([kb.T(ph, [128, 512], BF16) for _ in range(2)])
        NVH = 20 if layer == 0 else 16
        psA = Rot([PS[1], PS[2]])
        psN = PS[3]
        psV = Rot([PS[4], PS[5]])
        psC = PS[6]
        NKN = 16
        kn = kb.T(ph, [128, NKN], F32)
        kms = kb.T(ph, [128, NKN], F32)
        tm2 = kb.T(ph, [128, 1], F32)
        kb.op("dve", lambda e: e.memset(kn.t[:], 0.0), w=[kn])

        def head_pair_K(h, c0, KA, hd0, col0, knc, scale=None):
            ps = psA.next()
            projT(h, wsb, c0, ps)
            s = stg.next()
            kb.op("act", lambda e: e.activation(out=s.t[:], in_=ps.t[:], func=AF.Copy,
                                                scale=(1.0 if scale is None else scale)), r=[ps], w=[s])
            q = sqb.next()
            kb.op("act", lambda e: e.activation(out=q.t[:], in_=ps.t[:], func=AF.Square), r=[ps], w=[q])
            kb.dma("sp", KA[hd0, 0:64, col0:col0 + 512], s.t[0:64, :], r=[s])
            kb.dma("sp", KA[hd0 + 1, 0:64, col0:col0 + 512], s.t[64:128, :], r=[s])
            kb.op("pe", lambda e: e.matmul(psN.t[0:2, :], lhsT=ind2.t[:, 0:2], rhs=q.t[:], start=True, stop=True),
                  r=[q, ind2], w=[psN])
            return ps

        def kn_update(knc):
            kb.op("dve", lambda e: e.reduce_max(out=tm2.t[0:2, :], in_=psN.t[0:2, :], axis=AX.X), r=[psN], w=[tm2])
            kb.op("dve", lambda e: e.tensor_max(out=kn.t[0:2, knc:knc + 1], in0=kn.t[0:2, knc:knc + 1],
                                                in1=tm2.t[0:2, :]), r=[kn, tm2], w=[kn])

        def tokV(h, c0, n, sv, h0, j):
            for (o, m) in ([(0, min(512, n))] + ([(512, n - 512)] if n > 512 else [])):
                ps = psV.next()
                projTok(h, j, wsb, c0 + o, m, ps)
                nh = m // 64
                hh = h0 + o // 64
                kb.op("dve", lambda e, ps=ps, m=m, nh=nh, hh=hh: e.tensor_copy(
                    out=sv.t[:, hh:hh + nh, j, 0:64], in_=ps.t[:, 0:m].rearrange("p (h c) -> p h c", h=nh)),
                    r=[ps], w=[sv])

        def flushV(sv, VDst, h0, nh, sg):
            for hh in range(nh):
                kb.dma("sp", VDst[hh, :, sg * 4:(sg + 1) * 4, :], sv.t[:, h0 + hh, :, :], r=[sv])

        if layer == 0:
            Lall = kb.T(ph, [128, NT, 8], F32)
            fb = kb.T(ph, [128, 8], F32)
            kb.dma("sp", fb.t[:], fbb[:, :], w=[fb])
            zt = kb.T(ph, [128, 8], F32)
            for p_ in range(2):
                kb.dma("sp", FcR[p_, :, :], bass.AP(tensor=FcD.tensor, offset=p_ * fgeo[1], ap=[[0, 128], [1, fgeo[1]]]))
            for g in range(3):
                W_ = dgeo[g][3]
                for q_ in range(8):
                    kb.dma("sp", FdR[g][q_, :, :], bass.AP(tensor=FdD[g].tensor, offset=q_ * W_, ap=[[0, 128], [1, W_]]))
            p1 = ExitStack()
            stv = Rot([kb.T(p1, [128, NVH, 4, 128], BF16) for _ in range(1)])
            for v_ in stv.items:
                kb.op("pool", lambda e, v_=v_: e.memset(v_.t[:, :, :, 64:128], 1.0), w=[v_])
            for sg in range(NG):
                xg = xgr.next()
                kb.dma("sp", xg.t[:], xTv[:, :, sg * 512:(sg + 1) * 512], w=[xg])
                h = hr.next()
                rms_mod(rmt, xg, gs1, SH1, h)
                for c4 in range(4):
                    head_pair_K(h, 512 + 128 * c4, KAF, 2 * c4, sg * 512, c4)
                    kn_update(c4)
                for c6 in range(6):
                    head_pair_K(h, 2312 + 128 * c6, KAD, 2 * c6, sg * 512, 4 + c6)
                    kn_update(4 + c6)
                sv = stv.next()
                for j in range(4):
                    tokV(h, 1024, 512, sv, 0, j)
                    tokV(h, 3080, 768, sv, 8, j)
                flushV(sv, VF, 0, 8, sg)
                flushV(sv, VD, 8, 12, sg)
                for j in range(4):
                    projTok(h, j, wsb, 1536, 8, psC)
                    kb.op("dve", lambda e: e.tensor_tensor(out=zt.t[:], in0=psC.t[:, 0:8], in1=fb.t[:], op=ALU.add),
                          r=[psC, fb], w=[zt])
                    kb.op("act", lambda e: e.activation(out=zt.t[:], in_=zt.t[:], func=AF.Exp, scale=-1.0), r=[zt], w=[zt])
                    kb.op("act", lambda e, j=j, sg=sg: e.activation(out=Lall.t[:, sg * 4 + j, :], in_=zt.t[:], func=AF.Ln,
                                                                    bias=1.0), r=[zt], w=[Lall])
                kb.dma("sp", KAF[0:8, 64, sg * 512:(sg + 1) * 512], onesb.t[0:8, :], r=[onesb])
                kb.dma("sp", KAF[0:8, 65, sg * 512:(sg + 1) * 512], onesb.t[0:8, :], r=[onesb])
                kb.dma("sp", KAF[0:8, 66, sg * 512:(sg + 1) * 512], onesb.t[0:8, :], r=[onesb])
                kb.dma("sp", KAF[0:8, 70, sg * 512:(sg + 1) * 512], onesb.t[0:8, :], r=[onesb])
                kb.dma("sp", KAD[0:12, 64, sg * 512:(sg + 1) * 512], onesb.t[0:12, :], r=[onesb])
                if sg < NQ:
                    for rr in (67, 68, 69):
                        kb.dma("sp", QAF[0:8, rr, sg * 512:(sg + 1) * 512], onesb.t[0:8, :], r=[onesb])
            kb.barrier()
            p1.close()
            p1b = ExitStack()
            Rsb = kb.T(p1b, [128, 8, 1024], F32)
            kb.dma("sp", Rsb.t[:], RD.rearrange("a p c -> p a c"), w=[Rsb])
            carry = kb.T(p1b, [128, 1], F32)
            kb.op("dve", lambda e: e.memset(carry.t[:], 0.0), w=[carry])
            Cg = kb.T(p1b, [128, 512], F32)
            r1 = kb.T(p1b, [128, 512], F32)
            cbr = Rot([kb.T(p1b, [128, 512], BF16) for _ in range(4)])
            for i in range(NQ):
                tiles = [4 * i + a for a in range(4)] + [NH + 4 * i + a for a in range(4)]
                for half in range(2):
                    for tt in range(8):
                        kb.op("pe", lambda e, tt=tt, half=half: e.matmul(
                            psC.t[0:8, :], lhsT=Lall.t[:, tiles[tt], :], rhs=Rsb.t[:, tt, half * 512:(half + 1) * 512],
                            start=(tt == 0), stop=(tt == 7)), r=[Lall, Rsb], w=[psC])
                    kb.op("dve", lambda e: e.tensor_scalar(out=Cg.t[0:8, :], in0=psC.t[0:8, :], scalar1=carry.t[0:8, 0:1],
                                                           scalar2=None, op0=ALU.add), r=[psC, carry], w=[Cg])
                    cur = Cg
                    scol = (i * 512) if half == 0 else (SO + i * 512)
                    for p3 in range(3):
                        cb = cbr.next()
                        kb.op("dve", lambda e, cur=cur, cb=cb: e.tensor_copy(out=cb.t[0:8, :], in_=cur.t[0:8, :]),
                              r=[cur], w=[cb])
                        kb.dma("sp", KAF[0:8, 67 + p3, scol:scol + 512], cb.t[0:8, :], r=[cb])
                        if half == 0:
                            nb = cbr.next()
                            kb.op("dve", lambda e, cb=cb, nb=nb: e.tensor_scalar(
                                out=nb.t[0:8, :], in0=cb.t[0:8, :], scalar1=-1.0, scalar2=None, op0=ALU.mult),
                                r=[cb], w=[nb])
                            kb.dma("sp", QAF[0:8, 64 + p3, i * 512:(i + 1) * 512], nb.t[0:8, :], r=[nb])
                        if p3 < 2:
                            kb.op("dve", lambda e, cur=cur, cb=cb: e.tensor_tensor(
                                out=r1.t[0:8, :], in0=cur.t[0:8, :], in1=cb.t[0:8, :], op=ALU.subtract),
                                r=[cur, cb], w=[r1])
                            cur = r1
                for tt in range(8):
                    kb.op("pe", lambda e, tt=tt: e.matmul(psC.t[0:8, 0:1], lhsT=Lall.t[:, tiles[tt], :],
                                                         rhs=onesf.t[:, 0:1], start=(tt == 0), stop=(tt == 7)),
                          r=[Lall, onesf], w=[psC])
                kb.op("dve", lambda e: e.tensor_tensor(out=carry.t[0:8, :], in0=carry.t[0:8, :], in1=psC.t[0:8, 0:1],
                                                       op=ALU.add), r=[carry, psC], w=[carry])
            kb.barrier()
            p1b.close()
        else:
            wu = kb.T(ph, [128, 2, 2048], BF16)
            wuv = w_ukv.rearrange("(k p) n -> p k n", p=128)
            for k in range(0 if 'U' in cut else 2):
                kb.dma("pool", wu.t[:, k, :], wuv[:, k, :], w=[wu])
            kvns = kb.T(ph, [128, 2], F32)
            if 'N' not in cut:
                kb.dma("sp", kvns.t[:], kvn[:, :], w=[kvns])
            ckf = Rot([kb.T(ph, [128, 2, 512], F32) for _ in range(2)])
            ckb = Rot([kb.T(ph, [128, 2, 512], BF16) for _ in range(2)])
            wki = kb.T(ph, [128, 8, 128], BF16)
            if 'W' not in cut:
                kb.op("dve", lambda e: e.tensor_copy(out=wki.t[:, :, 0:64], in_=wsb.t[:, :, 1792:1856]), r=[wsb], w=[wki])
                kb.op("dve", lambda e: e.tensor_copy(out=wki.t[:, :, 64:128], in_=wsb.t[:, :, 1792:1856]), r=[wsb], w=[wki])
            p1 = ExitStack()
            stv = Rot([kb.T(p1, [128, NVH, 4, 128], BF16) for _ in range(1)])
            for v_ in stv.items:
                kb.op("pool", lambda e, v_=v_: e.memset(v_.t[:, :, :, 64:128], 1.0), w=[v_])
            for sg in range(NG):
                xg = xgr.next()
                kb.dma("sp", xg.t[:], xTv[:, :, sg * 512:(sg + 1) * 512], w=[xg])
                h = hr.next()
                rms_mod(rmt, xg, gs1, SH1, h)
                cf = ckf.next()
                cb_ = ckb.next()
                pss = PS[0]
                if 'B' in cut:
                    continue
                qs_ = []
                for k2 in range(2):
                    ps = psA.next()
                    projT(h, wsb, 1024 + 128 * k2, ps)
                    kb.op("act", lambda e, ps=ps, k2=k2: e.activation(out=cf.t[:, k2, :], in_=ps.t[:], func=AF.Copy), r=[ps], w=[cf])
                    q = sqr.next()
                    kb.op("act", lambda e, q=q, ps=ps: e.activation(out=q.t[:], in_=ps.t[:], func=AF.Square), r=[ps], w=[q])
                    qs_.append(q)
                if 'P' in cut:
                    continue
                for k2 in range(2):
                    kb.op("pe", lambda e, k2=k2: e.matmul(pss.t[:], lhsT=onesf.t[:], rhs=qs_[k2].t[:], start=(k2 == 0),
                                                          stop=(k2 == 1)), r=[qs_[k2], onesf], w=[pss])
                if 'R' in cut:
                    continue
                kb.op("dve", lambda e: e.tensor_scalar(out=rstd.t[:], in0=pss.t[:], scalar1=1.0 / 256, scalar2=EPS,
                                                       op0=ALU.mult, op1=ALU.add), r=[pss], w=[rstd])
                kb.op("act", lambda e: e.activation(out=rstd.t[:], in_=rstd.t[:], func=AF.Sqrt), r=[rstd], w=[rstd])
                kb.op("dve", lambda e: e.reciprocal(out=rstd.t[:], in_=rstd.t[:]), r=[rstd], w=[rstd])
                if 'T' in cut:
                    continue
                for k2 in range(2):
                    tm = tmpr.next()
                    kb.op("dve", lambda e, tm=tm, k2=k2: e.tensor_tensor(out=tm.t[:], in0=cf.t[:, k2, :], in1=rstd.t[:],
                                                                         op=ALU.mult), r=[cf, rstd], w=[tm])
                    kb.op("act", lambda e, tm=tm, k2=k2: e.activation(out=cb_.t[:, k2, :], in_=tm.t[:], func=AF.Identity,
                                                                      scale=kvns.t[:, k2:k2 + 1], bias=zcol.t[:, 0:1]),
                          r=[tm, kvns, zcol], w=[cb_])
                for c8 in range(0 if 'C' in cut else 8):
                    ps = psA.next()
                    for k2 in range(2):
                        kb.op("pe", lambda e, k2=k2, c8=c8, ps=ps: e.matmul(
                            ps.t[:], lhsT=wu.t[:, k2, c8 * 128:(c8 + 1) * 128], rhs=cb_.t[:, k2, :],
                            start=(k2 == 0), stop=(k2 == 1)), r=[wu, cb_], w=[ps])
                    s = stg.next()
                    kb.op("act", lambda e, s=s, ps=ps: e.activation(out=s.t[:], in_=ps.t[:], func=AF.Copy), r=[ps], w=[s])
                    q = sqb.next()
                    kb.op("act", lambda e, q=q, ps=ps: e.activation(out=q.t[:], in_=ps.t[:], func=AF.Square), r=[ps], w=[q])
                    kb.dma("sp", KAS[2 * c8, 0:64, sg * 512:(sg + 1) * 512], s.t[0:64, :], r=[s])
                    kb.dma("sp", KAS[2 * c8 + 1, 0:64, sg * 512:(sg + 1) * 512], s.t[64:128, :], r=[s])
                    kb.op("pe", lambda e, q=q: e.matmul(psN.t[0:2, :], lhsT=ind2.t[:, 0:2], rhs=q.t[:], start=True,
                                                       stop=True), r=[q, ind2], w=[psN])
                    kn_update(c8)
                sv = stv.next()
                for j in range(0 if 'D' in cut else 4):
                    for o in (0, 512):
                        ps = psV.next()
                        for k2 in range(2):
                            kb.op("pe", lambda e, k2=k2, o=o, ps=ps, j=j: e.matmul(
                                ps.t[:], lhsT=cb_.t[:, k2, j * 128:(j + 1) * 128], rhs=wu.t[:, k2, 1024 + o:1536 + o],
                                start=(k2 == 0), stop=(k2 == 1)), r=[wu, cb_], w=[ps])
                        kb.op("dve", lambda e, o=o, ps=ps, j=j: e.tensor_copy(
                            out=sv.t[:, o // 64:o // 64 + 8, j, 0:64], in_=ps.t[:, :].rearrange("p (h c) -> p h c", h=8)),
                            r=[ps], w=[sv])
                if 'D' not in cut:
                    flushV(sv, VS, 0, 16, sg)
                if 'E' in cut:
                    continue
                ps = psA.next()
                projT(h, wki, 0, ps)
                s = stg.next()
                kb.op("act", lambda e, s=s, ps=ps: e.activation(out=s.t[:], in_=ps.t[:], func=AF.Copy), r=[ps], w=[s])
                kb.dma("sp", KI[:, sg * 512:(sg + 1) * 512], s.t[:, :], r=[s])
                kb.dma("sp", KAS[0:16, 64, sg * 512:(sg + 1) * 512], onesb.t[0:16, :], r=[onesb])
            kb.barrier()
            p1.close()

        kb.op("act", lambda e: e.activation(out=kms.t[0:2, :], in_=kn.t[0:2, :], func=AF.Sqrt), r=[kn], w=[kms])
        kb.op("dve", lambda e: e.tensor_scalar(out=kms.t[0:2, :], in0=kms.t[0:2, :], scalar1=0.125 * 1.05, scalar2=None,
                                               op0=ALU.mult), r=[kms], w=[kms])
        nqr = Rot([kb.T(ph, [128, 512], F32) for _ in range(2)])
        mgr = Rot([kb.T(ph, [128, 512], F32) for _ in range(4)])
        mrr = Rot([kb.T(ph, [128, 512], BF16) for _ in range(2)])

        def q_pair(h, c0, QA, hd0, col0):
            ps = psA.next()
            projT(h, wsb, c0, ps)
            s = stg.next()
            kb.op("act", lambda e: e.activation(out=s.t[:], in_=ps.t[:], func=AF.Copy, scale=0.125), r=[ps], w=[s])
            q = sqb.next()
            kb.op("act", lambda e: e.activation(out=q.t[:], in_=ps.t[:], func=AF.Square), r=[ps], w=[q])
            kb.dma("sp", QA[hd0, 0:64, col0:col0 + 512], s.t[0:64, :], r=[s])
            kb.dma("sp", QA[hd0 + 1, 0:64, col0:col0 + 512], s.t[64:128, :], r=[s])
            kb.op("pe", lambda e: e.matmul(psN.t[0:2, :], lhsT=ind2.t[:, 0:2], rhs=q.t[:], start=True, stop=True),
                  r=[q, ind2], w=[psN])
            nq = nqr.next()
            kb.op("act", lambda e: e.activation(out=nq.t[0:2, :], in_=psN.t[0:2, :], func=AF.Sqrt), r=[psN], w=[nq])
            return nq

        for i in range(NQ):
            xg = xgr.next()
            kb.dma("sp", xg.t[:], xTv[:, :, i * 512:(i + 1) * 512], w=[xg])
            h = hr.next()
            rms_mod(rmt, xg, gs1, SH1, h)
            c0s = i * 512
            if layer == 0:
                for c4 in range(4):
                    nq = q_pair(h, 128 * c4, QAF, 2 * c4, c0s)
                    mr = mrr.next()
                    kb.op("dve", lambda e, nq=nq, mr=mr, c4=c4: e.tensor_scalar(
                        out=mr.t[0:2, :], in0=nq.t[0:2, :], scalar1=kms.t[0:2, c4:c4 + 1], scalar2=-1.0,
                        op0=ALU.mult, op1=ALU.mult), r=[nq, kms], w=[mr])
                    kb.dma("sp", QAF[2 * c4, 70, c0s:c0s + 512], mr.t[0:1, :], r=[mr])
                    kb.dma("sp", QAF[2 * c4 + 1, 70, c0s:c0s + 512], mr.t[1:2, :], r=[mr])
                for sp in range(2):
                    mgs = []
                    for gp in range(3):
                        c6 = 2 * gp + sp
                        nq = q_pair(h, 1544 + 128 * c6, QAD, 2 * c6, c0s)
                        mg = mgr.next()
                        kb.op("dve", lambda e, nq=nq, mg=mg, c6=c6: e.tensor_scalar(
                            out=mg.t[0:2, :], in0=nq.t[0:2, :], scalar1=kms.t[0:2, 4 + c6:5 + c6], scalar2=None,
                            op0=ALU.mult), r=[nq, kms], w=[mg])
                        mgs.append(mg)
                    kb.op("dve", lambda e: e.tensor_max(out=mgs[0].t[0:2, :], in0=mgs[0].t[0:2, :], in1=mgs[1].t[0:2, :]),
                          r=[mgs[0], mgs[1]], w=[mgs[0]])
                    kb.op("dve", lambda e: e.tensor_max(out=mgs[0].t[0:2, :], in0=mgs[0].t[0:2, :], in1=mgs[2].t[0:2, :]),
                          r=[mgs[0], mgs[2]], w=[mgs[0]])
                    mr = mrr.next()
                    kb.op("dve", lambda e, mr=mr: e.tensor_scalar(out=mr.t[0:2, :], in0=mgs[0].t[0:2, :],
                                                                  scalar1=bmax.t[0:2, 0:1], scalar2=-1.0, op0=ALU.add,
                                                                  op1=ALU.mult), r=[mgs[0], bmax], w=[mr])
                    for gp in range(3):
                        c6 = 2 * gp + sp
                        kb.dma("sp", QAD[2 * c6, 64, c0s:c0s + 512], mr.t[0:1, :], r=[mr])
                        kb.dma("sp", QAD[2 * c6 + 1, 64, c0s:c0s + 512], mr.t[1:2, :], r=[mr])
            else:
                for c8 in range(0 if 'F' in cut else 8):
                    nq = q_pair(h, 128 * c8, QAS, 2 * c8, c0s)
                    mg = mgr.next()
                    kb.op("dve", lambda e, nq=nq, mg=mg, c8=c8: e.tensor_scalar(
                        out=mg.t[0:2, :], in0=nq.t[0:2, :], scalar1=kms.t[0:2, c8:c8 + 1], scalar2=None,
                        op0=ALU.mult), r=[nq, kms], w=[mg])
                    mr2 = mrr.next()
                    kb.op("dve", lambda e, mg=mg, mr2=mr2: e.tensor_scalar(
                        out=mr2.t[0:2, :], in0=mg.t[0:2, :], scalar1=bmax.t[0:2, 0:1], scalar2=-1.0, op0=ALU.add,
                        op1=ALU.mult), r=[mg, bmax], w=[mr2])
                    kb.dma("sp", QAS[2 * c8, 64, c0s:c0s + 512], mr2.t[0:1, :], r=[mr2])
                    kb.dma("sp", QAS[2 * c8 + 1, 64, c0s:c0s + 512], mr2.t[1:2, :], r=[mr2])
                for c4 in range(0 if 'G' in cut else 4):
                    ps = psA.next()
                    projT(h, wsb, 1280 + 128 * c4, ps)
                    s = stg.next()
                    kb.op("act", lambda e, s=s, ps=ps: e.activation(out=s.t[:], in_=ps.t[:], func=AF.Copy), r=[ps], w=[s])
                    kb.dma("sp", QI[c4 * 128:(c4 + 1) * 128, c0s:c0s + 512], s.t[:, :], r=[s])
                for j in range(0 if 'H' in cut else 4):
                    projTok(h, j, wsb, 1856, 8, psC)
                    wt = tmpr.next()
                    kb.op("dve", lambda e, wt=wt: e.tensor_copy(out=wt.t[:, 0:8], in_=psC.t[:, 0:8]), r=[psC], w=[wt])
                    kb.dma("sp", WI[c0s + j * 128:c0s + (j + 1) * 128, :], wt.t[:, 0:128], r=[wt])
        kb.barrier()

    if stop_after <= 1:
        return nc, es, kb
    def attention_head(ph, kaD, qaD, vD, vc0, Kd, blocks_fn, finalize, bufs, expbias=None, tail_fn=None):
        ka, qa, vp, psS, psO, pTr = bufs
        kb.dma("sp", ka.t[0:Kd, :], kaD[0:Kd, :], w=[ka])
        kb.dma("sp", qa.t[0:Kd, :], qaD[0:Kd, :], w=[qa])
        kb.dma("sp", vp.t[:], vD, w=[vp])
        for i in range(NQ):
            blocks = blocks_fn(i)
            po = psO.next()
            nb = len(blocks)
            pend = []
            for n in range(nb + 2):
                if n < nb:
                    kt, masks, far = blocks[n]
                    if callable(masks):
                        masks = masks()
                    ps = psS.next()
                    nm_ = len(masks)
                    kb.op("pe", lambda e, kt=kt, ps=ps, nm_=nm_: e.matmul(
                        ps.t[:], lhsT=ka.t[0:Kd, kt * 128:(kt + 1) * 128], rhs=qa.t[0:Kd, i * 512:(i + 1) * 512],
                        start=True, stop=(nm_ == 0)), r=[ka, qa], w=[ps])
                    for mi, (lt, mk, mkap) in enumerate(masks):
                        kb.op("pe", lambda e, lt=lt, mkap=mkap, ps=ps, mi=mi, nm_=nm_: e.matmul(
                            ps.t[:], lhsT=lt.t[:], rhs=mkap, start=False, stop=(mi == nm_ - 1)), r=[lt, mk], w=[ps])
                    pT = pTr.next()
                    if far and expbias is not None:
                        kb.op("act", lambda e, pT=pT, ps=ps: e.activation(out=pT.t[:], in_=ps.t[:], func=AF.Exp,
                                                                          bias=expbias[1]), r=[ps, expbias[0]], w=[pT])
                    else:
                        kb.op("act", lambda e, pT=pT, ps=ps: e.activation(out=pT.t[:], in_=ps.t[:], func=AF.Exp),
                              r=[ps], w=[pT])
                    pend.append((kt, pT))
                if n >= 2:
                    kt, pT = pend[n - 2]
                    kb.op("pe", lambda e, kt=kt, pT=pT, n=n: e.matmul(
                        po.t[:], lhsT=vp.t[:, kt, :], rhs=pT.t[:], start=(n == 2),
                        stop=(n == nb + 1 and tail_fn is None)), r=[vp, pT], w=[po])
            if tail_fn is not None:
                tail_fn(i, po)
            finalize(i, po)

    def norm_write(ph_t, src_ps_or_sb, srcdeps, row0, i):
        rz, on = ph_t
        kb.op("dve", lambda e: e.reciprocal(out=rz.t[64:128, :], in_=src_ps_or_sb.t[64:128, :]), r=srcdeps, w=[rz])
        o = on.next()
        kb.op("dve", lambda e: e.tensor_tensor(out=o.t[0:64, :], in0=src_ps_or_sb.t[0:64, :], in1=rz.t[64:128, :],
                                               op=ALU.mult), r=srcdeps + [rz], w=[o])
        kb.dma("sp", MT[row0:row0 + 64, i * 512:(i + 1) * 512], o.t[0:64, :], r=[o])

    def toep_tile(mk, FR, idx, W_, dmin, e_):
        src = bass.AP(tensor=FR.tensor, offset=idx * 128 * W_ + 256 * e_ - dmin, ap=[[W_ - 1, 128], [256, 4], [1, 128]])
        kb.dma("pool", mk.t[:].rearrange("p (a c) -> p a c", a=4), src, w=[mk])

    with ExitStack() as ph:
        kar = Rot([kb.T(ph, [128, S], BF16) for _ in range(2)])
        qar = Rot([kb.T(ph, [128, SO], BF16) for _ in range(2)])
        vpr = Rot([kb.T(ph, [128, NT, 128], BF16) for _ in range(2)])
        psS = Rot([PS[0], PS[1], PS[2], PS[3]])
        psO = Rot([PS[4], PS[5]])
        pTr = Rot([kb.T(ph, [128, 512], BF16) for _ in range(4)])
        rz = kb.T(ph, [128, 512], F32)
        onr = Rot([kb.T(ph, [128, 512], BF16) for _ in range(2)])
        if layer == 0:
            cm = {}
            for part in range(2):
                for e_ in FOX_E:
                    mk = kb.T(ph, [128, 512], BF16)
                    toep_tile(mk, FcR, part, fgeo[1], fgeo[0], e_)
                    cm[(part, e_)] = mk

            def fox_blocks(i):
                bl = []
                for part in range(2):
                    for b in range(0, 4 * i + 4):
                        e_ = 4 * i - b
                        ms = [] if e_ >= 1 else [(identb, cm[(part, e_)], cm[(part, e_)].t[:])]
                        bl.append((b + part * NH, ms, False))
                return bl

            for hd in range(8):
                bufs = (kar.next(), qar.next(), vpr.next(), psS, psO, pTr)
                attention_head(ph, KAF[hd], QAF[hd], VF[hd], 0, 71, fox_blocks,
                               lambda i, po, hd=hd: norm_write((rz, onr), po, [po], hd * 64, i), bufs)
            oacc = kb.T(ph, [128, SO], F32)
            tot = kb.T(ph, [128, 512], F32)
            for j in range(4):
                for g in range(3):
                    eo, et, dmin, W_ = dgeo[g]
                    hd = g * 4 + j
                    dm = {}
                    with ExitStack() as ph2:
                        for part, el in ((0, eo), (1, et)):
                            for e_ in el:
                                mk = kb.T(ph2, [128, 512], BF16)
                                toep_tile(mk, FdR[g], j * 2 + part, W_, dmin, e_)
                                dm[(part, e_)] = mk

                        def dil_blocks(i, eo=eo, et=et, dm=dm):
                            bl = []
                            for part, el in ((0, eo), (1, et)):
                                for e_ in el:
                                    b = 4 * i - e_
                                    if 0 <= b < NH:
                                        bl.append((b + part * NH, [(identb, dm[(part, e_)], dm[(part, e_)].t[:])], False))
                            return bl

                        def dil_fin(i, po, g=g, j=j):
                            sl = oacc.t[:, i * 512:(i + 1) * 512]
                            if g == 0:
                                kb.op("act", lambda e: e.activation(out=sl, in_=po.t[:], func=AF.Copy), r=[po], w=[oacc])
                            elif g == 1:
                                kb.op("dve", lambda e: e.tensor_tensor(out=sl, in0=po.t[:], in1=sl, op=ALU.add),
                                      r=[po, oacc], w=[oacc])
                            else:
                                norm_write((rz, onr), po, [po], 512 + j * 64, i)

                        def dil_tail(i, po):
                            kb.op("pe", lambda e: e.matmul(po.t[:], lhsT=identf.t[:], rhs=oacc.t[:, i * 512:(i + 1) * 512],
                                                           start=False, stop=True), r=[identf, oacc], w=[po])

                        bufs = (kar.next(), qar.next(), vpr.next(), psS, psO, pTr)
                        attention_head(ph2, KAD[hd], QAD[hd], VD[hd], 0, 65, dil_blocks, dil_fin, bufs,
                                       tail_fn=(dil_tail if g == 2 else None))
                        kb.barrier()
        if layer == 1:
            pass
        kb.barrier()
    if layer == 1:
        NIT = 26
        with ExitStack() as ph:
            pst = Tl(ph.enter_context(nc.psum_tensor(tag + "pst", [128, 1024], BF16)))
            qis = kb.T(ph, [128, 4, SO], BF16)
            kb.dma("sp", qis.t[:], QI.rearrange("(c p) t -> p c t", p=128), w=[qis])
            kis = kb.T(ph, [128, S], BF16)
            kb.dma("sp", kis.t[:], KI[:, :], w=[kis])
            sc = kb.T(ph, [128, S], F32)
            junk = kb.T(ph, [128, S], BF16)
            nmb = kb.T(ph, [128, S], BF16)
            stage = kb.T(ph, [128, NT, 512], BF16)
            rlr = Rot([kb.T(ph, [128, 512], F32) for _ in range(3)])
            wq = kb.T(ph, [128, 8], F32)
            trim = kb.T(ph, [128, 128], F32)
            othd = kb.T(ph, [128, 128], F32)
            pw2 = kb.T(ph, [128, 32], F32)
            kb.dma("sp", trim.t[:], trimD[:, :], w=[trim])
            kb.dma("sp", othd.t[:], othdD[:, :], w=[othd])
            kb.dma("sp", pw2.t[:], pow2D[:, :], w=[pw2])
            dhs = kb.T(ph, [128, 32], F32)
            cnt = kb.T(ph, [128, 32], F32)
            lo = kb.T(ph, [128, 1], F32)
            mid = kb.T(ph, [128, 1], F32)
            st4 = kb.T(ph, [128, 4], F32)
            psI = Rot([PS[0], PS[1], PS[2], PS[3]])
            for i in range(NQ):
                NCH = i + 1
                RW = 2 * NCH * 512
                for a4 in range(4):
                    aq = 4 * i + a4
                    kb.dma("sp", wq.t[:], WI[aq * 128:(aq + 1) * 128, 0:8], w=[wq])
                    for kc in range(NCH):
                        for part in range(2):
                            c0 = (2 * kc + part) * 512
                            k0 = part * SO + kc * 512
                            for hh in range(8):
                                pb = (hh % 2) * 64
                                ps = psI.next()
                                kb.op("pe", lambda e, hh=hh, pb=pb, ps=ps, k0=k0: e.matmul(
                                    ps.t[:], lhsT=qis.t[pb:pb + 64, hh // 2, aq * 128:(aq + 1) * 128],
                                    rhs=kis.t[pb:pb + 64, k0:k0 + 512], start=True, stop=True), r=[qis, kis], w=[ps])
                                rl = rlr.next()
                                kb.op("act", lambda e, rl=rl, ps=ps: e.activation(out=rl.t[:], in_=ps.t[:], func=AF.Relu),
                                      r=[ps], w=[rl])
                                if hh == 0:
                                    kb.op("dve", lambda e, rl=rl, c0=c0: e.tensor_scalar(
                                        out=sc.t[:, c0:c0 + 512], in0=rl.t[:], scalar1=wq.t[:, 0:1], scalar2=None,
                                        op0=ALU.mult), r=[rl, wq], w=[sc])
                                else:
                                    kb.op("dve", lambda e, rl=rl, c0=c0, hh=hh: e.scalar_tensor_tensor(
                                        out=sc.t[:, c0:c0 + 512], in0=rl.t[:], scalar=wq.t[:, hh:hh + 1],
                                        in1=sc.t[:, c0:c0 + 512], op0=ALU.mult, op1=ALU.add), r=[rl, wq, sc], w=[sc])
                    kb.op("dve", lambda e: e.tensor_reduce(out=st4.t[:, 0:1], in_=sc.t[:, 0:RW], axis=AX.X, op=ALU.max),
                          r=[sc], w=[st4])
                    kb.op("dve", lambda e: e.tensor_reduce(out=st4.t[:, 1:2], in_=sc.t[:, 0:RW], axis=AX.X, op=ALU.min),
                          r=[sc], w=[st4])
                    for part in range(2):
                        base = (2 * i + part) * 512
                        for j in range(4):
                            blk = sc.t[:, base + j * 128:base + (j + 1) * 128]
                            if j > a4:
                                kb.op("pool", lambda e, blk=blk: e.memset(blk, -1.0e9), w=[sc])
                            elif j == a4:
                                mt_ = trim if part == 0 else othd
                                kb.op("dve", lambda e, blk=blk, mt_=mt_: e.tensor_tensor(out=blk, in0=blk, in1=mt_.t[:],
                                                                                         op=ALU.add), r=[sc, mt_], w=[sc])
                    if aq == 0:
                        kb.op("dve", lambda e: e.memset(lo.t[:], -1.0e8), w=[lo])
                    else:
                        kb.op("dve", lambda e: e.tensor_tensor(out=st4.t[:, 2:3], in0=st4.t[:, 0:1], in1=st4.t[:, 1:2],
                                                               op=ALU.subtract), r=[st4], w=[st4])
                        kb.op("dve", lambda e: e.tensor_scalar(out=st4.t[:, 2:3], in0=st4.t[:, 2:3], scalar1=2.0, scalar2=None,
                                                               op0=ALU.add), r=[st4], w=[st4])
                        kb.op("dve", lambda e: e.tensor_scalar(out=dhs.t[:], in0=pw2.t[:], scalar1=st4.t[:, 2:3], scalar2=None,
                                                               op0=ALU.mult), r=[pw2, st4], w=[dhs])
                        kb.op("dve", lambda e: e.tensor_scalar(out=lo.t[:], in0=st4.t[:, 1:2], scalar1=-1.0, scalar2=None,
                                                               op0=ALU.add), r=[st4], w=[lo])
                        kb.op("dve", lambda e: e.memset(cnt.t[:], 0.0), w=[cnt])
                        for it in range(NIT):
                            kb.op("dve", lambda e, it=it: e.tensor_tensor(out=mid.t[:], in0=lo.t[:], in1=dhs.t[:, it:it + 1],
                                                                          op=ALU.add), r=[lo, dhs], w=[mid])
                            kb.op("dve", lambda e, it=it: e.tensor_scalar(
                                out=junk.t[:, 0:RW], in0=sc.t[:, 0:RW], scalar1=mid.t[:, 0:1], scalar2=0.0, op0=ALU.is_ge,
                                op1=ALU.add, accum_out=cnt.t[:, it:it + 1]), r=[sc, mid], w=[junk, cnt])
                            kb.op("dve", lambda e, it=it: e.scalar_tensor_tensor(
                                out=mid.t[:], in0=cnt.t[:, it:it + 1], scalar=255.5, in1=dhs.t[:, it:it + 1], op0=ALU.is_ge,
                                op1=ALU.mult), r=[cnt, dhs], w=[mid])
                            kb.op("dve", lambda e: e.tensor_tensor(out=lo.t[:], in0=lo.t[:], in1=mid.t[:], op=ALU.add),
                                  r=[lo, mid], w=[lo])
                    kb.op("dve", lambda e: e.tensor_scalar(out=nmb.t[:, 0:RW], in0=sc.t[:, 0:RW], scalar1=lo.t[:, 0:1],
                                                           scalar2=1.0, op0=ALU.is_ge, op1=ALU.subtract), r=[sc, lo], w=[nmb])
                    nblk = RW // 128
                    for b0 in range(0, nblk, 8):
                        nb_ = min(8, nblk - b0)
                        for bb in range(nb_):
                            kb.op("pe", lambda e, bb=bb, b0=b0: e.transpose(
                                out=pst.t[:, bb * 128:(bb + 1) * 128], in_=nmb.t[:, (b0 + bb) * 128:(b0 + bb + 1) * 128],
                                identity=identb.t[:]), r=[nmb, identb], w=[pst])
                        for half in range(nb_ // 4):
                            cb = (b0 // 4) + half
                            kt0 = (cb % 2) * NH + (cb // 2) * 4
                            kb.op("act", lambda e, half=half, kt0=kt0: e.activation(
                                out=stage.t[:, kt0:kt0 + 4, a4 * 128:(a4 + 1) * 128],
                                in_=pst.t[:, half * 512:(half + 1) * 512].rearrange("p (j c) -> p j c", j=4), func=AF.Copy),
                                r=[pst], w=[stage])
                for part in range(2):
                    kb.dma("sp", NM[i, part * NH:part * NH + 4 * NCH].rearrange("k p c -> p k c"),
                           stage.t[:, part * NH:part * NH + 4 * NCH, :], r=[stage])
            kb.barrier()

        with ExitStack() as ph:
            kar = Rot([kb.T(ph, [128, S], BF16) for _ in range(2)])
            qar = Rot([kb.T(ph, [128, SO], BF16) for _ in range(2)])
            vpr = Rot([kb.T(ph, [128, NT, 128], BF16) for _ in range(2)])
            psS = Rot([PS[0], PS[1], PS[2], PS[3]])
            psO = Rot([PS[4], PS[5]])
            pTr = Rot([kb.T(ph, [128, 512], BF16) for _ in range(4)])
            rz = kb.T(ph, [128, 512], F32)
            onr = Rot([kb.T(ph, [128, 512], BF16) for _ in range(2)])
            i3b = kb.T(ph, [128, 128], BF16)
            kb.op("dve", lambda e: e.tensor_scalar(out=i3b.t[:], in0=identf.t[:], scalar1=-NEG, scalar2=None, op0=ALU.mult),
                  r=[identf], w=[i3b])
            rbf_ = kb.T(ph, [128, 16], F32)
            kb.dma("sp", rbf_.t[:], rbfar[:, :], w=[rbf_])
            for q_ in range(32):
                W_ = sgeo[1]
                kb.dma("sp", FsR[q_, :, :], bass.AP(tensor=FsD.tensor, offset=q_ * W_, ap=[[0, 128], [1, W_]]))
            kb.barrier()
            nmr = Rot([kb.T(ph, [128, 4, 512], BF16) for _ in range(6)])
            bt = {}
            for part in range(2):
                for e_ in DSA_NEAR_E:
                    bt[(part, e_)] = kb.T(ph, [128, 512], BF16)
            for hd in range(16):
                for part in range(2):
                    for e_ in DSA_NEAR_E:
                        toep_tile(bt[(part, e_)], FsR, hd * 2 + part, sgeo[1], sgeo[0], e_)

                def dsa_blocks(i, hd=hd):
                    pieces = [(part, kc) for part in range(2) for kc in range(i + 1)]
                    tiles = {}

                    def load(pi):
                        if pi >= len(pieces) or pi in tiles:
                            return
                        part, kc = pieces[pi]
                        nt_ = nmr.next()
                        kb.dma("sp", nt_.t[:], NM[i, part * NH + 4 * kc:part * NH + 4 * kc + 4].rearrange("k p c -> p k c"),
                               w=[nt_])
                        tiles[pi] = nt_

                    bl = []
                    for pi, (part, kc) in enumerate(pieces):
                        for j in range(4):
                            b = 4 * kc + j
                            e_ = 4 * i - b
                            far = e_ > DSA_NEAR_E[-1]

                            def mk(pi=pi, j=j, part=part, e_=e_, far=far):
                                load(pi)
                                if j == 0:
                                    load(pi + 1)
                                    load(pi + 2)
                                nt_ = tiles[pi]
                                ms = [(i3b, nt_, nt_.t[:, j, :])]
                                if not far:
                                    ms.append((identb, bt[(part, e_)], bt[(part, e_)].t[:]))
                                return ms
                            bl.append((b + part * NH, mk, far))
                    return bl

                bufs = (kar.next(), qar.next(), vpr.next(), psS, psO, pTr)
                attention_head(ph, KAS[hd], QAS[hd], VS[hd], 0, 65, dsa_blocks,
                               lambda i, po, hd=hd: norm_write((rz, onr), po, [po], hd * 64, i), bufs,
                               expbias=(rbf_, rbf_.t[:, hd:hd + 1]))
            kb.barrier()
    if stop_after <= 2:
        return nc, es, kb

    KC = NMO // 128
    with ExitStack() as ph:
        wo = kb.T(ph, [128, KC, 1024], BF16)
        for kc in range(KC):
            kb.dma("pool", wo.t[:, kc, :], w_out[kc * 128:(kc + 1) * 128, :], w=[wo])
        rwf = kb.T(ph, [128, 8, NE], F32)
        kb.dma("sp", rwf.t[:], rw.rearrange("(k p) e -> p k e", p=128), w=[rwf])
        rbs = kb.T(ph, [128, NE], F32)
        kb.dma("sp", rbs.t[:], rbb[:, :], w=[rbs])
        xgr = Rot([kb.T(ph, [128, 8, 512], F32) for _ in range(2)])
        mtr = Rot([kb.T(ph, [128, KC, 512], BF16) for _ in range(2)])
        x1r = Rot([kb.T(ph, [128, 8, 512], F32) for _ in range(2)])
        h2f = kb.T(ph, [128, 8, 512], F32)
        h2r = Rot([kb.T(ph, [128, 8, 512], BF16) for _ in range(2)])
        sqr = Rot([kb.T(ph, [128, 512], F32) for _ in range(2)])
        tmpr = Rot([kb.T(ph, [128, 512], F32) for _ in range(2)])
        rstd = kb.T(ph, [128, 512], F32)
        rmt = (sqr, rstd, tmpr, PS[0])
        psA = Rot([PS[1], PS[2]])
        psL = Rot([PS[3], PS[4]])
        psT = PS[5]
        lg = kb.T(ph, [128, NE], F32)
        ex = kb.T(ph, [128, NE], F32)
        mk_ = kb.T(ph, [128, NE], F32)
        gg = kb.T(ph, [128, NE], F32)
        mx8 = kb.T(ph, [128, 8], F32)
        sm1 = kb.T(ph, [128, 2], F32)
        gTr = Rot([kb.T(ph, [128, 512], F32) for _ in range(2)])
        MTv = MT.rearrange("(k p) t -> p k t", p=128)
        X1v = X1.rearrange("(k p) t -> p k t", p=128)
        H2v = H2.rearrange("(k p) t -> p k t", p=128)
        for i in range(NQ):
            cs = slice(i * 512, (i + 1) * 512)
            xg = xgr.next()
            kb.dma("sp", xg.t[:], xTv[:, :, cs], w=[xg])
            mt = mtr.next()
            kb.dma("sp", mt.t[:], MTv[:, :, cs], w=[mt])
            x1 = x1r.next()
            for dc in range(8):
                ps = psA.next()
                for kc in range(KC):
                    kb.op("pe", lambda e, kc=kc, dc=dc, ps=ps: e.matmul(
                        ps.t[:], lhsT=wo.t[:, kc, dc * 128:(dc + 1) * 128], rhs=mt.t[:, kc, :], start=(kc == 0),
                        stop=(kc == KC - 1)), r=[wo, mt], w=[ps])
                kb.op("dve", lambda e, dc=dc, ps=ps: e.scalar_tensor_tensor(
                    out=x1.t[:, dc, :], in0=ps.t[:], scalar=mods.t[:, GT1 + dc:GT1 + dc + 1], in1=xg.t[:, dc, :],
                    op0=ALU.mult, op1=ALU.add), r=[ps, mods, xg], w=[x1])
            kb.dma("sp", X1v[:, :, cs], x1.t[:], r=[x1])
            h2 = h2r.next()
            rms_mod(rmt, x1, gs2, SH2, h2, hf32=h2f)
            kb.dma("sp", H2v[:, :, cs], h2.t[:], r=[h2])
            gT = gTr.next()
            for j in range(4):
                pl = psL.next()
                for k in range(8):
                    kb.op("pe", lambda e, k=k, j=j, pl=pl: e.matmul(
                        pl.t[:, 0:NE], lhsT=h2f.t[:, k, j * 128:(j + 1) * 128], rhs=rwf.t[:, k, :], start=(k == 0),
                        stop=(k == 7)), r=[h2f, rwf], w=[pl])
                kb.op("dve", lambda e, pl=pl: e.tensor_tensor(out=lg.t[:], in0=pl.t[:, 0:NE], in1=rbs.t[:], op=ALU.add),
                      r=[pl, rbs], w=[lg])
                kb.op("dve", lambda e: e.max(out=mx8.t[:], in_=lg.t[:]), r=[lg], w=[mx8])
                kb.op("dve", lambda e: e.tensor_scalar(out=sm1.t[:, 0:1], in0=mx8.t[:, 0:1], scalar1=-1.0, scalar2=None,
                                                       op0=ALU.mult), r=[mx8], w=[sm1])
                kb.op("act", lambda e: e.activation(out=ex.t[:], in_=lg.t[:], func=AF.Exp, bias=sm1.t[:, 0:1]),
                      r=[lg, sm1], w=[ex])
                kb.op("dve", lambda e: e.tensor_scalar(out=mk_.t[:], in0=lg.t[:], scalar1=mx8.t[:, 3:4], scalar2=None,
                                                       op0=ALU.is_ge), r=[lg, mx8], w=[mk_])
                kb.op("dve", lambda e: e.tensor_tensor(out=gg.t[:], in0=ex.t[:], in1=mk_.t[:], op=ALU.mult),
                      r=[ex, mk_], w=[gg])
                kb.op("dve", lambda e: e.reduce_sum(out=sm1.t[:, 1:2], in_=gg.t[:], axis=AX.X), r=[gg], w=[sm1])
                kb.op("dve", lambda e: e.reciprocal(out=sm1.t[:, 1:2], in_=sm1.t[:, 1:2]), r=[sm1], w=[sm1])
                kb.op("dve", lambda e: e.tensor_scalar(out=gg.t[:], in0=gg.t[:], scalar1=sm1.t[:, 1:2], scalar2=None,
                                                       op0=ALU.mult), r=[gg, sm1], w=[gg])
                kb.op("pe", lambda e: e.transpose(out=psT.t[0:NE, 0:128], in_=gg.t[:], identity=identf.t[:]),
                      r=[gg, identf], w=[psT])
                kb.op("act", lambda e, j=j: e.activation(out=gT.t[0:NE, j * 128:(j + 1) * 128], in_=psT.t[0:NE, 0:128],
                                                         func=AF.Copy), r=[psT], w=[gT])
            kb.dma("sp", GTs[:, cs], gT.t[0:NE, :], r=[gT])
        kb.barrier()
    if stop_after <= 3:
        return nc, es, kb

    P = min(1024, SO)
    NPG = P // 512
    outv = outD.rearrange("(k p) t -> p k t", p=128)
    with ExitStack() as ph:
        PS7 = Tl(ph.enter_context(nc.psum_tensor(tag + "ps7", [128, 512], F32)))
        b1c = kb.T(ph, [128, NE * 16], F32)
        kb.dma("sp", b1c.t[:], b1col[:, :], w=[b1c])
        b1p = kb.T(ph, [128, NE * 16], F32)
        kb.op("dve", lambda e: e.tensor_scalar(out=b1p.t[:], in0=b1c.t[:], scalar1=1.0, scalar2=None, op0=ALU.add),
              r=[b1c], w=[b1p])
        selb = kb.T(ph, [128, NE * 128], BF16)
        kb.dma("pool", selb.t[0:NE, :], selD[:, :], w=[selb])
        b2f = kb.T(ph, [128, D], F32)
        kb.dma("sp", b2f.t[0:NE, :], b2[:, :], w=[b2f])
        H2p = kb.T(ph, [128, 8, P], BF16)
        acc = kb.T(ph, [128, 8, P], F32)
        gtf = kb.T(ph, [128, P], F32)
        gtr_ = kb.T(ph, [128, P], F32)
        gth = kb.T(ph, [128, P], BF16)
        gtl = kb.T(ph, [128, P], BF16)
        if layer == 1:
            fns = kb.T(ph, [128, 8], F32)
            kb.dma("sp", fns.t[:], fnc[:, :], w=[fns])
        X1v = X1.rearrange("(k p) t -> p k t", p=128)
        H2v = H2.rearrange("(k p) t -> p k t", p=128)
        for pz in range(SO // P):
            c0 = pz * P
            kb.dma("sp", H2p.t[:], H2v[:, :, c0:c0 + P], w=[H2p])
            kb.dma("sp", gtf.t[0:NE, :], GTs[:, c0:c0 + P], w=[gtf])
            kb.op("dve", lambda e: e.tensor_copy(out=gth.t[0:NE, :], in_=gtf.t[0:NE, :]), r=[gtf], w=[gth])
            kb.op("dve", lambda e: e.tensor_tensor(out=gtr_.t[0:NE, :], in0=gtf.t[0:NE, :], in1=gth.t[0:NE, :],
                                                   op=ALU.subtract), r=[gtf, gth], w=[gtr_])
            kb.op("dve", lambda e: e.tensor_copy(out=gtl.t[0:NE, :], in_=gtr_.t[0:NE, :]), r=[gtr_], w=[gtl])
            with ExitStack() as pe_:
                w1r = Rot([kb.T(pe_, [128, 8, 2048], BF16) for _ in range(2)])
                w2r = Rot([kb.T(pe_, [128, 8, 1024], BF16) for _ in range(2)])
                glr = Rot([kb.T(pe_, [128, 512], F32) for _ in range(2)])
                sgr = Rot([kb.T(pe_, [128, 512], F32) for _ in range(2)])
                lir = Rot([kb.T(pe_, [128, 512], F32) for _ in range(2)])
                t1r = Rot([kb.T(pe_, [128, 512], F32) for _ in range(2)])
                Gsr = Rot([kb.T(pe_, [128, 512], F32) for _ in range(2)])
                abr = Rot([kb.T(pe_, [128, 512], BF16) for _ in range(10)])
                psg = Rot([PS[0], PS[1]])
                psl = Rot([PS[2], PS[3]])
                psG = PS[4]
                psy = Rot([PS[5], PS[6]])
                psB = PS7
                for gi in range(NPG):
                    cs = slice(gi * 512, (gi + 1) * 512)
                    for dc in range(8):
                        kb.op("pe", lambda e, dc=dc, cs=cs: e.matmul(
                            psB.t[:], lhsT=b2f.t[0:NE, dc * 128:(dc + 1) * 128], rhs=gtf.t[0:NE, cs], start=True,
                            stop=True), r=[b2f, gtf], w=[psB])
                        kb.op("act", lambda e, dc=dc, cs=cs: e.activation(out=acc.t[:, dc, cs], in_=psB.t[:], func=AF.Copy),
                              r=[psB], w=[acc])
                def load_w(x_):
                    a_ = w1r.next()
                    b_ = w2r.next()
                    kb.dma("pool", a_.t[:], w1[x_].rearrange("(k p) n -> p k n", p=128), w=[a_])
                    kb.dma("pool", b_.t[:], w2[x_].rearrange("(k p) n -> p k n", p=128), w=[b_])
                    return a_, b_

                nxt_w = load_w(0)
                for ex_ in range(NE):
                    w1e, w2e = nxt_w
                    if ex_ + 1 < NE:
                        nxt_w = load_w(ex_ + 1)
                    for gi in range(NPG):
                        cs = slice(gi * 512, (gi + 1) * 512)
                        kb.op("pe", lambda e, cs=cs: e.matmul(psG.t[:], lhsT=selb.t[0:NE, ex_ * 128:(ex_ + 1) * 128],
                                                              rhs=gth.t[0:NE, cs], start=True, stop=False),
                              r=[selb, gth], w=[psG])
                        kb.op("pe", lambda e, cs=cs: e.matmul(psG.t[:], lhsT=selb.t[0:NE, ex_ * 128:(ex_ + 1) * 128],
                                                              rhs=gtl.t[0:NE, cs], start=False, stop=True),
                              r=[selb, gtl], w=[psG])
                        Gs = Gsr.next()
                        kb.op("act", lambda e, Gs=Gs: e.activation(out=Gs.t[:], in_=psG.t[:], func=AF.Copy), r=[psG], w=[Gs])
                        acts = []
                        for hc in range(8):
                            pg = psg.next()
                            pl = psl.next()
                            for k in range(8):
                                kb.op("pe", lambda e, k=k, hc=hc, pg=pg, cs=cs: e.matmul(
                                    pg.t[:], lhsT=w1e.t[:, k, hc * 128:(hc + 1) * 128], rhs=H2p.t[:, k, cs],
                                    start=(k == 0), stop=(k == 7)), r=[w1e, H2p], w=[pg])
                            for k in range(8):
                                kb.op("pe", lambda e, k=k, hc=hc, pl=pl, cs=cs: e.matmul(
                                    pl.t[:], lhsT=w1e.t[:, k, 1024 + hc * 128:1024 + (hc + 1) * 128], rhs=H2p.t[:, k, cs],
                                    start=(k == 0), stop=(k == 7)), r=[w1e, H2p], w=[pl])
                            gl = glr.next()
                            sg_ = sgr.next()
                            li = lir.next()
                            t1 = t1r.next()
                            ab_ = abr.next()
                            bc = ex_ * 16 + hc
                            kb.op("dve", lambda e, gl=gl, pg=pg, bc=bc: e.tensor_scalar(
                                out=gl.t[:], in0=pg.t[:], scalar1=b1c.t[:, bc:bc + 1], scalar2=7.0, op0=ALU.add,
                                op1=ALU.min), r=[pg, b1c], w=[gl])
                            kb.op("act", lambda e, gl=gl, sg_=sg_: e.activation(out=sg_.t[:], in_=gl.t[:], func=AF.Sigmoid,
                                                                                scale=1.702), r=[gl], w=[sg_])
                            kb.op("act", lambda e, li=li, pl=pl, bc=bc: e.activation(
                                out=li.t[:], in_=pl.t[:], func=AF.Identity, bias=b1p.t[:, bc + 8:bc + 9]), r=[pl, b1p], w=[li])
                            kb.op("dve", lambda e, li=li: e.tensor_scalar(out=li.t[:], in0=li.t[:], scalar1=-6.0, scalar2=8.0,
                                                                          op0=ALU.max, op1=ALU.min), r=[li], w=[li])
                            kb.op("dve", lambda e, t1=t1, gl=gl, sg_=sg_: e.tensor_tensor(out=t1.t[:], in0=gl.t[:], in1=sg_.t[:],
                                                                                          op=ALU.mult), r=[gl, sg_], w=[t1])
                            kb.op("dve", lambda e, t1=t1, li=li: e.tensor_tensor(out=t1.t[:], in0=t1.t[:], in1=li.t[:],
                                                                                 op=ALU.mult), r=[t1, li], w=[t1])
                            kb.op("dve", lambda e, ab_=ab_, t1=t1, Gs=Gs: e.tensor_tensor(out=ab_.t[:], in0=t1.t[:], in1=Gs.t[:],
                                                                                          op=ALU.mult), r=[t1, Gs], w=[ab_])
                            acts.append(ab_)
                        for dc in range(8):
                            py = psy.next()
                            for hc in range(8):
                                kb.op("pe", lambda e, hc=hc, dc=dc, py=py: e.matmul(
                                    py.t[:], lhsT=w2e.t[:, hc, dc * 128:(dc + 1) * 128], rhs=acts[hc].t[:],
                                    start=(hc == 0), stop=(hc == 7)), r=[w2e, acts[hc]], w=[py])
                            kb.op("dve", lambda e, dc=dc, py=py, cs=cs: e.tensor_tensor(
                                out=acc.t[:, dc, cs], in0=py.t[:], in1=acc.t[:, dc, cs], op=ALU.add), r=[py, acc], w=[acc])
                kb.barrier()
            with ExitStack() as pf:
                x1r = Rot([kb.T(pf, [128, 8, 512], F32) for _ in range(2)])
                x2r = Rot([kb.T(pf, [128, 8, 512], F32) for _ in range(2)])
                sqr = Rot([kb.T(pf, [128, 512], F32) for _ in range(2)])
                rstd = kb.T(pf, [128, 512], F32)
                for gi in range(NPG):
                    cs = slice(gi * 512, (gi + 1) * 512)
                    gs_ = slice(c0 + gi * 512, c0 + (gi + 1) * 512)
                    x1 = x1r.next()
                    kb.dma("sp", x1.t[:], X1v[:, :, gs_], w=[x1])
                    x2 = x2r.next()
                    for dc in range(8):
                        kb.op("dve", lambda e, dc=dc, cs=cs: e.scalar_tensor_tensor(
                            out=x2.t[:, dc, :], in0=acc.t[:, dc, cs], scalar=mods.t[:, GT2 + dc:GT2 + dc + 1],
                            in1=x1.t[:, dc, :], op0=ALU.mult, op1=ALU.add), r=[acc, mods, x1], w=[x2])
                    if layer == 1:
                        ps = PS7
                        for k in range(8):
                            sq = sqr.next()
                            kb.op("act", lambda e, sq=sq, k=k: e.activation(out=sq.t[:], in_=x2.t[:, k, :], func=AF.Square),
                                  r=[x2], w=[sq])
                            kb.op("pe", lambda e, sq=sq, k=k: e.matmul(ps.t[:], lhsT=onesf.t[:], rhs=sq.t[:], start=(k == 0),
                                                                      stop=(k == 7)), r=[sq, onesf], w=[ps])
                        kb.op("dve", lambda e: e.tensor_scalar(out=rstd.t[:], in0=ps.t[:], scalar1=1.0 / D, scalar2=EPS,
                                                               op0=ALU.mult, op1=ALU.add), r=[ps], w=[rstd])
                        kb.op("act", lambda e: e.activation(out=rstd.t[:], in_=rstd.t[:], func=AF.Sqrt), r=[rstd], w=[rstd])
                        kb.op("dve", lambda e: e.reciprocal(out=rstd.t[:], in_=rstd.t[:]), r=[rstd], w=[rstd])
                        for k in range(8):
                            kb.op("dve", lambda e, k=k: e.scalar_tensor_tensor(
                                out=x2.t[:, k, :], in0=x2.t[:, k, :], scalar=fns.t[:, k:k + 1], in1=rstd.t[:],
                                op0=ALU.mult, op1=ALU.mult), r=[x2, fns, rstd], w=[x2])
                    kb.dma("sp", outv[:, :, gs_], x2.t[:], r=[x2])
                kb.barrier()
    kb.barrier()
    return nc, es, kb


def col8(v):
    return np.ascontiguousarray(np.asarray(v, np.float32).reshape(-1, 128).T)


def toep_vec(fn, dmin, W, shift):
    d = np.arange(W) + dmin + shift
    return fn(d).astype(np.float32)


def prep_common(p, pre, xb, cb, hf, S):
    perm = sigma_perm(S, hf)
    m = {}
    if xb is not None:
        m["xT"] = np.ascontiguousarray(xb[perm].T)
    m["ccol"] = col8(cb)
    m["ada_w"] = p[pre + "ada_w"]
    m["adab"] = col8(p[pre + "ada_b"])
    m["n1col"] = col8(p[pre + "norm1"])
    m["n2col"] = col8(p[pre + "norm2"])
    m["w_in"] = p[pre + "w_in"]
    m["w_out"] = p[pre + "w_out"]
    m["rw"] = p[pre + "router_w"]
    m["rbb"] = np.ascontiguousarray(np.tile(p[pre + "router_b"][None, :], (128, 1)))
    m["w1"] = p[pre + "w1"]
    m["b1col"] = np.ascontiguousarray(p[pre + "b1"].reshape(NE, 16, 128).transpose(2, 0, 1).reshape(128, NE * 16))
    m["w2"] = p[pre + "w2"]
    m["b2"] = p[pre + "b2"]
    m["ident"] = np.eye(128, dtype=np.float32)
    sel = np.zeros((NE, NE * 128), np.float32)
    for e in range(NE):
        sel[e, e * 128:(e + 1) * 128] = 1.0
    m["sel"] = sel
    m["rbflat"] = np.ascontiguousarray(np.tile(p["rel_bias"].reshape(1, 512), (128, 1)))
    return m


def prep_l0(p, xb, cb, hf, S):
    m = prep_common(p, "l0_", xb, cb, hf, S)
    rb = p["rel_bias"]
    m["fbb"] = np.ascontiguousarray(np.tile(p["l0_fox_fb"][None, :], (128, 1)))
    m["R"] = fox_R(hf)
    sh = 128 * (2 * hf - 1)
    dmin, W = toep_geom(-3, 0)
    causal = lambda d: np.where(d >= 0, 0.0, NEG)
    m["Fc"] = np.stack([toep_vec(causal, dmin, W, 0), toep_vec(causal, dmin, W, sh)])
    for g, (win, r) in enumerate(DIL_PAIRS):
        eo, et = dil_evals(win)
        dmin, W = toep_geom(min(eo + et), max(eo + et))
        rows = []
        for j in range(4):
            tab = rb[:, g * 4 + j]

            def fn(d, tab=tab, win=win, r=r):
                ok = (d >= 0) & (d <= win) & (d % r == 0)
                return np.where(ok, tab[t5_bucket_np(np.clip(d, 0, None))], NEG)
            rows.append(toep_vec(fn, dmin, W, 0))
            rows.append(toep_vec(fn, dmin, W, sh))
        m[f"Fd{g}"] = np.stack(rows)
    return m


def prep_l1(p, xb, cb, hf, S):
    m = prep_common(p, "l1_", xb, cb, hf, S)
    rb = p["rel_bias"]
    m["kvn"] = col8(p["l1_kv_norm"])
    m["w_ukv"] = p["l1_w_ukv"]
    m["fncol"] = col8(p["final_norm"])
    tri = np.where(np.arange(128)[None, :] <= np.arange(128)[:, None], 0.0, -1.0e9).astype(np.float32)
    m["trim"] = tri
    m["othd"] = np.full((128, 128), 0.0 if hf == 1 else -1.0e9, np.float32)
    m["pow2"] = np.ascontiguousarray(np.tile((2.0 ** -(np.arange(32) + 1.0))[None, :], (128, 1)).astype(np.float32))
    sh = 128 * (2 * hf - 1)
    dmin, W = toep_geom(-3, DSA_NEAR_E[-1])
    rows = []
    for h in range(16):
        tab = rb[:, h]
        fn = lambda d, tab=tab: np.where(d >= 0, tab[t5_bucket_np(np.clip(d, 0, None))], 0.0)
        rows.append(toep_vec(fn, dmin, W, 0))
        rows.append(toep_vec(fn, dmin, W, sh))
    m["Fs"] = np.stack(rows)
    m["rbfar"] = np.ascontiguousarray(np.tile(rb[31:32, :], (128, 1)))
    return m


_PROG = {}
PV0 = ("xT", "R", "Fc", "Fd0", "Fd1", "Fd2")


def build_fused(S):
    SO = S // 2
    nc = bass.Bass("TRN2", target_bir_lowering=False)
    es = ExitStack()
    kb = KB(nc, es)
    PS = [Tl(es.enter_context(nc.psum_tensor(f"ps{i}", [128, 512], F32))) for i in range(7)]
    ctx = (nc, es, kb, PS)
    shared = {}
    X1F = nc.dram_tensor("X1F", [D, S], F32, kind="Internal").ap()
    build_layer(0, S, ctx=ctx, tag="a_", shared=shared, pervar=PV0, out_ap=X1F[:, 0:SO])
    build_layer(0, S, ctx=ctx, tag="b_", shared=shared, pervar=PV0, out_ap=X1F[:, SO:S])
    build_layer(1, S, ctx=ctx, tag="c_", shared=shared, xT_in=X1F)
    es.close()
    return nc


def _get_prog(S):
    if S not in _PROG:
        _PROG[S] = build_fused(S)
    return _PROG[S]


def kernel(**inputs):
    p = {k: np.ascontiguousarray(np.asarray(v, dtype=np.float32)) for k, v in inputs.items()}
    x = p["x"]
    B, S = x.shape[0], x.shape[1]
    SO = S // 2
    n = 2 * B
    nc = _get_prog(S)
    maps = []
    for c in range(n):
        b, hf = c // 2, c % 2
        ma = prep_l0(p, x[b], p["c"][b], hf, S)
        mb = prep_l0(p, x[b], p["c"][b], 1 - hf, S)
        m1 = prep_l1(p, None, p["c"][b], hf, S)
        m = {}
        for k, v in ma.items():
            m[("a_" + k) if k in PV0 else ("w0_" + k)] = v
        for k in PV0:
            m["b_" + k] = mb[k]
        for k, v in m1.items():
            m["w1_" + k] = v
        maps.append(m)
    res = run_bass_kernel_spmd(nc, maps, core_ids=list(range(n)))
    out = np.empty_like(x)
    for c in range(n):
        own = sigma_perm(S, c % 2)[:SO]
        out[c // 2][own] = np.asarray(res.results[c]["c_out"], np.float32).T
    return out
```

```python
import math
from contextlib import ExitStack
import numpy as np
import concourse.bass as bass
import concourse.mybir as mybir
from concourse.bass_utils import run_bass_kernel_spmd

F32, BF16 = mybir.dt.float32, mybir.dt.bfloat16
ALU = mybir.AluOpType
AF = mybir.ActivationFunctionType
AX = mybir.AxisListType

NDS = 12
ROT = 24000
NEG = -30000.0
D = 1024
NE = 32
EPS = 1e-6


class Dep:
    __slots__ = ("lw", "rd")

    def __init__(self):
        self.lw = None
        self.rd = {}


class Tl:
    __slots__ = ("t", "d", "_pre")

    def __init__(self, t):
        self.t = t
        self.d = Dep()
        self._pre = False


class KB:
    def __init__(self, nc, es):
        self.nc = nc
        self.es = es
        self.eng = {"pe": nc.tensor, "dve": nc.vector, "act": nc.scalar, "pool": nc.gpsimd, "sp": nc.sync}
        self.sem = {}
        self.cnt = {}
        self.nsem = 0
        self.final_val = {}
        for e in self.eng:
            self.sem[e] = self._newsem("e_" + e)
            self.cnt[e] = 0
        self.waited = {e: {} for e in self.eng}
        self.dq = ("sp", "pool")
        self.dsem = {q: [self._newsem(f"d_{q}{i}") for i in range(NDS)] for q in self.dq}
        self.dval = {q: [0] * NDS for q in self.dq}
        self.dnext = {q: 0 for q in self.dq}
        self.ninst = 0
        self.nt = 0

    def _newsem(self, name):
        self.nsem += 1
        s = self.es.enter_context(self.nc.semaphore(f"{name}_{self.nsem}"))
        self.final_val[s] = 0
        return s

    def _waitv(self, e, s, v):
        if v <= 0 or self.waited[e].get(s, 0) >= v:
            return
        self.eng[e].wait_ge(s, v)
        self.waited[e][s] = v

    def _deps(self, e, r, w):
        need = {}
        for d in r:
            if d.lw is not None:
                s, v = d.lw
                if need.get(s, 0) < v:
                    need[s] = v
        for d in w:
            if d.lw is not None:
                s, v = d.lw
                if need.get(s, 0) < v:
                    need[s] = v
            for s, v in d.rd.items():
                if need.get(s, 0) < v:
                    need[s] = v
        for s, v in need.items():
            if e == "pe" and s is self.sem["pe"]:
                continue
            self._waitv(e, s, v)

    @staticmethod
    def _dl(x):
        return [a.d if isinstance(a, Tl) else a for a in x]

    def op(self, e, fn, r=(), w=()):
        r = self._dl(r)
        w = self._dl(w)
        if self.cnt[e] >= ROT:
            self.sem[e] = self._newsem("e_" + e)
            self.cnt[e] = 0
        self._deps(e, r, w)
        ins = fn(self.eng[e])
        self.cnt[e] += 1
        s = self.sem[e]
        ins.then_inc(s, 1)
        self.final_val[s] = self.cnt[e]
        self.ninst += 1
        for d in r:
            d.rd[s] = self.cnt[e]
        for d in w:
            d.lw = (s, self.cnt[e])
            d.rd = {}

    def dma(self, q, out, in_, r=(), w=()):
        r = self._dl(r)
        w = self._dl(w)
        i = self.dnext[q]
        self.dnext[q] = (i + 1) % NDS
        s = self.dsem[q][i]
        self._waitv(q, s, self.dval[q][i])
        self._deps(q, r, w)
        ins = self.eng[q].dma_start(out=out, in_=in_)
        self.dval[q][i] += 16
        v = self.dval[q][i]
        ins.then_inc(s, 16)
        self.final_val[s] = v
        self.ninst += 1
        for d in r:
            d.rd[s] = v
        for d in w:
            d.lw = (s, v)
            d.rd = {}

    def barrier(self):
        for e in self.eng:
            for s, v in self.final_val.items():
                self._waitv(e, s, v)

    def T(self, es, shape, dt, name=None):
        self.nt += 1
        t = es.enter_context(self.nc.sbuf_tensor(f"{name or 't'}_{self.nt}", list(shape), dt))
        return Tl(t)


class Rot:
    def __init__(self, items):
        self.items = items
        self.i = 0

    def next(self):
        x = self.items[self.i % len(self.items)]
        self.i += 1
        return x


def t5_bucket_np(dist):
    nb, md = 32, 2048
    me = nb // 2
    d = np.maximum(dist, 0)
    df = np.maximum(d, 1).astype(np.float32)
    large = me + (np.log(df / me) / math.log(md / me) * (nb - me)).astype(np.int32)
    large = np.minimum(large, nb - 1)
    return np.where(d < me, d, large)


DIL_PAIRS = ((128, 1), (512, 4), (2048, 16))


def toep_geom(emin, emax):
    dmin = 256 * emin - 127 - 128
    dmax = 256 * (emax + 3) + 127 + 128
    return dmin, dmax - dmin + 1


def dil_evals(window, part_shift_opts=(-128, 128)):
    own, oth = [], []
    for e in range(-3, 16):
        ok_own = ok_oth = False
        for a in range(4):
            lo = 256 * (a + e) - 127
            hi = 256 * (a + e) + 127
            if hi >= 0 and lo <= window:
                ok_own = True
            for sh in part_shift_opts:
                if hi + sh >= 0 and lo + sh <= window:
                    ok_oth = True
        if ok_own:
            own.append(e)
        if ok_oth:
            oth.append(e)
    return own, oth


FOX_E = [-3, -2, -1, 0]
DSA_NEAR_E = list(range(-3, 10))


def true_tile(st, hf, NH):
    return 2 * st + hf if st < NH else 2 * (st - NH) + 1 - hf


def sigma_perm(S, hf):
    NT = S // 128
    NH = NT // 2
    idx = []
    for st in range(NT):
        T = true_tile(st, hf, NH)
        idx.extend(range(T * 128, T * 128 + 128))
    return np.array(idx)


def fox_R(hf):
    tpos = np.zeros(1024, np.int64)
    for tt in range(8):
        T = (2 * tt + hf) if tt < 4 else (2 * (tt - 4) + 1 - hf)
        tpos[tt * 128:(tt + 1) * 128] = T * 128 + np.arange(128)
    R = (tpos[:, None] <= tpos[None, :]).astype(np.float32)
    return R.reshape(8, 128, 1024)


def build_layer(layer, S, dbg=False, stop_after=99, cut='', ctx=None, tag='', shared=None, xT_in=None, out_ap=None,
                pervar=()):
    NT = S // 128
    NH = NT // 2
    NQ = NH // 4
    NG = S // 512
    SO = S // 2
    if ctx is None:
        nc = bass.Bass("TRN2", target_bir_lowering=False)
        es = ExitStack()
        kb = KB(nc, es)
        PS = [Tl(es.enter_context(nc.psum_tensor(f"ps{i}", [128, 512], F32))) for i in range(7)]
    else:
        nc, es, kb, PS = ctx
    if shared is None:
        shared = {}

    def din(name, shape):
        key = (tag + name) if (name in pervar or not tag) else ("w%d_" % layer + name)
        if key not in shared:
            shared[key] = nc.dram_tensor(key, list(shape), F32, kind="ExternalInput").ap()
        return shared[key]

    def dscr(name, shape, dt):
        return nc.dram_tensor(tag + name, list(shape), dt, kind=("ExternalOutput" if dbg else "Internal")).ap()

    WIN = 3848 if layer == 0 else 1864
    xT = xT_in if xT_in is not None else din("xT", [D, S])
    ccol = din("ccol", [128, 8])
    ada_w = din("ada_w", [D, 6 * D])
    adab = din("adab", [128, 48])
    n1col = din("n1col", [128, 8])
    n2col = din("n2col", [128, 8])
    w_in = din("w_in", [D, WIN])
    NMO = 768 if layer == 0 else 1024
    w_out = din("w_out", [NMO, D])
    rw = din("rw", [D, NE])
    rbb = din("rbb", [128, NE])
    w1 = din("w1", [NE, D, 2 * D])
    b1col = din("b1col", [128, NE * 16])
    w2 = din("w2", [NE, D, D])
    b2 = din("b2", [NE, D])
    identD = din("ident", [128, 128])
    selD = din("sel", [NE, NE * 128])
    rbflat = din("rbflat", [128, 512])
    if layer == 0:
        fbb = din("fbb", [128, 8])
        RD = din("R", [8, 128, 1024])
        dgeo = []
        for (w_, r_) in DIL_PAIRS:
            eo, et = dil_evals(w_)
            dgeo.append((eo, et) + toep_geom(min(eo + et), max(eo + et)))
        fgeo = toep_geom(-3, 0)
        FcD = din("Fc", [2, fgeo[1]])
        FdD = [din(f"Fd{g}", [8, dgeo[g][3]]) for g in range(3)]
        FcR = dscr("FcR", [2, 128, fgeo[1]], F32)
        FdR = [dscr(f"FdR{g}", [8, 128, dgeo[g][3]], F32) for g in range(3)]
    else:
        kvn = din("kvn", [128, 2])
        w_ukv = din("w_ukv", [256, 2048])
        fnc = din("fncol", [128, 8])
        trimD = din("trim", [128, 128])
        othdD = din("othd", [128, 128])
        pow2D = din("pow2", [128, 32])
        sgeo = toep_geom(-3, DSA_NEAR_E[-1])
        FsD = din("Fs", [32, sgeo[1]])
        FsR = dscr("FsR", [32, 128, sgeo[1]], F32)
        rbfar = din("rbfar", [128, 16])
    outD = out_ap if out_ap is not None else nc.dram_tensor(tag + "out", [D, SO], F32, kind="ExternalOutput").ap()

    MT = dscr("MT", [NMO, SO], BF16)
    X1 = dscr("X1", [D, SO], F32)
    H2 = dscr("H2", [D, SO], BF16)
    GTs = dscr("GTs", [NE, SO], F32)
    if layer == 0:
        KAF = dscr("KAF", [8, 72, S], BF16)
        QAF = dscr("QAF", [8, 72, SO], BF16)
        VF = dscr("VF", [8, 128, NT, 128], BF16)
        KAD = dscr("KAD", [12, 72, S], BF16)
        QAD = dscr("QAD", [12, 72, SO], BF16)
        VD = dscr("VD", [12, 128, NT, 128], BF16)
    else:
        KAS = dscr("KAS", [16, 72, S], BF16)
        QAS = dscr("QAS", [16, 72, SO], BF16)
        VS = dscr("VS", [16, 128, NT, 128], BF16)
        KI = dscr("KI", [128, S], BF16)
        QI = dscr("QI", [512, SO], BF16)
        WI = dscr("WI", [SO, 128], F32)
        NM = dscr("NM", [NQ, NT, 128, 512], BF16)


    kb.PS = PS
    kb.shared = shared
    if not hasattr(kb, "P0t"):
        P0 = ExitStack()
        es.enter_context(P0)
        identf = kb.T(P0, [128, 128], F32)
        identb = kb.T(P0, [128, 128], BF16)
        onesf = kb.T(P0, [128, 128], F32)
        onesb = kb.T(P0, [128, 512], BF16)
        ind2 = kb.T(P0, [128, 2], BF16)
        mods = kb.T(P0, [128, 48], F32)
        gs1 = kb.T(P0, [128, 8], F32)
        gs2 = kb.T(P0, [128, 8], F32)
        bmax = kb.T(P0, [128, 1], F32)
        zcol = kb.T(P0, [128, 1], F32)
        kb.op("dve", lambda e: e.memset(zcol.t[:], 0.0), w=[zcol])
        kb.dma("sp", identf.t[:], identD[:, :], w=[identf])
        kb.op("dve", lambda e: e.tensor_copy(out=identb.t[:], in_=identf.t[:]), r=[identf], w=[identb])
        kb.op("dve", lambda e: e.memset(onesf.t[:], 1.0), w=[onesf])
        kb.op("dve", lambda e: e.memset(onesb.t[:], 1.0), w=[onesb])
        kb.op("dve", lambda e: e.memset(ind2.t[:], 0.0), w=[ind2])
        kb.op("dve", lambda e: e.memset(ind2.t[0:64, 0:1], 1.0), w=[ind2])
        kb.op("dve", lambda e: e.memset(ind2.t[64:128, 1:2], 1.0), w=[ind2])
        kb.P0t = (identf, identb, onesf, onesb, ind2, mods, gs1, gs2, bmax, zcol)
    identf, identb, onesf, onesb, ind2, mods, gs1, gs2, bmax, zcol = kb.P0t

    with ExitStack() as ph:
        cc = kb.T(ph, [128, 8], F32)
        sc = kb.T(ph, [128, 8], F32)
        ab = kb.T(ph, [128, 48], F32)
        n1 = kb.T(ph, [128, 8], F32)
        n2 = kb.T(ph, [128, 8], F32)
        rbf = kb.T(ph, [128, 512], F32)
        kb.dma("sp", cc.t[:], ccol[:, :], w=[cc])
        kb.dma("sp", ab.t[:], adab[:, :], w=[ab])
        kb.dma("sp", n1.t[:], n1col[:, :], w=[n1])
        kb.dma("sp", n2.t[:], n2col[:, :], w=[n2])
        kb.dma("sp", rbf.t[:], rbflat[:, :], w=[rbf])
        kb.op("dve", lambda e: e.reduce_max(out=bmax.t[:], in_=rbf.t[:], axis=AX.X), r=[rbf], w=[bmax])
        kb.op("act", lambda e: e.activation(out=sc.t[:], in_=cc.t[:], func=AF.Silu), r=[cc], w=[sc])
        aw = Rot([kb.T(ph, [128, 8, 1024], F32) for _ in range(2)])
        awv = ada_w.rearrange("(k p) f -> p k f", p=128)
        psm = PS[0]
        for j in range(6):
            a = aw.next()
            kb.dma("sp", a.t[:], awv[:, :, j * 1024:(j + 1) * 1024], w=[a])
            for fc in range(8):
                for k in range(8):
                    kb.op("pe", lambda e, a=a, fc=fc, k=k, j=j: e.matmul(
                        psm.t[:, j * 8 + fc:j * 8 + fc + 1], lhsT=a.t[:, k, fc * 128:(fc + 1) * 128],
                        rhs=sc.t[:, k:k + 1], start=(k == 0), stop=(k == 7)), r=[a, sc], w=[psm])
        kb.op("dve", lambda e: e.tensor_tensor(out=mods.t[:], in0=psm.t[:, 0:48], in1=ab.t[:], op=ALU.add),
              r=[psm, ab], w=[mods])
        kb.op("dve", lambda e: e.scalar_tensor_tensor(out=gs1.t[:], in0=mods.t[:, 8:16], scalar=1.0, in1=n1.t[:],
                                                     op0=ALU.add, op1=ALU.mult), r=[mods, n1], w=[gs1])
        kb.op("dve", lambda e: e.scalar_tensor_tensor(out=gs2.t[:], in0=mods.t[:, 32:40], scalar=1.0, in1=n2.t[:],
                                                     op0=ALU.add, op1=ALU.mult), r=[mods, n2], w=[gs2])
        kb.barrier()
    SH1, GT1, SH2, GT2 = 0, 16, 24, 40
    if stop_after <= 0:
        return nc, es, kb

    def rms_mod(ph_tiles, xin, gs, sh_off, hout, hf32=None):
        sqr, rstd, tmpr, pss = ph_tiles
        ps = pss
        for k in range(8):
            sq = sqr.next()
            kb.op("act", lambda e, sq=sq, k=k: e.activation(out=sq.t[:], in_=xin.t[:, k, :], func=AF.Square),
                  r=[xin], w=[sq])
            kb.op("pe", lambda e, sq=sq, k=k: e.matmul(ps.t[:], lhsT=onesf.t[:], rhs=sq.t[:], start=(k == 0),
                                                      stop=(k == 7)), r=[sq, onesf], w=[ps])
        kb.op("dve", lambda e: e.tensor_scalar(out=rstd.t[:], in0=ps.t[:], scalar1=1.0 / D, scalar2=EPS,
                                               op0=ALU.mult, op1=ALU.add), r=[ps], w=[rstd])
        kb.op("act", lambda e: e.activation(out=rstd.t[:], in_=rstd.t[:], func=AF.Sqrt), r=[rstd], w=[rstd])
        kb.op("dve", lambda e: e.reciprocal(out=rstd.t[:], in_=rstd.t[:]), r=[rstd], w=[rstd])
        for k in range(8):
            tm = tmpr.next()
            kb.op("dve", lambda e, tm=tm, k=k: e.tensor_tensor(out=tm.t[:], in0=xin.t[:, k, :], in1=rstd.t[:],
                                                               op=ALU.mult), r=[xin, rstd], w=[tm])
            if hf32 is not None:
                kb.op("act", lambda e, tm=tm, k=k: e.activation(
                    out=hf32.t[:, k, :], in_=tm.t[:], func=AF.Identity, scale=gs.t[:, k:k + 1],
                    bias=mods.t[:, sh_off + k:sh_off + k + 1]), r=[tm, gs, mods], w=[hf32])
                kb.op("pool", lambda e, k=k: e.tensor_copy(out=hout.t[:, k, :], in_=hf32.t[:, k, :]),
                      r=[hf32], w=[hout])
            else:
                kb.op("act", lambda e, tm=tm, k=k: e.activation(
                    out=hout.t[:, k, :], in_=tm.t[:], func=AF.Identity, scale=gs.t[:, k:k + 1],
                    bias=mods.t[:, sh_off + k:sh_off + k + 1]), r=[tm, gs, mods], w=[hout])

    xTv = xT.rearrange("(k p) t -> p k t", p=128)

    def projT(h, w, c0, ps, ncols=128):
        for k in range(8):
            kb.op("pe", lambda e, k=k: e.matmul(ps.t[0:ncols, :], lhsT=w.t[:, k, c0:c0 + ncols], rhs=h.t[:, k, :],
                                               start=(k == 0), stop=(k == 7)), r=[h, w], w=[ps])

    def projTok(h, j, w, c0, n, ps):
        for k in range(8):
            kb.op("pe", lambda e, k=k: e.matmul(ps.t[:, 0:n], lhsT=h.t[:, k, j * 128:(j + 1) * 128],
                                               rhs=w.t[:, k, c0:c0 + n], start=(k == 0), stop=(k == 7)),
                  r=[h, w], w=[ps])

    with ExitStack() as ph:
        wsb = kb.T(ph, [128, 8, WIN], BF16)
        wv = w_in.rearrange("(k p) n -> p k n", p=128)
        for k in range(8):
            kb.dma("pool", wsb.t[:, k, :], wv[:, k, :], w=[wsb])
        xgr = Rot([kb.T(ph, [128, 8, 512], F32) for _ in range(2)])
        hr = Rot([kb.T(ph, [128, 8, 512], BF16) for _ in range(2)])
        sqr = Rot([kb.T(ph, [128, 512], F32) for _ in range(2)])
        tmpr = Rot([kb.T(ph, [128, 512], F32) for _ in range(2)])
        rstd = kb.T(ph, [128, 512], F32)
        rmt = (sqr, rstd, tmpr, PS[0])
        stg = Rot([kb.T(ph, [128, 512], BF16) for _ in range(3)])
        sqb = Rot([kb.T(ph, [128, 512], BF16) for _ in range(2)])
        NVH = 20 if layer == 0 else 16
        psA = Rot([PS[1], PS[2]])
        psN = PS[3]
        psV = Rot([PS[4], PS[5]])
        psC = PS[6]
        NKN = 16
        kn = kb.T(ph, [128, NKN], F32)
        kms = kb.T(ph, [128, NKN], F32)
        tm2 = kb.T(ph, [128, 1], F32)
        kb.op("dve", lambda e: e.memset(kn.t[:], 0.0), w=[kn])

        def head_pair_K(h, c0, KA, hd0, col0, knc, scale=None):
            ps = psA.next()
            projT(h, wsb, c0, ps)
            s = stg.next()
            kb.op("act", lambda e: e.activation(out=s.t[:], in_=ps.t[:], func=AF.Copy,
                                                scale=(1.0 if scale is None else scale)), r=[ps], w=[s])
            q = sqb.next()
            kb.op("act", lambda e: e.activation(out=q.t[:], in_=ps.t[:], func=AF.Square), r=[ps], w=[q])
            kb.dma("sp", KA[hd0, 0:64, col0:col0 + 512], s.t[0:64, :], r=[s])
            kb.dma("sp", KA[hd0 + 1, 0:64, col0:col0 + 512], s.t[64:128, :], r=[s])
            kb.op("pe", lambda e: e.matmul(psN.t[0:2, :], lhsT=ind2.t[:, 0:2], rhs=q.t[:], start=True, stop=True),
                  r=[q, ind2], w=[psN])
            return ps

        def kn_update(knc):
            kb.op("dve", lambda e: e.reduce_max(out=tm2.t[0:2, :], in_=psN.t[0:2, :], axis=AX.X), r=[psN], w=[tm2])
            kb.op("dve", lambda e: e.tensor_max(out=kn.t[0:2, knc:knc + 1], in0=kn.t[0:2, knc:knc + 1],
                                                in1=tm2.t[0:2, :]), r=[kn, tm2], w=[kn])

        def tokV(h, c0, n, sv, h0, j):
            for (o, m) in ([(0, min(512, n))] + ([(512, n - 512)] if n > 512 else [])):
                ps = psV.next()
                projTok(h, j, wsb, c0 + o, m, ps)
                nh = m // 64
                hh = h0 + o // 64
                kb.op("dve", lambda e, ps=ps, m=m, nh=nh, hh=hh: e.tensor_copy(
                    out=sv.t[:, hh:hh + nh, j, 0:64], in_=ps.t[:, 0:m].rearrange("p (h c) -> p h c", h=nh)),
                    r=[ps], w=[sv])

        def flushV(sv, VDst, h0, nh, sg):
            for hh in range(nh):
                kb.dma("sp", VDst[hh, :, sg * 4:(sg + 1) * 4, :], sv.t[:, h0 + hh, :, :], r=[sv])

        if layer == 0:
            Lall = kb.T(ph, [128, NT, 8], F32)
            fb = kb.T(ph, [128, 8], F32)
            kb.dma("sp", fb.t[:], fbb[:, :], w=[fb])
            zt = kb.T(ph, [128, 8], F32)
            for p_ in range(2):
                kb.dma("sp", FcR[p_, :, :], bass.AP(tensor=FcD.tensor, offset=p_ * fgeo[1], ap=[[0, 128], [1, fgeo[1]]]))
            for g in range(3):
                W_ = dgeo[g][3]
                for q_ in range(8):
                    kb.dma("sp", FdR[g][q_, :, :], bass.AP(tensor=FdD[g].tensor, offset=q_ * W_, ap=[[0, 128], [1, W_]]))
            p1 = ExitStack()
            stv = Rot([kb.T(p1, [128, NVH, 4, 128], BF16) for _ in range(1)])
            for v_ in stv.items:
                kb.op("pool", lambda e, v_=v_: e.memset(v_.t[:, :, :, 64:128], 1.0), w=[v_])
            for sg in range(NG):
                xg = xgr.next()
                kb.dma("sp", xg.t[:], xTv[:, :, sg * 512:(sg + 1) * 512], w=[xg])
                h = hr.next()
                rms_mod(rmt, xg, gs1, SH1, h)
                for c4 in range(4):
                    head_pair_K(h, 512 + 128 * c4, KAF, 2 * c4, sg * 512, c4)
                    kn_update(c4)
                for c6 in range(6):
                    head_pair_K(h, 2312 + 128 * c6, KAD, 2 * c6, sg * 512, 4 + c6)
                    kn_update(4 + c6)
                sv = stv.next()
                for j in range(4):
                    tokV(h, 1024, 512, sv, 0, j)
                    tokV(h, 3080, 768, sv, 8, j)
                flushV(sv, VF, 0, 8, sg)
                flushV(sv, VD, 8, 12, sg)
                for j in range(4):
                    projTok(h, j, wsb, 1536, 8, psC)
                    kb.op("dve", lambda e: e.tensor_tensor(out=zt.t[:], in0=psC.t[:, 0:8], in1=fb.t[:], op=ALU.add),
                          r=[psC, fb], w=[zt])
                    kb.op("act", lambda e: e.activation(out=zt.t[:], in_=zt.t[:], func=AF.Exp, scale=-1.0), r=[zt], w=[zt])
                    kb.op("act", lambda e, j=j, sg=sg: e.activation(out=Lall.t[:, sg * 4 + j, :], in_=zt.t[:], func=AF.Ln,
                                                                    bias=1.0), r=[zt], w=[Lall])
                kb.dma("sp", KAF[0:8, 64, sg * 512:(sg + 1) * 512], onesb.t[0:8, :], r=[onesb])
                kb.dma("sp", KAF[0:8, 65, sg * 512:(sg + 1) * 512], onesb.t[0:8, :], r=[onesb])
                kb.dma("sp", KAF[0:8, 66, sg * 512:(sg + 1) * 512], onesb.t[0:8, :], r=[onesb])
                kb.dma("sp", KAF[0:8, 70, sg * 512:(sg + 1) * 512], onesb.t[0:8, :], r=[onesb])
                kb.dma("sp", KAD[0:12, 64, sg * 512:(sg + 1) * 512], onesb.t[0:12, :], r=[onesb])
                if sg < NQ:
                    for rr in (67, 68, 69):
                        kb.dma("sp", QAF[0:8, rr, sg * 512:(sg + 1) * 512], onesb.t[0:8, :], r=[onesb])
            kb.barrier()
            p1.close()
            p1b = ExitStack()
            Rsb = kb.T(p1b, [128, 8, 1024], F32)
            kb.dma("sp", Rsb.t[:], RD.rearrange("a p c -> p a c"), w=[Rsb])
            carry = kb.T(p1b, [128, 1], F32)
            kb.op("dve", lambda e: e.memset(carry.t[:], 0.0), w=[carry])
            Cg = kb.T(p1b, [128, 512], F32)
            r1 = kb.T(p1b, [128, 512], F32)
            cbr = Rot([kb.T(p1b, [128, 512], BF16) for _ in range(4)])
            for i in range(NQ):
                tiles = [4 * i + a for a in range(4)] + [NH + 4 * i + a for a in range(4)]
                for half in range(2):
                    for tt in range(8):
                        kb.op("pe", lambda e, tt=tt, half=half: e.matmul(
                            psC.t[0:8, :], lhsT=Lall.t[:, tiles[tt], :], rhs=Rsb.t[:, tt, half * 512:(half + 1) * 512],
                            start=(tt == 0), stop=(tt == 7)), r=[Lall, Rsb], w=[psC])
                    kb.op("dve", lambda e: e.tensor_scalar(out=Cg.t[0:8, :], in0=psC.t[0:8, :], scalar1=carry.t[0:8, 0:1],
                                                           scalar2=None, op0=ALU.add), r=[psC, carry], w=[Cg])
                    cur = Cg
                    scol = (i * 512) if half == 0 else (SO + i * 512)
                    for p3 in range(3):
                        cb = cbr.next()
                        kb.op("dve", lambda e, cur=cur, cb=cb: e.tensor_copy(out=cb.t[0:8, :], in_=cur.t[0:8, :]),
                              r=[cur], w=[cb])
                        kb.dma("sp", KAF[0:8, 67 + p3, scol:scol + 512], cb.t[0:8, :], r=[cb])
                        if half == 0:
                            nb = cbr.next()
                            kb.op("dve", lambda e, cb=cb, nb=nb: e.tensor_scalar(
                                out=nb.t[0:8, :], in0=cb.t[0:8, :], scalar1=-1.0, scalar2=None, op0=ALU.mult),
                                r=[cb], w=[nb])
                            kb.dma("sp", QAF[0:8, 64 + p3, i * 512:(i + 1) * 512], nb.t[0:8, :], r=[nb])
                        if p3 < 2:
                            kb.op("dve", lambda e, cur=cur, cb=cb: e.tensor_tensor(
                                out=r1.t[0:8, :], in0=cur.t[0:8, :], in1=cb.t[0:8, :], op=ALU.subtract),
                                r=[cur, cb], w=[r1])
                            cur = r1
                for tt in range(8):
                    kb.op("pe", lambda e, tt=tt: e.matmul(psC.t[0:8, 0:1], lhsT=Lall.t[:, tiles[tt], :],
                                                         rhs=onesf.t[:, 0:1], start=(tt == 0), stop=(tt == 7)),
                          r=[Lall, onesf], w=[psC])
                kb.op("dve", lambda e: e.tensor_tensor(out=carry.t[0:8, :], in0=carry.t[0:8, :], in1=psC.t[0:8, 0:1],
                                                       op=ALU.add), r=[carry, psC], w=[carry])
            kb.barrier()
            p1b.close()
        else:
            wu = kb.T(ph, [128, 2, 2048], BF16)
            wuv = w_ukv.rearrange("(k p) n -> p k n", p=128)
            for k in range(0 if 'U' in cut else 2):
                kb.dma("pool", wu.t[:, k, :], wuv[:, k, :], w=[wu])
            kvns = kb.T(ph, [128, 2], F32)
            if 'N' not in cut:
                kb.dma("sp", kvns.t[:], kvn[:, :], w=[kvns])
            ckf = Rot([kb.T(ph, [128, 2, 512], F32) for _ in range(2)])
            ckb = Rot([kb.T(ph, [128, 2, 512], BF16) for _ in range(2)])
            wki = kb.T(ph, [128, 8, 128], BF16)
            if 'W' not in cut:
                kb.op("dve", lambda e: e.tensor_copy(out=wki.t[:, :, 0:64], in_=wsb.t[:, :, 1792:1856]), r=[wsb], w=[wki])
                kb.op("dve", lambda e: e.tensor_copy(out=wki.t[:, :, 64:128], in_=wsb.t[:, :, 1792:1856]), r=[wsb], w=[wki])
            p1 = ExitStack()
            stv = Rot([kb.T(p1, [128, NVH, 4, 128], BF16) for _ in range(1)])
            for v_ in stv.items:
                kb.op("pool", lambda e, v_=v_: e.memset(v_.t[:, :, :, 64:128], 1.0), w=[v_])
            for sg in range(NG):
                xg = xgr.next()
                kb.dma("sp", xg.t[:], xTv[:, :, sg * 512:(sg + 1) * 512], w=[xg])
                h = hr.next()
                rms_mod(rmt, xg, gs1, SH1, h)
                cf = ckf.next()
                cb_ = ckb.next()
                pss = PS[0]
                if 'B' in cut:
                    continue
                qs_ = []
                for k2 in range(2):
                    ps = psA.next()
                    projT(h, wsb, 1024 + 128 * k2, ps)
                    kb.op("act", lambda e, ps=ps, k2=k2: e.activation(out=cf.t[:, k2, :], in_=ps.t[:], func=AF.Copy), r=[ps], w=[cf])
                    q = sqr.next()
                    kb.op("act", lambda e, q=q, ps=ps: e.activation(out=q.t[:], in_=ps.t[:], func=AF.Square), r=[ps], w=[q])
                    qs_.append(q)
                if 'P' in cut:
                    continue
                for k2 in range(2):
                    kb.op("pe", lambda e, k2=k2: e.matmul(pss.t[:], lhsT=onesf.t[:], rhs=qs_[k2].t[:], start=(k2 == 0),
                                                          stop=(k2 == 1)), r=[qs_[k2], onesf], w=[pss])
                if 'R' in cut:
                    continue
                kb.op("dve", lambda e: e.tensor_scalar(out=rstd.t[:], in0=pss.t[:], scalar1=1.0 / 256, scalar2=EPS,
                                                       op0=ALU.mult, op1=ALU.add), r=[pss], w=[rstd])
                kb.op("act", lambda e: e.activation(out=rstd.t[:], in_=rstd.t[:], func=AF.Sqrt), r=[rstd], w=[rstd])
                kb.op("dve", lambda e: e.reciprocal(out=rstd.t[:], in_=rstd.t[:]), r=[rstd], w=[rstd])
                if 'T' in cut:
                    continue
                for k2 in range(2):
                    tm = tmpr.next()
                    kb.op("dve", lambda e, tm=tm, k2=k2: e.tensor_tensor(out=tm.t[:], in0=cf.t[:, k2, :], in1=rstd.t[:],
                                                                         op=ALU.mult), r=[cf, rstd], w=[tm])
                    kb.op("act", lambda e, tm=tm, k2=k2: e.activation(out=cb_.t[:, k2, :], in_=tm.t[:], func=AF.Identity,
                                                                      scale=kvns.t[:, k2:k2 + 1], bias=zcol.t[:, 0:1]),
                          r=[tm, kvns, zcol], w=[cb_])
                for c8 in range(0 if 'C' in cut else 8):
                    ps = psA.next()
                    for k2 in range(2):
                        kb.op("pe", lambda e, k2=k2, c8=c8, ps=ps: e.matmul(
                            ps.t[:], lhsT=wu.t[:, k2, c8 * 128:(c8 + 1) * 128], rhs=cb_.t[:, k2, :],
                            start=(k2 == 0), stop=(k2 == 1)), r=[wu, cb_], w=[ps])
                    s = stg.next()
                    kb.op("act", lambda e, s=s, ps=ps: e.activation(out=s.t[:], in_=ps.t[:], func=AF.Copy), r=[ps], w=[s])
                    q = sqb.next()
                    kb.op("act", lambda e, q=q, ps=ps: e.activation(out=q.t[:], in_=ps.t[:], func=AF.Square), r=[ps], w=[q])
                    kb.dma("sp", KAS[2 * c8, 0:64, sg * 512:(sg + 1) * 512], s.t[0:64, :], r=[s])
                    kb.dma("sp", KAS[2 * c8 + 1, 0:64, sg * 512:(sg + 1) * 512], s.t[64:128, :], r=[s])
                    kb.op("pe", lambda e, q=q: e.matmul(psN.t[0:2, :], lhsT=ind2.t[:, 0:2], rhs=q.t[:], start=True,
                                                       stop=True), r=[q, ind2], w=[psN])
                    kn_update(c8)
                sv = stv.next()
                for j in range(0 if 'D' in cut else 4):
                    for o in (0, 512):
                        ps = psV.next()
                        for k2 in range(2):
                            kb.op("pe", lambda e, k2=k2, o=o, ps=ps, j=j: e.matmul(
                                ps.t[:], lhsT=cb_.t[:, k2, j * 128:(j + 1) * 128], rhs=wu.t[:, k2, 1024 + o:1536 + o],
                                start=(k2 == 0), stop=(k2 == 1)), r=[wu, cb_], w=[ps])
                        kb.op("dve", lambda e, o=o, ps=ps, j=j: e.tensor_copy(
                            out=sv.t[:, o // 64:o // 64 + 8, j, 0:64], in_=ps.t[:, :].rearrange("p (h c) -> p h c", h=8)),
                            r=[ps], w=[sv])
                if 'D' not in cut:
                    flushV(sv, VS, 0, 16, sg)
                if 'E' in cut:
                    continue
                ps = psA.next()
                projT(h, wki, 0, ps)
                s = stg.next()
                kb.op("act", lambda e, s=s, ps=ps: e.activation(out=s.t[:], in_=ps.t[:], func=AF.Copy), r=[ps], w=[s])
                kb.dma("sp", KI[:, sg * 512:(sg + 1) * 512], s.t[:, :], r=[s])
                kb.dma("sp", KAS[0:16, 64, sg * 512:(sg + 1) * 512], onesb.t[0:16, :], r=[onesb])
            kb.barrier()
            p1.close()

        kb.op("act", lambda e: e.activation(out=kms.t[0:2, :], in_=kn.t[0:2, :], func=AF.Sqrt), r=[kn], w=[kms])
        kb.op("dve", lambda e: e.tensor_scalar(out=kms.t[0:2, :], in0=kms.t[0:2, :], scalar1=0.125 * 1.05, scalar2=None,
                                               op0=ALU.mult), r=[kms], w=[kms])
        nqr = Rot([kb.T(ph, [128, 512], F32) for _ in range(2)])
        mgr = Rot([kb.T(ph, [128, 512], F32) for _ in range(4)])
        mrr = Rot([kb.T(ph, [128, 512], BF16) for _ in range(2)])

        def q_pair(h, c0, QA, hd0, col0):
            ps = psA.next()
            projT(h, wsb, c0, ps)
            s = stg.next()
            kb.op("act", lambda e: e.activation(out=s.t[:], in_=ps.t[:], func=AF.Copy, scale=0.125), r=[ps], w=[s])
            q = sqb.next()
            kb.op("act", lambda e: e.activation(out=q.t[:], in_=ps.t[:], func=AF.Square), r=[ps], w=[q])
            kb.dma("sp", QA[hd0, 0:64, col0:col0 + 512], s.t[0:64, :], r=[s])
            kb.dma("sp", QA[hd0 + 1, 0:64, col0:col0 + 512], s.t[64:128, :], r=[s])
            kb.op("pe", lambda e: e.matmul(psN.t[0:2, :], lhsT=ind2.t[:, 0:2], rhs=q.t[:], start=True, stop=True),
                  r=[q, ind2], w=[psN])
            nq = nqr.next()
            kb.op("act", lambda e: e.activation(out=nq.t[0:2, :], in_=psN.t[0:2, :], func=AF.Sqrt), r=[psN], w=[nq])
            return nq

        for i in range(NQ):
            xg = xgr.next()
            kb.dma("sp", xg.t[:], xTv[:, :, i * 512:(i + 1) * 512], w=[xg])
            h = hr.next()
            rms_mod(rmt, xg, gs1, SH1, h)
            c0s = i * 512
            if layer == 0:
                for c4 in range(4):
                    nq = q_pair(h, 128 * c4, QAF, 2 * c4, c0s)
                    mr = mrr.next()
                    kb.op("dve", lambda e, nq=nq, mr=mr, c4=c4: e.tensor_scalar(
                        out=mr.t[0:2, :], in0=nq.t[0:2, :], scalar1=kms.t[0:2, c4:c4 + 1], scalar2=-1.0,
                        op0=ALU.mult, op1=ALU.mult), r=[nq, kms], w=[mr])
                    kb.dma("sp", QAF[2 * c4, 70, c0s:c0s + 512], mr.t[0:1, :], r=[mr])
                    kb.dma("sp", QAF[2 * c4 + 1, 70, c0s:c0s + 512], mr.t[1:2, :], r=[mr])
                for sp in range(2):
                    mgs = []
                    for gp in range(3):
                        c6 = 2 * gp + sp
                        nq = q_pair(h, 1544 + 128 * c6, QAD, 2 * c6, c0s)
                        mg = mgr.next()
                        kb.op("dve", lambda e, nq=nq, mg=mg, c6=c6: e.tensor_scalar(
                            out=mg.t[0:2, :], in0=nq.t[0:2, :], scalar1=kms.t[0:2, 4 + c6:5 + c6], scalar2=None,
                            op0=ALU.mult), r=[nq, kms], w=[mg])
                        mgs.append(mg)
                    kb.op("dve", lambda e: e.tensor_max(out=mgs[0].t[0:2, :], in0=mgs[0].t[0:2, :], in1=mgs[1].t[0:2, :]),
                          r=[mgs[0], mgs[1]], w=[mgs[0]])
                    kb.op("dve", lambda e: e.tensor_max(out=mgs[0].t[0:2, :], in0=mgs[0].t[0:2, :], in1=mgs[2].t[0:2, :]),
                          r=[mgs[0], mgs[2]], w=[mgs[0]])
                    mr = mrr.next()
                    kb.op("dve", lambda e, mr=mr: e.tensor_scalar(out=mr.t[0:2, :], in0=mgs[0].t[0:2, :],
                                                                  scalar1=bmax.t[0:2, 0:1], scalar2=-1.0, op0=ALU.add,
                                                                  op1=ALU.mult), r=[mgs[0], bmax], w=[mr])
                    for gp in range(3):
                        c6 = 2 * gp + sp
                        kb.dma("sp", QAD[2 * c6, 64, c0s:c0s + 512], mr.t[0:1, :], r=[mr])
                        kb.dma("sp", QAD[2 * c6 + 1, 64, c0s:c0s + 512], mr.t[1:2, :], r=[mr])
            else:
                for c8 in range(0 if 'F' in cut else 8):
                    nq = q_pair(h, 128 * c8, QAS, 2 * c8, c0s)
                    mg = mgr.next()
                    kb.op("dve", lambda e, nq=nq, mg=mg, c8=c8: e.tensor_scalar(
                        out=mg.t[0:2, :], in0=nq.t[0:2, :], scalar1=kms.t[0:2, c8:c8 + 1], scalar2=None,
                        op0=ALU.mult), r=[nq, kms], w=[mg])
                    mr2 = mrr.next()
                    kb.op("dve", lambda e, mg=mg, mr2=mr2: e.tensor_scalar(
                        out=mr2.t[0:2, :], in0=mg.t[0:2, :], scalar1=bmax.t[0:2, 0:1], scalar2=-1.0, op0=ALU.add,
                        op1=ALU.mult), r=[mg, bmax], w=[mr2])
                    kb.dma("sp", QAS[2 * c8, 64, c0s:c0s + 512], mr2.t[0:1, :], r=[mr2])
                    kb.dma("sp", QAS[2 * c8 + 1, 64, c0s:c0s + 512], mr2.t[1:2, :], r=[mr2])
                for c4 in range(0 if 'G' in cut else 4):
                    ps = psA.next()
                    projT(h, wsb, 1280 + 128 * c4, ps)
                    s = stg.next()
                    kb.op("act", lambda e, s=s, ps=ps: e.activation(out=s.t[:], in_=ps.t[:], func=AF.Copy), r=[ps], w=[s])
                    kb.dma("sp", QI[c4 * 128:(c4 + 1) * 128, c0s:c0s + 512], s.t[:, :], r=[s])
                for j in range(0 if 'H' in cut else 4):
                    projTok(h, j, wsb, 1856, 8, psC)
                    wt = tmpr.next()
                    kb.op("dve", lambda e, wt=wt: e.tensor_copy(out=wt.t[:, 0:8], in_=psC.t[:, 0:8]), r=[psC], w=[wt])
                    kb.dma("sp", WI[c0s + j * 128:c0s + (j + 1) * 128, :], wt.t[:, 0:128], r=[wt])
        kb.barrier()

    if stop_after <= 1:
        return nc, es, kb
    def attention_head(ph, kaD, qaD, vD, vc0, Kd, blocks_fn, finalize, bufs, expbias=None, tail_fn=None):
        ka, qa, vp, psS, psO, pTr = bufs
        if not getattr(ka, "_pre", False):
            attn_load(ka, qa, vp, kaD, qaD, vD, Kd)
        ka._pre = False
        for i in range(NQ):
            blocks = blocks_fn(i)
            po = psO.next()
            nb = len(blocks)
            pend = []
            for n in range(nb + 2):
                if n < nb:
                    kt, masks, far = blocks[n]
                    if callable(masks):
                        masks = masks()
                    ps = psS.next()
                    nm_ = len(masks)
                    kb.op("pe", lambda e, kt=kt, ps=ps, nm_=nm_: e.matmul(
                        ps.t[:], lhsT=ka.t[0:Kd, kt * 128:(kt + 1) * 128], rhs=qa.t[0:Kd, i * 512:(i + 1) * 512],
                        start=True, stop=(nm_ == 0)), r=[ka, qa], w=[ps])
                    for mi, (lt, mk, mkap) in enumerate(masks):
                        kb.op("pe", lambda e, lt=lt, mkap=mkap, ps=ps, mi=mi, nm_=nm_: e.matmul(
                            ps.t[:], lhsT=lt.t[:], rhs=mkap, start=False, stop=(mi == nm_ - 1)), r=[lt, mk], w=[ps])
                    pT = pTr.next()
                    if far and expbias is not None:
                        kb.op("act", lambda e, pT=pT, ps=ps: e.activation(out=pT.t[:], in_=ps.t[:], func=AF.Exp,
                                                                          bias=expbias[1]), r=[ps, expbias[0]], w=[pT])
                    else:
                        kb.op("act", lambda e, pT=pT, ps=ps: e.activation(out=pT.t[:], in_=ps.t[:], func=AF.Exp),
                              r=[ps], w=[pT])
                    pend.append((kt, pT))
                if n >= 2:
                    kt, pT = pend[n - 2]
                    kb.op("pe", lambda e, kt=kt, pT=pT, n=n: e.matmul(
                        po.t[:], lhsT=vp.t[:, kt, :], rhs=pT.t[:], start=(n == 2),
                        stop=(n == nb + 1 and tail_fn is None)), r=[vp, pT], w=[po])
            if tail_fn is not None:
                tail_fn(i, po)
            finalize(i, po)

    def attn_load(ka, qa, vp, kaD, qaD, vD, Kd, pre=False):
        kb.dma("sp", ka.t[0:Kd, :], kaD[0:Kd, :], w=[ka])
        kb.dma("sp", qa.t[0:Kd, :], qaD[0:Kd, :], w=[qa])
        kb.dma("sp", vp.t[:], vD, w=[vp])
        ka._pre = pre

    def norm_write(ph_t, src_ps_or_sb, srcdeps, row0, i):
        rz, on = ph_t
        kb.op("dve", lambda e: e.reciprocal(out=rz.t[64:128, :], in_=src_ps_or_sb.t[64:128, :]), r=srcdeps, w=[rz])
        o = on.next()
        kb.op("dve", lambda e: e.tensor_tensor(out=o.t[0:64, :], in0=src_ps_or_sb.t[0:64, :], in1=rz.t[64:128, :],
                                               op=ALU.mult), r=srcdeps + [rz], w=[o])
        kb.dma("sp", MT[row0:row0 + 64, i * 512:(i + 1) * 512], o.t[0:64, :], r=[o])

    def toep_tile(mk, FR, idx, W_, dmin, e_):
        src = bass.AP(tensor=FR.tensor, offset=idx * 128 * W_ + 256 * e_ - dmin, ap=[[W_ - 1, 128], [256, 4], [1, 128]])
        kb.dma("pool", mk.t[:].rearrange("p (a c) -> p a c", a=4), src, w=[mk])

    with ExitStack() as ph:
        kar = Rot([kb.T(ph, [128, S], BF16) for _ in range(2)])
        qar = Rot([kb.T(ph, [128, SO], BF16) for _ in range(2)])
        vpr = Rot([kb.T(ph, [128, NT, 128], BF16) for _ in range(2)])
        psS = Rot([PS[0], PS[1], PS[2], PS[3]])
        psO = Rot([PS[4], PS[5]])
        pTr = Rot([kb.T(ph, [128, 512], BF16) for _ in range(4)])
        rz = kb.T(ph, [128, 512], F32)
        onr = Rot([kb.T(ph, [128, 512], BF16) for _ in range(2)])
        if layer == 0:
            cm = {}
            for part in range(2):
                for e_ in FOX_E:
                    mk = kb.T(ph, [128, 512], BF16)
                    toep_tile(mk, FcR, part, fgeo[1], fgeo[0], e_)
                    cm[(part, e_)] = mk

            def fox_blocks(i):
                bl = []
                for part in range(2):
                    for b in range(0, 4 * i + 4):
                        e_ = 4 * i - b
                        ms = [] if e_ >= 1 else [(identb, cm[(part, e_)], cm[(part, e_)].t[:])]
                        bl.append((b + part * NH, ms, False))
                return bl

            nxt_b = (kar.next(), qar.next(), vpr.next())
            attn_load(nxt_b[0], nxt_b[1], nxt_b[2], KAF[0], QAF[0], VF[0], 71, pre=True)
            for hd in range(8):
                cur_b = nxt_b
                if hd + 1 < 8:
                    nxt_b = (kar.next(), qar.next(), vpr.next())
                    attn_load(nxt_b[0], nxt_b[1], nxt_b[2], KAF[hd + 1], QAF[hd + 1], VF[hd + 1], 71, pre=True)
                bufs = cur_b + (psS, psO, pTr)
                attention_head(ph, KAF[hd], QAF[hd], VF[hd], 0, 71, fox_blocks,
                               lambda i, po, hd=hd: norm_write((rz, onr), po, [po], hd * 64, i), bufs)
            oacc = kb.T(ph, [128, SO], F32)
            tot = kb.T(ph, [128, 512], F32)
            for j in range(4):
                for g in range(3):
                    eo, et, dmin, W_ = dgeo[g]
                    hd = g * 4 + j
                    dm = {}
                    with ExitStack() as ph2:
                        for part, el in ((0, eo), (1, et)):
                            for e_ in el:
                                mk = kb.T(ph2, [128, 512], BF16)
                                toep_tile(mk, FdR[g], j * 2 + part, W_, dmin, e_)
                                dm[(part, e_)] = mk

                        def dil_blocks(i, eo=eo, et=et, dm=dm):
                            bl = []
                            for part, el in ((0, eo), (1, et)):
                                for e_ in el:
                                    b = 4 * i - e_
                                    if 0 <= b < NH:
                                        bl.append((b + part * NH, [(identb, dm[(part, e_)], dm[(part, e_)].t[:])], False))
                            return bl

                        def dil_fin(i, po, g=g, j=j):
                            sl = oacc.t[:, i * 512:(i + 1) * 512]
                            if g == 0:
                                kb.op("act", lambda e: e.activation(out=sl, in_=po.t[:], func=AF.Copy), r=[po], w=[oacc])
                            elif g == 1:
                                kb.op("dve", lambda e: e.tensor_tensor(out=sl, in0=po.t[:], in1=sl, op=ALU.add),
                                      r=[po, oacc], w=[oacc])
                            else:
                                norm_write((rz, onr), po, [po], 512 + j * 64, i)

                        def dil_tail(i, po):
                            kb.op("pe", lambda e: e.matmul(po.t[:], lhsT=identf.t[:], rhs=oacc.t[:, i * 512:(i + 1) * 512],
                                                           start=False, stop=True), r=[identf, oacc], w=[po])

                        bufs = (kar.next(), qar.next(), vpr.next(), psS, psO, pTr)
                        attention_head(ph2, KAD[hd], QAD[hd], VD[hd], 0, 65, dil_blocks, dil_fin, bufs,
                                       tail_fn=(dil_tail if g == 2 else None))
                        kb.barrier()
        if layer == 1:
            pass
        kb.barrier()
    if layer == 1:
        NIT = 20
        with ExitStack() as ph:
            pst = Tl(ph.enter_context(nc.psum_tensor(tag + "pst", [128, 1024], BF16)))
            qis = kb.T(ph, [128, 4, SO], BF16)
            kb.dma("sp", qis.t[:], QI.rearrange("(c p) t -> p c t", p=128), w=[qis])
            kis = kb.T(ph, [128, S], BF16)
            kb.dma("sp", kis.t[:], KI[:, :], w=[kis])
            sc = kb.T(ph, [128, S], F32)
            junk = kb.T(ph, [128, S], BF16)
            nmb = kb.T(ph, [128, S], BF16)
            stage = kb.T(ph, [128, NT, 512], BF16)
            rlr = Rot([kb.T(ph, [128, 512], F32) for _ in range(3)])
            wq = kb.T(ph, [128, 8], F32)
            trim = kb.T(ph, [128, 128], F32)
            othd = kb.T(ph, [128, 128], F32)
            pw2 = kb.T(ph, [128, 32], F32)
            kb.dma("sp", trim.t[:], trimD[:, :], w=[trim])
            kb.dma("sp", othd.t[:], othdD[:, :], w=[othd])
            kb.dma("sp", pw2.t[:], pow2D[:, :], w=[pw2])
            dhs = kb.T(ph, [128, 32], F32)
            cnt = kb.T(ph, [128, 32], F32)
            lo = kb.T(ph, [128, 1], F32)
            mid = kb.T(ph, [128, 1], F32)
            st4 = kb.T(ph, [128, 4], F32)
            psI = Rot([PS[0], PS[1], PS[2], PS[3]])
            for i in range(NQ):
                NCH = i + 1
                RW = 2 * NCH * 512
                for a4 in range(4):
                    aq = 4 * i + a4
                    kb.dma("sp", wq.t[:], WI[aq * 128:(aq + 1) * 128, 0:8], w=[wq])
                    for kc in range(NCH):
                        for part in range(2):
                            c0 = (2 * kc + part) * 512
                            k0 = part * SO + kc * 512
                            for hh in range(8):
                                pb = (hh % 2) * 64
                                ps = psI.next()
                                kb.op("pe", lambda e, hh=hh, pb=pb, ps=ps, k0=k0: e.matmul(
                                    ps.t[:], lhsT=qis.t[pb:pb + 64, hh // 2, aq * 128:(aq + 1) * 128],
                                    rhs=kis.t[pb:pb + 64, k0:k0 + 512], start=True, stop=True), r=[qis, kis], w=[ps])
                                rl = rlr.next()
                                kb.op("act", lambda e, rl=rl, ps=ps: e.activation(out=rl.t[:], in_=ps.t[:], func=AF.Relu),
                                      r=[ps], w=[rl])
                                if hh == 0:
                                    kb.op("dve", lambda e, rl=rl, c0=c0: e.tensor_scalar(
                                        out=sc.t[:, c0:c0 + 512], in0=rl.t[:], scalar1=wq.t[:, 0:1], scalar2=None,
                                        op0=ALU.mult), r=[rl, wq], w=[sc])
                                else:
                                    kb.op("dve", lambda e, rl=rl, c0=c0, hh=hh: e.scalar_tensor_tensor(
                                        out=sc.t[:, c0:c0 + 512], in0=rl.t[:], scalar=wq.t[:, hh:hh + 1],
                                        in1=sc.t[:, c0:c0 + 512], op0=ALU.mult, op1=ALU.add), r=[rl, wq, sc], w=[sc])
                    kb.op("dve", lambda e: e.tensor_reduce(out=st4.t[:, 0:1], in_=sc.t[:, 0:RW], axis=AX.X, op=ALU.max),
                          r=[sc], w=[st4])
                    kb.op("dve", lambda e: e.tensor_reduce(out=st4.t[:, 1:2], in_=sc.t[:, 0:RW], axis=AX.X, op=ALU.min),
                          r=[sc], w=[st4])
                    for part in range(2):
                        base = (2 * i + part) * 512
                        for j in range(4):
                            blk = sc.t[:, base + j * 128:base + (j + 1) * 128]
                            if j > a4:
                                kb.op("pool", lambda e, blk=blk: e.memset(blk, -1.0e9), w=[sc])
                            elif j == a4:
                                mt_ = trim if part == 0 else othd
                                kb.op("dve", lambda e, blk=blk, mt_=mt_: e.tensor_tensor(out=blk, in0=blk, in1=mt_.t[:],
                                                                                         op=ALU.add), r=[sc, mt_], w=[sc])
                    if aq == 0:
                        kb.op("dve", lambda e: e.memset(lo.t[:], -1.0e8), w=[lo])
                    else:
                        kb.op("dve", lambda e: e.tensor_tensor(out=st4.t[:, 2:3], in0=st4.t[:, 0:1], in1=st4.t[:, 1:2],
                                                               op=ALU.subtract), r=[st4], w=[st4])
                        kb.op("dve", lambda e: e.tensor_scalar(out=st4.t[:, 2:3], in0=st4.t[:, 2:3], scalar1=2.0, scalar2=None,
                                                               op0=ALU.add), r=[st4], w=[st4])
                        kb.op("dve", lambda e: e.tensor_scalar(out=dhs.t[:], in0=pw2.t[:], scalar1=st4.t[:, 2:3], scalar2=None,
                                                               op0=ALU.mult), r=[pw2, st4], w=[dhs])
                        kb.op("dve", lambda e: e.tensor_scalar(out=lo.t[:], in0=st4.t[:, 1:2], scalar1=-1.0, scalar2=None,
                                                               op0=ALU.add), r=[st4], w=[lo])
                        kb.op("dve", lambda e: e.memset(cnt.t[:], 0.0), w=[cnt])
                        for it in range(NIT):
                            kb.op("dve", lambda e, it=it: e.tensor_tensor(out=mid.t[:], in0=lo.t[:], in1=dhs.t[:, it:it + 1],
                                                                          op=ALU.add), r=[lo, dhs], w=[mid])
                            kb.op("dve", lambda e, it=it: e.tensor_scalar(
                                out=junk.t[:, 0:RW], in0=sc.t[:, 0:RW], scalar1=mid.t[:, 0:1], scalar2=0.0, op0=ALU.is_ge,
                                op1=ALU.add, accum_out=cnt.t[:, it:it + 1]), r=[sc, mid], w=[junk, cnt])
                            kb.op("dve", lambda e, it=it: e.scalar_tensor_tensor(
                                out=mid.t[:], in0=cnt.t[:, it:it + 1], scalar=255.5, in1=dhs.t[:, it:it + 1], op0=ALU.is_ge,
                                op1=ALU.mult), r=[cnt, dhs], w=[mid])
                            kb.op("dve", lambda e: e.tensor_tensor(out=lo.t[:], in0=lo.t[:], in1=mid.t[:], op=ALU.add),
                                  r=[lo, mid], w=[lo])
                    kb.op("dve", lambda e: e.tensor_scalar(out=nmb.t[:, 0:RW], in0=sc.t[:, 0:RW], scalar1=lo.t[:, 0:1],
                                                           scalar2=1.0, op0=ALU.is_ge, op1=ALU.subtract), r=[sc, lo], w=[nmb])
                    nblk = RW // 128
                    for b0 in range(0, nblk, 8):
                        nb_ = min(8, nblk - b0)
                        for bb in range(nb_):
                            kb.op("pe", lambda e, bb=bb, b0=b0: e.transpose(
                                out=pst.t[:, bb * 128:(bb + 1) * 128], in_=nmb.t[:, (b0 + bb) * 128:(b0 + bb + 1) * 128],
                                identity=identb.t[:]), r=[nmb, identb], w=[pst])
                        for half in range(nb_ // 4):
                            cb = (b0 // 4) + half
                            kt0 = (cb % 2) * NH + (cb // 2) * 4
                            kb.op("act", lambda e, half=half, kt0=kt0: e.activation(
                                out=stage.t[:, kt0:kt0 + 4, a4 * 128:(a4 + 1) * 128],
                                in_=pst.t[:, half * 512:(half + 1) * 512].rearrange("p (j c) -> p j c", j=4), func=AF.Copy),
                                r=[pst], w=[stage])
                for part in range(2):
                    kb.dma("sp", NM[i, part * NH:part * NH + 4 * NCH].rearrange("k p c -> p k c"),
                           stage.t[:, part * NH:part * NH + 4 * NCH, :], r=[stage])
            kb.barrier()

        with ExitStack() as ph:
            kar = Rot([kb.T(ph, [128, S], BF16) for _ in range(2)])
            qar = Rot([kb.T(ph, [128, SO], BF16) for _ in range(2)])
            vpr = Rot([kb.T(ph, [128, NT, 128], BF16) for _ in range(2)])
            psS = Rot([PS[0], PS[1], PS[2], PS[3]])
            psO = Rot([PS[4], PS[5]])
            pTr = Rot([kb.T(ph, [128, 512], BF16) for _ in range(4)])
            rz = kb.T(ph, [128, 512], F32)
            onr = Rot([kb.T(ph, [128, 512], BF16) for _ in range(2)])
            i3b = kb.T(ph, [128, 128], BF16)
            kb.op("dve", lambda e: e.tensor_scalar(out=i3b.t[:], in0=identf.t[:], scalar1=-NEG, scalar2=None, op0=ALU.mult),
                  r=[identf], w=[i3b])
            rbf_ = kb.T(ph, [128, 16], F32)
            kb.dma("sp", rbf_.t[:], rbfar[:, :], w=[rbf_])
            for q_ in range(32):
                W_ = sgeo[1]
                kb.dma("sp", FsR[q_, :, :], bass.AP(tensor=FsD.tensor, offset=q_ * W_, ap=[[0, 128], [1, W_]]))
            kb.barrier()
            nmr = Rot([kb.T(ph, [128, 4, 512], BF16) for _ in range(6)])
            bt = {}
            for part in range(2):
                for e_ in DSA_NEAR_E:
                    bt[(part, e_)] = kb.T(ph, [128, 512], BF16)
            for hd in range(16):
                for part in range(2):
                    for e_ in DSA_NEAR_E:
                        toep_tile(bt[(part, e_)], FsR, hd * 2 + part, sgeo[1], sgeo[0], e_)

                def dsa_blocks(i, hd=hd):
                    pieces = [(part, kc) for part in range(2) for kc in range(i + 1)]
                    tiles = {}

                    def load(pi):
                        if pi >= len(pieces) or pi in tiles:
                            return
                        part, kc = pieces[pi]
                        nt_ = nmr.next()
                        kb.dma("sp", nt_.t[:], NM[i, part * NH + 4 * kc:part * NH + 4 * kc + 4].rearrange("k p c -> p k c"),
                               w=[nt_])
                        tiles[pi] = nt_

                    bl = []
                    for pi, (part, kc) in enumerate(pieces):
                        for j in range(4):
                            b = 4 * kc + j
                            e_ = 4 * i - b
                            far = e_ > DSA_NEAR_E[-1]

                            def mk(pi=pi, j=j, part=part, e_=e_, far=far):
                                load(pi)
                                if j == 0:
                                    load(pi + 1)
                                    load(pi + 2)
                                nt_ = tiles[pi]
                                ms = [(i3b, nt_, nt_.t[:, j, :])]
                                if not far:
                                    ms.append((identb, bt[(part, e_)], bt[(part, e_)].t[:]))
                                return ms
                            bl.append((b + part * NH, mk, far))
                    return bl

                if hd == 0:
                    nxt_b = (kar.next(), qar.next(), vpr.next())
                    attn_load(nxt_b[0], nxt_b[1], nxt_b[2], KAS[0], QAS[0], VS[0], 65, pre=True)
                cur_b = nxt_b
                if hd + 1 < 16:
                    nxt_b = (kar.next(), qar.next(), vpr.next())
                    attn_load(nxt_b[0], nxt_b[1], nxt_b[2], KAS[hd + 1], QAS[hd + 1], VS[hd + 1], 65, pre=True)
                bufs = cur_b + (psS, psO, pTr)
                attention_head(ph, KAS[hd], QAS[hd], VS[hd], 0, 65, dsa_blocks,
                               lambda i, po, hd=hd: norm_write((rz, onr), po, [po], hd * 64, i), bufs,
                               expbias=(rbf_, rbf_.t[:, hd:hd + 1]))
            kb.barrier()
    if stop_after <= 2:
        return nc, es, kb

    KC = NMO // 128
    with ExitStack() as ph:
        wo = kb.T(ph, [128, KC, 1024], BF16)
        for kc in range(KC):
            kb.dma("pool", wo.t[:, kc, :], w_out[kc * 128:(kc + 1) * 128, :], w=[wo])
        rwf = kb.T(ph, [128, 8, NE], F32)
        kb.dma("sp", rwf.t[:], rw.rearrange("(k p) e -> p k e", p=128), w=[rwf])
        rbs = kb.T(ph, [128, NE], F32)
        kb.dma("sp", rbs.t[:], rbb[:, :], w=[rbs])
        xgr = Rot([kb.T(ph, [128, 8, 512], F32) for _ in range(2)])
        mtr = Rot([kb.T(ph, [128, KC, 512], BF16) for _ in range(2)])
        x1r = Rot([kb.T(ph, [128, 8, 512], F32) for _ in range(2)])
        h2f = kb.T(ph, [128, 8, 512], F32)
        h2r = Rot([kb.T(ph, [128, 8, 512], BF16) for _ in range(2)])
        sqr = Rot([kb.T(ph, [128, 512], F32) for _ in range(2)])
        tmpr = Rot([kb.T(ph, [128, 512], F32) for _ in range(2)])
        rstd = kb.T(ph, [128, 512], F32)
        rmt = (sqr, rstd, tmpr, PS[0])
        psA = Rot([PS[1], PS[2]])
        psL = Rot([PS[3], PS[4]])
        psT = PS[5]
        lg = kb.T(ph, [128, NE], F32)
        ex = kb.T(ph, [128, NE], F32)
        mk_ = kb.T(ph, [128, NE], F32)
        gg = kb.T(ph, [128, NE], F32)
        mx8 = kb.T(ph, [128, 8], F32)
        sm1 = kb.T(ph, [128, 2], F32)
        gTr = Rot([kb.T(ph, [128, 512], F32) for _ in range(2)])
        MTv = MT.rearrange("(k p) t -> p k t", p=128)
        X1v = X1.rearrange("(k p) t -> p k t", p=128)
        H2v = H2.rearrange("(k p) t -> p k t", p=128)
        for i in range(NQ):
            cs = slice(i * 512, (i + 1) * 512)
            xg = xgr.next()
            kb.dma("sp", xg.t[:], xTv[:, :, cs], w=[xg])
            mt = mtr.next()
            kb.dma("sp", mt.t[:], MTv[:, :, cs], w=[mt])
            x1 = x1r.next()
            for dc in range(8):
                ps = psA.next()
                for kc in range(KC):
                    kb.op("pe", lambda e, kc=kc, dc=dc, ps=ps: e.matmul(
                        ps.t[:], lhsT=wo.t[:, kc, dc * 128:(dc + 1) * 128], rhs=mt.t[:, kc, :], start=(kc == 0),
                        stop=(kc == KC - 1)), r=[wo, mt], w=[ps])
                kb.op("dve", lambda e, dc=dc, ps=ps: e.scalar_tensor_tensor(
                    out=x1.t[:, dc, :], in0=ps.t[:], scalar=mods.t[:, GT1 + dc:GT1 + dc + 1], in1=xg.t[:, dc, :],
                    op0=ALU.mult, op1=ALU.add), r=[ps, mods, xg], w=[x1])
            kb.dma("sp", X1v[:, :, cs], x1.t[:], r=[x1])
            h2 = h2r.next()
            rms_mod(rmt, x1, gs2, SH2, h2, hf32=h2f)
            kb.dma("sp", H2v[:, :, cs], h2.t[:], r=[h2])
            gT = gTr.next()
            for j in range(4):
                pl = psL.next()
                for k in range(8):
                    kb.op("pe", lambda e, k=k, j=j, pl=pl: e.matmul(
                        pl.t[:, 0:NE], lhsT=h2f.t[:, k, j * 128:(j + 1) * 128], rhs=rwf.t[:, k, :], start=(k == 0),
                        stop=(k == 7)), r=[h2f, rwf], w=[pl])
                kb.op("dve", lambda e, pl=pl: e.tensor_tensor(out=lg.t[:], in0=pl.t[:, 0:NE], in1=rbs.t[:], op=ALU.add),
                      r=[pl, rbs], w=[lg])
                kb.op("dve", lambda e: e.max(out=mx8.t[:], in_=lg.t[:]), r=[lg], w=[mx8])
                kb.op("dve", lambda e: e.tensor_scalar(out=sm1.t[:, 0:1], in0=mx8.t[:, 0:1], scalar1=-1.0, scalar2=None,
                                                       op0=ALU.mult), r=[mx8], w=[sm1])
                kb.op("act", lambda e: e.activation(out=ex.t[:], in_=lg.t[:], func=AF.Exp, bias=sm1.t[:, 0:1]),
                      r=[lg, sm1], w=[ex])
                kb.op("dve", lambda e: e.tensor_scalar(out=mk_.t[:], in0=lg.t[:], scalar1=mx8.t[:, 3:4], scalar2=None,
                                                       op0=ALU.is_ge), r=[lg, mx8], w=[mk_])
                kb.op("dve", lambda e: e.tensor_tensor(out=gg.t[:], in0=ex.t[:], in1=mk_.t[:], op=ALU.mult),
                      r=[ex, mk_], w=[gg])
                kb.op("dve", lambda e: e.reduce_sum(out=sm1.t[:, 1:2], in_=gg.t[:], axis=AX.X), r=[gg], w=[sm1])
                kb.op("dve", lambda e: e.reciprocal(out=sm1.t[:, 1:2], in_=sm1.t[:, 1:2]), r=[sm1], w=[sm1])
                kb.op("dve", lambda e: e.tensor_scalar(out=gg.t[:], in0=gg.t[:], scalar1=sm1.t[:, 1:2], scalar2=None,
                                                       op0=ALU.mult), r=[gg, sm1], w=[gg])
                kb.op("pe", lambda e: e.transpose(out=psT.t[0:NE, 0:128], in_=gg.t[:], identity=identf.t[:]),
                      r=[gg, identf], w=[psT])
                kb.op("act", lambda e, j=j: e.activation(out=gT.t[0:NE, j * 128:(j + 1) * 128], in_=psT.t[0:NE, 0:128],
                                                         func=AF.Copy), r=[psT], w=[gT])
            kb.dma("sp", GTs[:, cs], gT.t[0:NE, :], r=[gT])
        kb.barrier()
    if stop_after <= 3:
        return nc, es, kb

    P = min(1024, SO)
    NPG = P // 512
    outv = outD.rearrange("(k p) t -> p k t", p=128)
    with ExitStack() as ph:
        PS7 = Tl(ph.enter_context(nc.psum_tensor(tag + "ps7", [128, 512], F32)))
        b1c = kb.T(ph, [128, NE * 16], F32)
        kb.dma("sp", b1c.t[:], b1col[:, :], w=[b1c])
        b1p = kb.T(ph, [128, NE * 16], F32)
        kb.op("dve", lambda e: e.tensor_scalar(out=b1p.t[:], in0=b1c.t[:], scalar1=1.0, scalar2=None, op0=ALU.add),
              r=[b1c], w=[b1p])
        selb = kb.T(ph, [128, NE * 128], BF16)
        kb.dma("pool", selb.t[0:NE, :], selD[:, :], w=[selb])
        b2f = kb.T(ph, [128, D], F32)
        kb.dma("sp", b2f.t[0:NE, :], b2[:, :], w=[b2f])
        H2p = kb.T(ph, [128, 8, P], BF16)
        acc = kb.T(ph, [128, 8, P], F32)
        gtf = kb.T(ph, [128, P], F32)
        gtr_ = kb.T(ph, [128, P], F32)
        gth = kb.T(ph, [128, P], BF16)
        gtl = kb.T(ph, [128, P], BF16)
        if layer == 1:
            fns = kb.T(ph, [128, 8], F32)
            kb.dma("sp", fns.t[:], fnc[:, :], w=[fns])
        X1v = X1.rearrange("(k p) t -> p k t", p=128)
        H2v = H2.rearrange("(k p) t -> p k t", p=128)
        for pz in range(SO // P):
            c0 = pz * P
            kb.dma("sp", H2p.t[:], H2v[:, :, c0:c0 + P], w=[H2p])
            kb.dma("sp", gtf.t[0:NE, :], GTs[:, c0:c0 + P], w=[gtf])
            kb.op("dve", lambda e: e.tensor_copy(out=gth.t[0:NE, :], in_=gtf.t[0:NE, :]), r=[gtf], w=[gth])
            kb.op("dve", lambda e: e.tensor_tensor(out=gtr_.t[0:NE, :], in0=gtf.t[0:NE, :], in1=gth.t[0:NE, :],
                                                   op=ALU.subtract), r=[gtf, gth], w=[gtr_])
            kb.op("dve", lambda e: e.tensor_copy(out=gtl.t[0:NE, :], in_=gtr_.t[0:NE, :]), r=[gtr_], w=[gtl])
            with ExitStack() as pe_:
                w1r = Rot([kb.T(pe_, [128, 8, 2048], BF16) for _ in range(2)])
                w2r = Rot([kb.T(pe_, [128, 8, 1024], BF16) for _ in range(2)])
                glr = Rot([kb.T(pe_, [128, 512], F32) for _ in range(2)])
                sgr = Rot([kb.T(pe_, [128, 512], F32) for _ in range(2)])
                lir = Rot([kb.T(pe_, [128, 512], F32) for _ in range(2)])
                t1r = Rot([kb.T(pe_, [128, 512], F32) for _ in range(2)])
                Gsr = Rot([kb.T(pe_, [128, 512], F32) for _ in range(2)])
                abr = Rot([kb.T(pe_, [128, 512], BF16) for _ in range(10)])
                psg = Rot([PS[0], PS[1]])
                psl = Rot([PS[2], PS[3]])
                psG = PS[4]
                psy = Rot([PS[5], PS[6]])
                psB = PS7
                for gi in range(NPG):
                    cs = slice(gi * 512, (gi + 1) * 512)
                    for dc in range(8):
                        kb.op("pe", lambda e, dc=dc, cs=cs: e.matmul(
                            psB.t[:], lhsT=b2f.t[0:NE, dc * 128:(dc + 1) * 128], rhs=gtf.t[0:NE, cs], start=True,
                            stop=True), r=[b2f, gtf], w=[psB])
                        kb.op("act", lambda e, dc=dc, cs=cs: e.activation(out=acc.t[:, dc, cs], in_=psB.t[:], func=AF.Copy),
                              r=[psB], w=[acc])
                def load_w(x_):
                    a_ = w1r.next()
                    b_ = w2r.next()
                    kb.dma("pool", a_.t[:], w1[x_].rearrange("(k p) n -> p k n", p=128), w=[a_])
                    kb.dma("pool", b_.t[:], w2[x_].rearrange("(k p) n -> p k n", p=128), w=[b_])
                    return a_, b_

                nxt_w = load_w(0)
                for ex_ in range(NE):
                    w1e, w2e = nxt_w
                    if ex_ + 1 < NE:
                        nxt_w = load_w(ex_ + 1)
                    for gi in range(NPG):
                        cs = slice(gi * 512, (gi + 1) * 512)
                        kb.op("pe", lambda e, cs=cs: e.matmul(psG.t[:], lhsT=selb.t[0:NE, ex_ * 128:(ex_ + 1) * 128],
                                                              rhs=gth.t[0:NE, cs], start=True, stop=False),
                              r=[selb, gth], w=[psG])
                        kb.op("pe", lambda e, cs=cs: e.matmul(psG.t[:], lhsT=selb.t[0:NE, ex_ * 128:(ex_ + 1) * 128],
                                                              rhs=gtl.t[0:NE, cs], start=False, stop=True),
                              r=[selb, gtl], w=[psG])
                        Gs = Gsr.next()
                        kb.op("act", lambda e, Gs=Gs: e.activation(out=Gs.t[:], in_=psG.t[:], func=AF.Copy), r=[psG], w=[Gs])
                        acts = []
                        for hc in range(8):
                            pg = psg.next()
                            pl = psl.next()
                            for k in range(8):
                                kb.op("pe", lambda e, k=k, hc=hc, pg=pg, cs=cs: e.matmul(
                                    pg.t[:], lhsT=w1e.t[:, k, hc * 128:(hc + 1) * 128], rhs=H2p.t[:, k, cs],
                                    start=(k == 0), stop=(k == 7)), r=[w1e, H2p], w=[pg])
                            for k in range(8):
                                kb.op("pe", lambda e, k=k, hc=hc, pl=pl, cs=cs: e.matmul(
                                    pl.t[:], lhsT=w1e.t[:, k, 1024 + hc * 128:1024 + (hc + 1) * 128], rhs=H2p.t[:, k, cs],
                                    start=(k == 0), stop=(k == 7)), r=[w1e, H2p], w=[pl])
                            gl = glr.next()
                            sg_ = sgr.next()
                            li = lir.next()
                            t1 = t1r.next()
                            ab_ = abr.next()
                            bc = ex_ * 16 + hc
                            kb.op("dve", lambda e, gl=gl, pg=pg, bc=bc: e.tensor_scalar(
                                out=gl.t[:], in0=pg.t[:], scalar1=b1c.t[:, bc:bc + 1], scalar2=7.0, op0=ALU.add,
                                op1=ALU.min), r=[pg, b1c], w=[gl])
                            kb.op("act", lambda e, gl=gl, sg_=sg_: e.activation(out=sg_.t[:], in_=gl.t[:], func=AF.Sigmoid,
                                                                                scale=1.702), r=[gl], w=[sg_])
                            kb.op("act", lambda e, li=li, pl=pl, bc=bc: e.activation(
                                out=li.t[:], in_=pl.t[:], func=AF.Identity, bias=b1p.t[:, bc + 8:bc + 9]), r=[pl, b1p], w=[li])
                            kb.op("dve", lambda e, li=li: e.tensor_scalar(out=li.t[:], in0=li.t[:], scalar1=-6.0, scalar2=8.0,
                                                                          op0=ALU.max, op1=ALU.min), r=[li], w=[li])
                            kb.op("dve", lambda e, t1=t1, gl=gl, sg_=sg_: e.tensor_tensor(out=t1.t[:], in0=gl.t[:], in1=sg_.t[:],
                                                                                          op=ALU.mult), r=[gl, sg_], w=[t1])
                            kb.op("dve", lambda e, t1=t1, li=li: e.tensor_tensor(out=t1.t[:], in0=t1.t[:], in1=li.t[:],
                                                                                 op=ALU.mult), r=[t1, li], w=[t1])
                            kb.op("dve", lambda e, ab_=ab_, t1=t1, Gs=Gs: e.tensor_tensor(out=ab_.t[:], in0=t1.t[:], in1=Gs.t[:],
                                                                                          op=ALU.mult), r=[t1, Gs], w=[ab_])
                            acts.append(ab_)
                        for dc in range(8):
                            py = psy.next()
                            for hc in range(8):
                                kb.op("pe", lambda e, hc=hc, dc=dc, py=py: e.matmul(
                                    py.t[:], lhsT=w2e.t[:, hc, dc * 128:(dc + 1) * 128], rhs=acts[hc].t[:],
                                    start=(hc == 0), stop=(hc == 7)), r=[w2e, acts[hc]], w=[py])
                            kb.op("dve", lambda e, dc=dc, py=py, cs=cs: e.tensor_tensor(
                                out=acc.t[:, dc, cs], in0=py.t[:], in1=acc.t[:, dc, cs], op=ALU.add), r=[py, acc], w=[acc])
                kb.barrier()
            with ExitStack() as pf:
                x1r = Rot([kb.T(pf, [128, 8, 512], F32) for _ in range(2)])
                x2r = Rot([kb.T(pf, [128, 8, 512], F32) for _ in range(2)])
                sqr = Rot([kb.T(pf, [128, 512], F32) for _ in range(2)])
                rstd = kb.T(pf, [128, 512], F32)
                for gi in range(NPG):
                    cs = slice(gi * 512, (gi + 1) * 512)
                    gs_ = slice(c0 + gi * 512, c0 + (gi + 1) * 512)
                    x1 = x1r.next()
                    kb.dma("sp", x1.t[:], X1v[:, :, gs_], w=[x1])
                    x2 = x2r.next()
                    for dc in range(8):
                        kb.op("dve", lambda e, dc=dc, cs=cs: e.scalar_tensor_tensor(
                            out=x2.t[:, dc, :], in0=acc.t[:, dc, cs], scalar=mods.t[:, GT2 + dc:GT2 + dc + 1],
                            in1=x1.t[:, dc, :], op0=ALU.mult, op1=ALU.add), r=[acc, mods, x1], w=[x2])
                    if layer == 1:
                        ps = PS7
                        for k in range(8):
                            sq = sqr.next()
                            kb.op("act", lambda e, sq=sq, k=k: e.activation(out=sq.t[:], in_=x2.t[:, k, :], func=AF.Square),
                                  r=[x2], w=[sq])
                            kb.op("pe", lambda e, sq=sq, k=k: e.matmul(ps.t[:], lhsT=onesf.t[:], rhs=sq.t[:], start=(k == 0),
                                                                      stop=(k == 7)), r=[sq, onesf], w=[ps])
                        kb.op("dve", lambda e: e.tensor_scalar(out=rstd.t[:], in0=ps.t[:], scalar1=1.0 / D, scalar2=EPS,
                                                               op0=ALU.mult, op1=ALU.add), r=[ps], w=[rstd])
                        kb.op("act", lambda e: e.activation(out=rstd.t[:], in_=rstd.t[:], func=AF.Sqrt), r=[rstd], w=[rstd])
                        kb.op("dve", lambda e: e.reciprocal(out=rstd.t[:], in_=rstd.t[:]), r=[rstd], w=[rstd])
                        for k in range(8):
                            kb.op("dve", lambda e, k=k: e.scalar_tensor_tensor(
                                out=x2.t[:, k, :], in0=x2.t[:, k, :], scalar=fns.t[:, k:k + 1], in1=rstd.t[:],
                                op0=ALU.mult, op1=ALU.mult), r=[x2, fns, rstd], w=[x2])
                    kb.dma("sp", outv[:, :, gs_], x2.t[:], r=[x2])
                kb.barrier()
    kb.barrier()
    return nc, es, kb


def col8(v):
    return np.ascontiguousarray(np.asarray(v, np.float32).reshape(-1, 128).T)


def toep_vec(fn, dmin, W, shift):
    d = np.arange(W) + dmin + shift
    return fn(d).astype(np.float32)


def prep_common(p, pre, xb, cb, hf, S):
    perm = sigma_perm(S, hf)
    m = {}
    if xb is not None:
        m["xT"] = np.ascontiguousarray(xb[perm].T)
    m["ccol"] = col8(cb)
    m["ada_w"] = p[pre + "ada_w"]
    m["adab"] = col8(p[pre + "ada_b"])
    m["n1col"] = col8(p[pre + "norm1"])
    m["n2col"] = col8(p[pre + "norm2"])
    m["w_in"] = p[pre + "w_in"]
    m["w_out"] = p[pre + "w_out"]
    m["rw"] = p[pre + "router_w"]
    m["rbb"] = np.ascontiguousarray(np.tile(p[pre + "router_b"][None, :], (128, 1)))
    m["w1"] = p[pre + "w1"]
    m["b1col"] = np.ascontiguousarray(p[pre + "b1"].reshape(NE, 16, 128).transpose(2, 0, 1).reshape(128, NE * 16))
    m["w2"] = p[pre + "w2"]
    m["b2"] = p[pre + "b2"]
    m["ident"] = np.eye(128, dtype=np.float32)
    sel = np.zeros((NE, NE * 128), np.float32)
    for e in range(NE):
        sel[e, e * 128:(e + 1) * 128] = 1.0
    m["sel"] = sel
    m["rbflat"] = np.ascontiguousarray(np.tile(p["rel_bias"].reshape(1, 512), (128, 1)))
    return m


def prep_l0(p, xb, cb, hf, S):
    m = prep_common(p, "l0_", xb, cb, hf, S)
    rb = p["rel_bias"]
    m["fbb"] = np.ascontiguousarray(np.tile(p["l0_fox_fb"][None, :], (128, 1)))
    m["R"] = fox_R(hf)
    sh = 128 * (2 * hf - 1)
    dmin, W = toep_geom(-3, 0)
    causal = lambda d: np.where(d >= 0, 0.0, NEG)
    m["Fc"] = np.stack([toep_vec(causal, dmin, W, 0), toep_vec(causal, dmin, W, sh)])
    for g, (win, r) in enumerate(DIL_PAIRS):
        eo, et = dil_evals(win)
        dmin, W = toep_geom(min(eo + et), max(eo + et))
        rows = []
        for j in range(4):
            tab = rb[:, g * 4 + j]

            def fn(d, tab=tab, win=win, r=r):
                ok = (d >= 0) & (d <= win) & (d % r == 0)
                return np.where(ok, tab[t5_bucket_np(np.clip(d, 0, None))], NEG)
            rows.append(toep_vec(fn, dmin, W, 0))
            rows.append(toep_vec(fn, dmin, W, sh))
        m[f"Fd{g}"] = np.stack(rows)
    return m


def prep_l1(p, xb, cb, hf, S):
    m = prep_common(p, "l1_", xb, cb, hf, S)
    rb = p["rel_bias"]
    m["kvn"] = col8(p["l1_kv_norm"])
    m["w_ukv"] = p["l1_w_ukv"]
    m["fncol"] = col8(p["final_norm"])
    tri = np.where(np.arange(128)[None, :] <= np.arange(128)[:, None], 0.0, -1.0e9).astype(np.float32)
    m["trim"] = tri
    m["othd"] = np.full((128, 128), 0.0 if hf == 1 else -1.0e9, np.float32)
    m["pow2"] = np.ascontiguousarray(np.tile((2.0 ** -(np.arange(32) + 1.0))[None, :], (128, 1)).astype(np.float32))
    sh = 128 * (2 * hf - 1)
    dmin, W = toep_geom(-3, DSA_NEAR_E[-1])
    rows = []
    for h in range(16):
        tab = rb[:, h]
        fn = lambda d, tab=tab: np.where(d >= 0, tab[t5_bucket_np(np.clip(d, 0, None))], 0.0)
        rows.append(toep_vec(fn, dmin, W, 0))
        rows.append(toep_vec(fn, dmin, W, sh))
    m["Fs"] = np.stack(rows)
    m["rbfar"] = np.ascontiguousarray(np.tile(rb[31:32, :], (128, 1)))
    return m


_PROG = {}
PV0 = ("xT", "R", "Fc", "Fd0", "Fd1", "Fd2")


def build_fused(S):
    SO = S // 2
    nc = bass.Bass("TRN2", target_bir_lowering=False)
    es = ExitStack()
    kb = KB(nc, es)
    PS = [Tl(es.enter_context(nc.psum_tensor(f"ps{i}", [128, 512], F32))) for i in range(7)]
    ctx = (nc, es, kb, PS)
    shared = {}
    X1F = nc.dram_tensor("X1F", [D, S], F32, kind="Internal").ap()
    build_layer(0, S, ctx=ctx, tag="a_", shared=shared, pervar=PV0, out_ap=X1F[:, 0:SO])
    build_layer(0, S, ctx=ctx, tag="b_", shared=shared, pervar=PV0, out_ap=X1F[:, SO:S])
    build_layer(1, S, ctx=ctx, tag="c_", shared=shared, xT_in=X1F)
    es.close()
    return nc


def _get_prog(S):
    if S not in _PROG:
        _PROG[S] = build_fused(S)
    return _PROG[S]


def kernel(**inputs):
    p = {k: np.ascontiguousarray(np.asarray(v, dtype=np.float32)) for k, v in inputs.items()}
    x = p["x"]
    B, S = x.shape[0], x.shape[1]
    SO = S // 2
    n = 2 * B
    nc = _get_prog(S)
    maps = []
    for c in range(n):
        b, hf = c // 2, c % 2
        ma = prep_l0(p, x[b], p["c"][b], hf, S)
        mb = prep_l0(p, x[b], p["c"][b], 1 - hf, S)
        m1 = prep_l1(p, None, p["c"][b], hf, S)
        m = {}
        for k, v in ma.items():
            m[("a_" + k) if k in PV0 else ("w0_" + k)] = v
        for k in PV0:
            m["b_" + k] = mb[k]
        for k, v in m1.items():
            m["w1_" + k] = v
        maps.append(m)
    res = run_bass_kernel_spmd(nc, maps, core_ids=list(range(n)))
    out = np.empty_like(x)
    for c in range(n):
        own = sigma_perm(S, c % 2)[:SO]
        out[c // 2][own] = np.asarray(res.results[c]["c_out"], np.float32).T
    return out
```
